# Optimizing a Trainium2 kernel written in Bass

```python
import jax, jax.numpy as jnp
from jax import lax
import numpy as np

D_MODEL = 1024
BATCH = 4
SEQ = 4096
DEPTH = 4

N_MIXERS = 2
N_GLA = (DEPTH + 1) // 2
N_FOX = DEPTH // 2
N_DENSE = (DEPTH + 1) // 2
N_MOE = DEPTH // 2

GLA_HEADS = 4
GLA_DK = D_MODEL // 2
GLA_DV = D_MODEL
GLA_HK = GLA_DK // GLA_HEADS
GLA_HV = GLA_DV // GLA_HEADS
GLA_GATE_RANK = 16
GLA_GATE_TAU = 16.0
GLA_CHUNK = 64
GLA_IN_DIM = 2 * GLA_DK + 2 * GLA_DV + GLA_GATE_RANK

FOX_HEAD_DIM = 64
FOX_HEADS = D_MODEL // FOX_HEAD_DIM
FOX_Q_BLOCK = 128
FOX_IN_DIM = 4 * D_MODEL + FOX_HEADS
FOX_FORGET_BIAS = 3.0

D_FF = 7 * D_MODEL // 2
N_EXPERTS = 8
TOP_K = 2

EPS = 1e-6

kernel_name = "hybrid_gla_fox_moe_adaln"


def rms_norm(x, gain):
    xf = x.astype(jnp.float32)
    y = xf * lax.rsqrt(jnp.mean(xf * xf, axis=-1, keepdims=True) + EPS)
    return (y * gain.astype(jnp.float32)).astype(x.dtype)


def swiglu(h, w_gate, w_up, w_down):
    return (jax.nn.silu(h @ w_gate) * (h @ w_up)) @ w_down


def gla_chunked(q, k, v, log_a):
    B, H, T, dk = q.shape
    dv = v.shape[-1]
    n = T // GLA_CHUNK

    def chunks(t):
        return t.reshape(B, H, n, GLA_CHUNK, t.shape[-1]).transpose(2, 0, 1, 3, 4)

    qc, kc, vc = chunks(q), chunks(k), chunks(v)
    bc = jnp.cumsum(chunks(log_a), axis=-2)
    causal = jnp.tril(jnp.ones((GLA_CHUNK, GLA_CHUNK), dtype=bool))

    def step(S, inp):
        qi, ki, vi, bi = inp
        b_last = bi[:, :, -1, :]
        q_dec = qi * jnp.exp(bi)
        k_inv = ki * jnp.exp(-bi)
        k_to_end = ki * jnp.exp(b_last[:, :, None, :] - bi)
        attn = jnp.where(causal, jnp.einsum('bhtd,bhsd->bhts', q_dec, k_inv), 0.0)
        o = (jnp.einsum('bhts,bhsv->bhtv', attn, vi)
             + jnp.einsum('bhtd,bhdv->bhtv', q_dec, S))
        S = jnp.exp(b_last)[..., None] * S + jnp.einsum('bhsd,bhsv->bhdv', k_to_end, vi)
        return S, o

    S0 = jnp.zeros((B, H, dk, dv), jnp.float32)
    _, oc = lax.scan(step, S0, (qc, kc, vc, bc))
    return oc.transpose(1, 2, 0, 3, 4).reshape(B, H, T, dv)


def gla_mixer(h, w_in, w_gate_up, b_gate, o_norm, w_out):
    B, T, _ = h.shape
    proj = h @ w_in
    q, k, v, r, g_low = jnp.split(
        proj, [GLA_DK, 2 * GLA_DK, 2 * GLA_DK + GLA_DV, 2 * GLA_DK + 2 * GLA_DV], axis=-1)
    log_a = jax.nn.log_sigmoid((g_low @ w_gate_up + b_gate).astype(jnp.float32)) / GLA_GATE_TAU

    def heads(t, hd):
        return t.reshape(B, T, GLA_HEADS, hd).transpose(0, 2, 1, 3).astype(jnp.float32)

    o = gla_chunked(heads(q, GLA_HK) * (GLA_HK ** -0.5), heads(k, GLA_HK),
                    heads(v, GLA_HV), heads(log_a, GLA_HK))
    o = rms_norm(o, o_norm).transpose(0, 2, 1, 3).reshape(B, T, GLA_DV).astype(h.dtype)
    return (o * jax.nn.silu(r)) @ w_out


def fox_mixer(h, w_in, b_f, q_norm, k_norm, w_out):
    B, T, D = h.shape
    proj = h @ w_in
    q, k, v, g, f = jnp.split(proj, [D, 2 * D, 3 * D, 4 * D], axis=-1)
    log_f = jax.nn.log_sigmoid((f + b_f).astype(jnp.float32))
    F = jnp.cumsum(log_f, axis=1).transpose(0, 2, 1)

    def heads(t):
        return t.reshape(B, T, FOX_HEADS, FOX_HEAD_DIM).transpose(0, 2, 1, 3)

    q = rms_norm(heads(q), q_norm)
    k = rms_norm(heads(k), k_norm)
    v = heads(v)
    scale = FOX_HEAD_DIM ** -0.5
    outs = []
    for blk in range(T // FOX_Q_BLOCK):
        s0 = blk * FOX_Q_BLOCK
        s1 = s0 + FOX_Q_BLOCK
        logits = (jnp.einsum('bhqd,bhkd->bhqk', q[:, :, s0:s1], k[:, :, :s1]).astype(jnp.float32)
                  * scale + F[:, :, s0:s1, None] - F[:, :, None, :s1])
        mask = (s0 + jnp.arange(FOX_Q_BLOCK))[:, None] >= jnp.arange(s1)[None, :]
        p = jax.nn.softmax(jnp.where(mask, logits, -jnp.inf), axis=-1)
        outs.append(jnp.einsum('bhqk,bhkd->bhqd', p.astype(v.dtype), v[:, :, :s1]))
    o = jnp.concatenate(outs, axis=2).transpose(0, 2, 1, 3).reshape(B, T, D)
    return (o * jax.nn.sigmoid(g)) @ w_out


def moe_swiglu(h, w_router, b_router, w_gate, w_up, w_down):
    logits = (h @ w_router).astype(jnp.float32) + b_router.astype(jnp.float32)
    top_vals, top_idx = lax.top_k(logits, TOP_K)
    top_w = jax.nn.softmax(top_vals, axis=-1)
    combine = jnp.sum(jax.nn.one_hot(top_idx, N_EXPERTS, dtype=jnp.float32)
                      * top_w[..., None], axis=-2)
    y = jnp.zeros_like(h)
    for e in range(N_EXPERTS):
        y = y + combine[..., e:e + 1].astype(h.dtype) * swiglu(h, w_gate[e], w_up[e], w_down[e])
    return y


def setup_inputs(seed: int = 0) -> dict:
    key = jax.random.key(seed)
    ks = jax.random.split(key, 24)
    D = D_MODEL
    f32 = jnp.float32

    def nrm(k, shape, fan_in, s=1.0):
        return (s * fan_in ** -0.5) * jax.random.normal(k, shape, f32)

    def rn(k, shape):
        return jax.random.normal(k, shape, f32)

    return {
        "x": rn(ks[0], (BATCH, SEQ, D)),
        "c": rn(ks[1], (BATCH, D)),
        "ada_w": nrm(ks[2], (DEPTH, D, 6 * D), D, 0.5),
        "ada_b": 0.02 * rn(ks[3], (DEPTH, 6 * D)),
        "norm_gain": 1.0 + 0.1 * rn(ks[4], (DEPTH, 2, D)),
        "gla_w_in": nrm(ks[5], (N_GLA, D, GLA_IN_DIM), D),
        "gla_w_gate_up": nrm(ks[6], (N_GLA, GLA_GATE_RANK, GLA_DK), GLA_GATE_RANK),
        "gla_b_gate": 0.1 * rn(ks[7], (N_GLA, GLA_DK)),
        "gla_o_norm": 1.0 + 0.1 * rn(ks[8], (N_GLA, GLA_HV)),
        "gla_w_out": nrm(ks[9], (N_GLA, GLA_DV, D), GLA_DV),
        "fox_w_in": nrm(ks[10], (N_FOX, D, FOX_IN_DIM), D),
        "fox_b_f": FOX_FORGET_BIAS + 0.1 * rn(ks[11], (N_FOX, FOX_HEADS)),
        "fox_q_norm": 1.0 + 0.1 * rn(ks[12], (N_FOX, FOX_HEAD_DIM)),
        "fox_k_norm": 1.0 + 0.1 * rn(ks[13], (N_FOX, FOX_HEAD_DIM)),
        "fox_w_out": nrm(ks[14], (N_FOX, D, D), D),
        "ffn_w_gate": nrm(ks[15], (N_DENSE, D, D_FF), D),
        "ffn_w_up": nrm(ks[16], (N_DENSE, D, D_FF), D),
        "ffn_w_down": nrm(ks[17], (N_DENSE, D_FF, D), D_FF),
        "moe_w_router": nrm(ks[18], (N_MOE, D, N_EXPERTS), D),
        "moe_b_router": 0.01 * rn(ks[19], (N_MOE, N_EXPERTS)),
        "moe_w_gate": nrm(ks[20], (N_MOE, N_EXPERTS, D, D_FF), D),
        "moe_w_up": nrm(ks[21], (N_MOE, N_EXPERTS, D, D_FF), D),
        "moe_w_down": nrm(ks[22], (N_MOE, N_EXPERTS, D_FF, D), D_FF),
    }


def reference(x, c, ada_w, ada_b, norm_gain, gla_w_in, gla_w_gate_up, gla_b_gate, gla_o_norm,
              gla_w_out, fox_w_in, fox_b_f, fox_q_norm, fox_k_norm, fox_w_out, ffn_w_gate,
              ffn_w_up, ffn_w_down, moe_w_router, moe_b_router, moe_w_gate, moe_w_up,
              moe_w_down):
    cond = jax.nn.silu(c)
    for i in range(DEPTH):
        j = i // N_MIXERS
        mod = (cond @ ada_w[i] + ada_b[i])[:, None, :]
        sh1, sc1, g1, sh2, sc2, g2 = jnp.split(mod, 6, axis=-1)

        h = rms_norm(x, norm_gain[i, 0]) * (1.0 + sc1) + sh1
        if i % N_MIXERS == 0:
            mix = gla_mixer(h, gla_w_in[j], gla_w_gate_up[j], gla_b_gate[j], gla_o_norm[j],
                            gla_w_out[j])
        else:
            mix = fox_mixer(h, fox_w_in[j], fox_b_f[j], fox_q_norm[j], fox_k_norm[j],
                            fox_w_out[j])
        x = x + g1 * mix

        h = rms_norm(x, norm_gain[i, 1]) * (1.0 + sc2) + sh2
        if i % 2 == 0:
            ffn = swiglu(h, ffn_w_gate[j], ffn_w_up[j], ffn_w_down[j])
        else:
            ffn = moe_swiglu(h, moe_w_router[j], moe_b_router[j], moe_w_gate[j], moe_w_up[j],
                             moe_w_down[j])
        x = x + g2 * ffn
    return x
```

```python
from contextlib import ExitStack


class Tok:
    __slots__ = ("sem", "count", "eng")

    def __init__(self, sem=None, count=None, eng=None):
        self.sem = sem
        self.count = count
        self.eng = eng


class Tracker:
    ROLL = 30000

    def __init__(self, nc, stack: ExitStack):
        self.nc = nc
        self.stack = stack
        self.engs = {"pe": nc.tensor, "act": nc.scalar, "dve": nc.vector,
                     "pool": nc.gpsimd, "sp": nc.sync}
        self.sem = {}
        self.cnt = {}
        self.nsem = 0
        for e in self.engs:
            self._new_sem(e)
        self.waited = {e: {} for e in self.engs}
        self.last_w = {}
        self.readers = {}
        self.pending = {e: [] for e in self.engs}
        self.rings = {}
        self.n_ins = 0

    def _alloc(self, name):
        self.nsem += 1
        return self.stack.enter_context(self.nc.semaphore(f"{name}_{self.nsem}"))

    def _new_sem(self, e):
        self.sem[e] = self._alloc(f"e_{e}")
        self.cnt[e] = 0

    def _wait(self, eng, tok):
        if tok is None:
            return
        if tok.eng == eng and eng == "pe":
            return
        assert tok.count is not None, "dependency on an unsignaled instruction"
        w = self.waited[eng]
        k = id(tok.sem)
        if w.get(k, 0) >= tok.count:
            return
        self.engs[eng].wait_ge(tok.sem, tok.count)
        w[k] = tok.count

    def _deps(self, reads, writes):
        deps = []
        for b in reads:
            t = self.last_w.get(b)
            if t is not None:
                deps.append(t)
        for b in writes:
            t = self.last_w.get(b)
            if t is not None:
                deps.append(t)
            deps.extend(self.readers.get(b, ()))
        return deps

    def _record(self, tok, reads, writes):
        for b in reads:
            self.readers.setdefault(b, []).append(tok)
        for b in writes:
            self.last_w[b] = tok
            self.readers[b] = []

    def op(self, eng, fn, reads=(), writes=(), signal=True):
        for t in self._deps(reads, writes):
            self._wait(eng, t)
        ins = fn(self.engs[eng])
        self.n_ins += 1
        if signal:
            if self.cnt[eng] >= self.ROLL:
                self._new_sem(eng)
            self.cnt[eng] += 1
            ins.then_inc(self.sem[eng], 1)
            tok = Tok(self.sem[eng], self.cnt[eng], eng)
            for p in self.pending[eng]:
                p.sem, p.count = tok.sem, tok.count
            self.pending[eng] = []
        else:
            tok = Tok(None, None, eng)
            self.pending[eng].append(tok)
        self._record(tok, reads, writes)
        return tok

    def dma(self, eng, fn, reads=(), writes=(), ring="d", nring=4):
        r = self.rings.get(ring)
        if r is None:
            r = self.rings[ring] = {"sems": [self._alloc(f"r_{ring}") for _ in range(nring)],
                                    "cnt": [0] * nring, "i": 0}
        i = r["i"]
        r["i"] = (i + 1) % len(r["sems"])
        sem = r["sems"][i]
        if r["cnt"][i] > 0:
            self._wait(eng, Tok(sem, r["cnt"][i], "dma"))
        for t in self._deps(reads, writes):
            self._wait(eng, t)
        ins = fn(self.engs[eng])
        self.n_ins += 1
        r["cnt"][i] += 16
        ins.then_inc(sem, 16)
        tok = Tok(sem, r["cnt"][i], "dma")
        self._record(tok, reads, writes)
        return tok

    def drain(self, eng="sp"):
        for r in self.rings.values():
            for sem, c in zip(r["sems"], r["cnt"]):
                if c:
                    self._wait(eng, Tok(sem, c, "dma"))
        for e in self.engs:
            assert not self.pending[e], f"unsignaled tail on {e}"
            if self.cnt[e]:
                self._wait(eng, Tok(self.sem[e], self.cnt[e], e))


import numpy as np
from contextlib import ExitStack
import concourse.bass as bass
import concourse.mybir as mybir

F32 = mybir.dt.float32
BF16 = mybir.dt.bfloat16
AF = mybir.ActivationFunctionType
ALU = mybir.AluOpType
AX = mybir.AxisListType

D = 1024
KC = 8
DFF = 3584
NE = 8
EPS = 1e-6

C_ID, C_MASK, C_TRI, C_REV, C_BD, C_ONES, C_SEL = 0, 128, 256, 386, 514, 642, 770
NCONST = 898


def make_consts():
    c = np.zeros((128, NCONST), np.float32)
    s = np.arange(128)[:, None]
    t = np.arange(128)[None, :]
    c[:, C_ID:C_ID + 128] = (s == t)
    c[:, C_MASK:C_MASK + 128] = (s <= t)
    c[:, C_TRI:C_TRI + 128] = (s <= t) / 16.0
    c[:, C_TRI + 128] = 1.0 / 16.0
    c[:, C_REV:C_REV + 128] = (s > t) / 16.0
    c[:, C_BD:C_BD + 128] = (s // 64 == t // 64)
    c[:, C_ONES:C_ONES + 128] = 1.0
    c[127, C_SEL:C_SEL + 128] = 1.0
    return c


SP = {}
_o = 0
for _n, _w in [("cT", 8), ("adab", 4 * 48), ("ngain", 4 * 2 * 8), ("wgu", 2 * 512), ("onorm", 2 * 2),
               ("bf", 2 * 16), ("qn", 2), ("kn", 2), ("wr", 2 * 8 * 8), ("br", 2 * 8)]:
    SP[_n] = (_o, _w)
    _o += _w
NSP = _o


def pack_small(inp, b):
    sp = np.zeros((128, NSP), np.float32)

    def put(name, arr):
        o, w = SP[name]
        arr = np.asarray(arr, np.float32).reshape(arr.shape[0], -1)
        assert arr.shape[1] == w, (name, arr.shape, w)
        sp[:arr.shape[0], o:o + w] = arr

    fm = lambda v, n: np.asarray(v, np.float32).reshape(n, 128).T
    put("cT", fm(inp["c"][b], 8))
    put("adab", np.stack([fm(inp["ada_b"][l], 48) for l in range(4)], 1))
    put("ngain", np.stack([np.stack([fm(inp["norm_gain"][l, s], 8) for s in range(2)], 1) for l in range(4)], 1))
    wgu = np.zeros((32, 2, 512), np.float32)
    for j in range(2):
        wgu[:16, j] = inp["gla_w_gate_up"][j]
        wgu[16, j] = inp["gla_b_gate"][j]
    put("wgu", wgu)
    put("onorm", np.stack([fm(inp["gla_o_norm"][j], 2) for j in range(2)], 1))
    put("bf", np.asarray(inp["fox_b_f"], np.float32).reshape(1, 32))
    put("qn", np.stack([np.tile(inp["fox_q_norm"][j], 2) for j in range(2)], 1))
    put("kn", np.stack([np.tile(inp["fox_k_norm"][j], 2) for j in range(2)], 1))
    put("wr", np.stack([np.asarray(inp["moe_w_router"][j]).reshape(8, 128, 8).transpose(1, 0, 2) for j in range(2)], 1))
    put("br", np.asarray(inp["moe_b_router"], np.float32).reshape(1, 16))
    return sp


class K:
    def __init__(self, T, dbg=()):
        self.T = T
        self.G = min(2048, T)
        self.dbg = set(dbg)
        nc = self.nc = bass.Bass("TRN2", target_bir_lowering=False)
        dt = nc.dram_tensor
        self.x = dt("x", [T, D], F32, kind="ExternalInput").ap()
        self.consts_d = dt("consts", [128, NCONST], F32, kind="ExternalInput").ap()
        self.sp_d = dt("sp", [128, NSP], F32, kind="ExternalInput").ap()
        self.w = {}
        self.wshape = {"ada_w": [D, 6 * D], "gla_w_in": [D, 3088], "gla_w_out": [D, D],
                       "fox_w_in": [D, 4112], "fox_w_out": [D, D],
                       "ffn_w_gate": [D, DFF], "ffn_w_up": [D, DFF], "ffn_w_down": [DFF, D],
                       "moe_w_gate": [NE, D, DFF], "moe_w_up": [NE, D, DFF], "moe_w_down": [NE, DFF, D]}
        self.out = dt("out", [T, D], F32, kind="ExternalOutput").ap()
        self.xsT = dt("xsT", [D, T], F32).ap()
        self.xsT_v = self.xsT.rearrange("(c p) t -> p c t", p=128)
        self.qs = dt("qs", [D, T], BF16).ap()
        self.ks = dt("ks", [D, T], BF16).ap()
        self.vs = dt("vs", [T, D], BF16).ap()
        self.gs = dt("gs", [D, T], BF16).ap()
        self.os_ = dt("os", [D, T], BF16).ap()
        self.dbg_out = {}

    def wt(self, name, j):
        key = f"{name}_{j}"
        if key not in self.w:
            self.w[key] = self.nc.dram_tensor(key, self.wshape[name], F32, kind="ExternalInput").ap()
        return self.w[key]

    def sb(self, st, name, shape, dtype):
        self.uid = getattr(self, "uid", 0) + 1
        return st.enter_context(self.nc.sbuf_tensor(f"{name}_u{self.uid}", shape, dtype))

    def setup(self, st):
        nc = self.nc
        self.tk = tk = Tracker(nc, st)
        self.c32 = self.sb(st, "c32", [128, NCONST], F32)
        self.spt = self.sb(st, "spt", [128, NSP], F32)
        self.cbf = self.sb(st, "cbf", [128, NCONST], BF16)
        self.mod = self.sb(st, "mod", [128, 48], F32)
        self.A = self.sb(st, "modA", [128, 2, 8], F32)
        self.ps = [st.enter_context(nc.psum_tensor(f"ps{i}", [128, 512], F32)) for i in range(8)]
        self.Fk = self.sb(st, "Fk", [128, self.T // 128, 16], F32)
        self.Fref = self.sb(st, "Fref", [128, max(1, self.T // 512), 16], F32)
        self.bfb = self.sb(st, "bfb", [1, 32], BF16)
        tk.dma("sp", lambda e: e.dma_start(out=self.c32[:], in_=self.consts_d), writes=["c32"], ring="c")
        tk.dma("sp", lambda e: e.dma_start(out=self.spt[:], in_=self.sp_d), writes=["spt"], ring="c")
        tk.op("dve", lambda e: e.tensor_copy(self.cbf[:], self.c32[:]), reads=["c32"], writes=["cbf"])
        ob, _ = SP["bf"]
        tk.op("dve", lambda e: e.tensor_copy(self.bfb[:], self.spt[0:1, ob:ob + 32]), reads=["spt"], writes=["bfb"])

    def spv(self, name):
        o, w = SP[name]
        return self.spt[:, o:o + w]

    def barrier(self):
        tk = self.tk
        tk.drain("sp")
        self.nc.all_engine_barrier()
        tk.last_w.clear()
        tk.readers.clear()

    def phase_in(self):
        tk, nc = self.tk, self.nc
        with ExitStack() as st:
            xin = [self.sb(st, f"xin{i}", [128, D], F32) for i in range(2)]
            xT = [self.sb(st, f"xTi{i}", [128, 8, 128], F32) for i in range(2)]
            ident = self.c32[:, C_ID:C_ID + 128]
            for tt in range(self.T // 128):
                b = tt % 2
                tk.dma("sp", lambda e: e.dma_start(out=xin[b][:], in_=self.x[tt * 128:(tt + 1) * 128, :]),
                       writes=[("xin", b)], ring="xin", nring=2)
                for half in range(2):
                    pb = (tt * 2 + half) % 8
                    for j in range(4):
                        c = half * 4 + j
                        tk.op("pe", lambda e: e.transpose(self.ps[pb][:, j * 128:(j + 1) * 128],
                                                          xin[b][:, c * 128:(c + 1) * 128], ident),
                              reads=[("xin", b), "c32"], writes=[("ps", pb)], signal=(j == 3))
                    src = self.ps[pb][:].rearrange("p (c t) -> p c t", c=4)
                    dst = xT[b][:, half * 4:(half + 1) * 4, :]
                    if half == 0:
                        tk.op("dve", lambda e: e.tensor_copy(dst, src), reads=[("ps", pb)], writes=[("xTi", b, half)])
                    else:
                        tk.op("act", lambda e: e.activation(out=dst, in_=src, func=AF.Copy),
                              reads=[("ps", pb)], writes=[("xTi", b, half)])
                tk.dma("sp", lambda e: e.dma_start(out=self.xsT_v[:, :, tt * 128:(tt + 1) * 128], in_=xT[b][:]),
                       reads=[("xTi", b, 0), ("xTi", b, 1)], writes=["xsT"], ring="xst", nring=2)
            self.barrier()

    def phase_out(self):
        tk, nc = self.tk, self.nc
        with ExitStack() as st:
            xin = [self.sb(st, f"xo{i}", [128, D], F32) for i in range(2)]
            xT = [self.sb(st, f"xTo{i}", [128, 8, 128], F32) for i in range(2)]
            ident = self.c32[:, C_ID:C_ID + 128]
            for tt in range(self.T // 128):
                b = tt % 2
                tk.dma("sp", lambda e: e.dma_start(out=xT[b][:], in_=self.xsT_v[:, :, tt * 128:(tt + 1) * 128]),
                       reads=["xsT"], writes=[("xTo", b)], ring="xld", nring=2)
                for half in range(2):
                    pb = (tt * 2 + half) % 8
                    for j in range(4):
                        c = half * 4 + j
                        tk.op("pe", lambda e: e.transpose(self.ps[pb][:, j * 128:(j + 1) * 128], xT[b][:, c, :], ident),
                              reads=[("xTo", b), "c32"], writes=[("ps", pb)], signal=(j == 3))
                    dst = xin[b][:, half * 512:(half + 1) * 512]
                    if half == 0:
                        tk.op("dve", lambda e: e.tensor_copy(dst, self.ps[pb][:]), reads=[("ps", pb)], writes=[("xo", b, half)])
                    else:
                        tk.op("act", lambda e: e.activation(out=dst, in_=self.ps[pb][:], func=AF.Copy),
                              reads=[("ps", pb)], writes=[("xo", b, half)])
                tk.dma("sp", lambda e: e.dma_start(out=self.out[tt * 128:(tt + 1) * 128, :], in_=xin[b][:]),
                       reads=[("xo", b, 0), ("xo", b, 1)], writes=[("out", tt)], ring="ost", nring=2)
            self.barrier()

    def phase_mod(self, l):
        tk, nc = self.tk, self.nc
        with ExitStack() as st:
            cond = self.sb(st, "cond", [128, 8], BF16)
            wa = [self.sb(st, f"wa{i}", [128, 8, 1536], BF16) for i in range(2)]
            tk.op("act", lambda e: e.activation(out=cond[:], in_=self.spv("cT"), func=AF.Silu),
                  reads=["spt"], writes=["cond"])
            wv = self.wt("ada_w", l).rearrange("(k p) j -> p k j", p=128)
            pm = self.ps[0]
            for q in range(4):
                b = q % 2
                tk.dma("pool", lambda e: e.dma_start(out=wa[b][:], in_=wv[:, :, q * 1536:(q + 1) * 1536]),
                       writes=[("wa", b)], ring="wa", nring=2)
                for jc in range(12):
                    col = q * 12 + jc
                    for k in range(8):
                        tk.op("pe", lambda e: e.matmul(pm[:, col:col + 1], wa[b][:, k, jc * 128:(jc + 1) * 128],
                                                       cond[:, k:k + 1], start=(k == 0), stop=(k == 7)),
                              reads=[("wa", b), "cond"], writes=[("ps", 0)], signal=(k == 7 and jc == 11))
            o, _ = SP["adab"]
            tk.op("dve", lambda e: e.tensor_tensor(self.mod[:], pm[:, 0:48], self.spt[:, o + l * 48:o + (l + 1) * 48], ALU.add),
                  reads=[("ps", 0), "spt"], writes=["mod"])
            og, _ = SP["ngain"]
            for s in range(2):
                sc = self.mod[:, 8:16] if s == 0 else self.mod[:, 32:40]
                gain = self.spt[:, og + (l * 2 + s) * 8:og + (l * 2 + s + 1) * 8]
                tk.op("dve", lambda e: e.scalar_tensor_tensor(self.A[:, s, :], sc, 1.0, gain, ALU.add, ALU.mult),
                      reads=["mod", "spt"], writes=["modA"])
            self.barrier()

    def shift(self, s):
        return self.mod[:, 0:8] if s == 0 else self.mod[:, 24:32]

    def gate(self, s):
        return self.mod[:, 16:24] if s == 0 else self.mod[:, 40:48]

    def norm_h(self, s, xT, xkey, n, sq, xr, rstd, outs, okeys, pbank, h32=None):
        tk = self.tk
        ones = self.cbf[:, C_ONES:C_ONES + 128]
        tk.op("act", lambda e: e.activation(out=sq[:, :, :n], in_=xT, func=AF.Square),
              reads=[xkey], writes=["sq"])
        for c in range(8):
            tk.op("pe", lambda e: e.matmul(self.ps[pbank][:, :n], ones, sq[:, c, :n], start=(c == 0), stop=(c == 7)),
                  reads=["sq", "cbf"], writes=[("ps", pbank)], signal=(c == 7))
        tk.op("act", lambda e: e.activation(out=rstd[:, :n], in_=self.ps[pbank][:, :n], func=AF.Sqrt, bias=EPS, scale=1.0 / D),
              reads=[("ps", pbank)], writes=["rstd"])
        tk.op("dve", lambda e: e.reciprocal(rstd[:, :n], rstd[:, :n]), reads=["rstd"], writes=["rstd"])
        rb = rstd[:, :n].unsqueeze(1).broadcast_to([128, 8, n])
        tk.op("dve", lambda e: e.tensor_tensor(xr[:, :, :n], xT, rb, ALU.mult),
              reads=[xkey, "rstd"], writes=["xr"])
        sh = self.shift(s)
        for c in range(8):
            dst = outs[c] if h32 is None else h32[:, c, :n]
            tk.op("act", lambda e: e.activation(out=dst, in_=xr[:, c, :n], func=AF.Identity,
                                                bias=sh[:, c:c + 1], scale=self.A[:, s, c:c + 1]),
                  reads=["xr", "mod", "modA"], writes=[okeys[c]] if h32 is None else [("h32", c)])
            if h32 is not None:
                tk.op("dve", lambda e: e.tensor_copy(outs[c], h32[:, c, :n]), reads=[("h32", c)], writes=[okeys[c]])

    def phase_ffn(self, l, moe):
        tk, nc, G = self.tk, self.nc, self.G
        j = l // 2
        NT = G // 512
        with ExitStack() as st:
            xT = self.sb(st, "f_xT", [128, 8, G], F32)
            hT = self.sb(st, "f_hT", [128, 8, G], BF16)
            wg = [self.sb(st, f"f_wg{i}", [128, 8, 512], BF16) for i in range(2)]
            wu = [self.sb(st, f"f_wu{i}", [128, 8, 512], BF16) for i in range(2)]
            wd = [self.sb(st, f"f_wd{i}", [128, 4, D], BF16) for i in range(2)]
            if moe:
                comb = self.sb(st, "f_comb", [128, G // 128, 8], F32)
                wr = self.spv("wr").rearrange("p (j k e) -> p j k e", j=2, k=8)
                obr, _ = SP["br"]
            g2 = self.gate(1)
            nexp = NE if moe else 1
            ident = self.c32[:, C_ID:C_ID + 128]
            ones32 = self.c32[:, C_ONES:C_ONES + 128]
            wi = 0
            for grp in range(self.T // G):
                t0 = grp * G
                tk.dma("sp", lambda e: e.dma_start(out=xT[:], in_=self.xsT_v[:, :, t0:t0 + G]),
                       reads=["xsT"], writes=[("fx", tt) for tt in range(NT)], ring="fxl", nring=1)
                with ExitStack() as st2:
                    sq = self.sb(st2, "f_sq", [128, 8, 512], BF16)
                    xr = self.sb(st2, "f_xr", [128, 8, 512], F32)
                    rstd = self.sb(st2, "f_rstd", [128, 512], F32)
                    if moe:
                        lg = self.sb(st2, "f_lg", [128, 8], F32)
                        mx = self.sb(st2, "f_mx", [128, 8], F32)
                        tmp8 = self.sb(st2, "f_tmp8", [128, 8], F32)
                        msk = self.sb(st2, "f_msk", [128, 8], F32)
                        nl1 = self.sb(st2, "f_nl1", [128, 1], F32)
                        den = self.sb(st2, "f_den", [128, 1], F32)
                        h32 = xr
                    for tt in range(NT):
                        cs = slice(tt * 512, (tt + 1) * 512)
                        self.norm_h(1, xT[:, :, cs], ("fx", tt), 512, sq, xr, rstd,
                                    [hT[:, c, cs] for c in range(8)], [("fh", tt)] * 8, 7,
                                    h32=(h32 if moe else None))
                        if moe:
                            for s4 in range(4):
                                si = tt * 4 + s4
                                for k in range(8):
                                    tk.op("pe", lambda e: e.matmul(self.ps[6][:, 0:8], h32[:, k, s4 * 128:(s4 + 1) * 128],
                                                                   wr[:, j, k, :], start=(k == 0), stop=False),
                                          reads=[("h32", k), "spt"], writes=[("ps", 6)], signal=False)
                                tk.op("pe", lambda e: e.matmul(self.ps[6][:, 0:8], ones32[0:1, :],
                                                               self.spt[0:1, obr + j * 8:obr + (j + 1) * 8], start=False, stop=True),
                                      reads=["c32", "spt"], writes=[("ps", 6)])
                                tk.op("dve", lambda e: e.tensor_copy(lg[:], self.ps[6][:, 0:8]), reads=[("ps", 6)], writes=["lg"])
                                tk.op("dve", lambda e: e.max(mx[:], lg[:]), reads=["lg"], writes=["mx"])
                                tk.op("dve", lambda e: e.tensor_scalar(msk[:], lg[:], mx[:, 1:2], None, ALU.is_ge),
                                      reads=["lg", "mx"], writes=["msk"])
                                tk.op("dve", lambda e: e.tensor_scalar(nl1[:], mx[:, 0:1], -1.0, None, ALU.mult),
                                      reads=["mx"], writes=["nl1"])
                                tk.op("act", lambda e: e.activation(out=tmp8[:], in_=lg[:], func=AF.Exp, bias=nl1[:, 0:1], scale=1.0),
                                      reads=["lg", "nl1"], writes=["tmp8"])
                                tk.op("dve", lambda e: e.tensor_tensor(tmp8[:], tmp8[:], msk[:], ALU.mult),
                                      reads=["tmp8", "msk"], writes=["tmp8"])
                                tk.op("dve", lambda e: e.reduce_sum(den[:], tmp8[:], AX.X), reads=["tmp8"], writes=["den"])
                                tk.op("dve", lambda e: e.reciprocal(den[:], den[:]), reads=["den"], writes=["den"])
                                tk.op("dve", lambda e: e.tensor_scalar(comb[:, si, :], tmp8[:], den[:, 0:1], None, ALU.mult),
                                      reads=["tmp8", "den"], writes=["comb"])
                    self.barrier()
                with ExitStack() as st3:
                    hid = self.sb(st3, "f_hid", [128, 4, G], BF16)
                    sg = [self.sb(st3, f"f_sg{i}", [128, 512], F32) for i in range(2)]
                    if moe:
                        combB = [self.sb(st3, f"f_combB{i}", [128, G], F32) for i in range(2)]
                        dg = self.sb(st3, "f_dg", [128, 128], F32)
                    for ex in range(nexp):
                        if moe:
                            cb = combB[ex % 2]
                            ckey = ("combB", ex % 2)
                            for si in range(G // 128):
                                tk.op("dve", lambda e: e.tensor_scalar(dg[:], ident, comb[:, si, ex:ex + 1], None, ALU.mult),
                                      reads=["comb", "c32"], writes=["dg"])
                                pb = 6
                                tk.op("pe", lambda e: e.matmul(self.ps[pb][:, 0:128], ones32, dg[:], start=True, stop=True),
                                      reads=["dg", "c32"], writes=[("ps", pb)])
                                tk.op("act", lambda e: e.activation(out=cb[:, si * 128:(si + 1) * 128], in_=self.ps[pb][:, 0:128], func=AF.Copy),
                                      reads=[("ps", pb)], writes=[ckey])
                            Wg = self.wt("moe_w_gate", j)[ex]
                            Wu = self.wt("moe_w_up", j)[ex]
                            Wd = self.wt("moe_w_down", j)[ex]
                        else:
                            Wg = self.wt("ffn_w_gate", j)
                            Wu = self.wt("ffn_w_up", j)
                            Wd = self.wt("ffn_w_down", j)
                        Wg = Wg.rearrange("(k p) f -> p k f", p=128)
                        Wu = Wu.rearrange("(k p) f -> p k f", p=128)
                        Wd = Wd.rearrange("(k p) o -> p k o", p=128)
                        for fc in range(DFF // 512):
                            b = wi % 2
                            wi += 1
                            fs = slice(fc * 512, (fc + 1) * 512)
                            tk.dma("pool", lambda e: e.dma_start(out=wg[b][:], in_=Wg[:, :, fs]), writes=[("wg", b)], ring="wg", nring=2)
                            tk.dma("pool", lambda e: e.dma_start(out=wu[b][:], in_=Wu[:, :, fs]), writes=[("wu", b)], ring="wu", nring=2)
                            tk.dma("pool", lambda e: e.dma_start(out=wd[b][:], in_=Wd[:, fc * 4:(fc + 1) * 4, :]), writes=[("wd", b)], ring="wd", nring=2)
                            it = 0
                            for ft in range(4):
                                for tt in range(NT):
                                    cs = slice(tt * 512, (tt + 1) * 512)
                                    pg, pu = (0, 1) if it % 2 == 0 else (2, 3)
                                    sgt = sg[it % 2]
                                    skey = ("sg", it % 2)
                                    it += 1
                                    for k in range(8):
                                        tk.op("pe", lambda e: e.matmul(self.ps[pg][:], wg[b][:, k, ft * 128:(ft + 1) * 128], hT[:, k, cs],
                                                                       start=(k == 0), stop=(k == 7)),
                                              reads=[("wg", b), ("fh", tt)], writes=[("ps", pg)], signal=(k == 7))
                                    for k in range(8):
                                        tk.op("pe", lambda e: e.matmul(self.ps[pu][:], wu[b][:, k, ft * 128:(ft + 1) * 128], hT[:, k, cs],
                                                                       start=(k == 0), stop=(k == 7)),
                                              reads=[("wu", b), ("fh", tt)], writes=[("ps", pu)], signal=(k == 7))
                                    tk.op("act", lambda e: e.activation(out=sgt[:], in_=self.ps[pg][:], func=AF.Silu),
                                          reads=[("ps", pg)], writes=[skey])
                                    if moe:
                                        tk.op("dve", lambda e: e.tensor_tensor(sgt[:], sgt[:], self.ps[pu][:], ALU.mult),
                                              reads=[skey, ("ps", pu)], writes=[skey])
                                        tk.op("dve", lambda e: e.tensor_tensor(hid[:, ft, cs], sgt[:], cb[:, cs], ALU.mult),
                                              reads=[skey, ckey], writes=[("hid", ft, tt)])
                                    else:
                                        tk.op("dve", lambda e: e.tensor_tensor(hid[:, ft, cs], sgt[:], self.ps[pu][:], ALU.mult),
                                              reads=[skey, ("ps", pu)], writes=[("hid", ft, tt)])
                            it = 0
                            for oc in range(8):
                                for tt in range(NT):
                                    cs = slice(tt * 512, (tt + 1) * 512)
                                    py = 4 + it % 2
                                    it += 1
                                    for ft in range(4):
                                        tk.op("pe", lambda e: e.matmul(self.ps[py][:], wd[b][:, ft, oc * 128:(oc + 1) * 128], hid[:, ft, cs],
                                                                       start=(ft == 0), stop=(ft == 3)),
                                              reads=[("wd", b), ("hid", ft, tt)], writes=[("ps", py)], signal=(ft == 3))
                                    tk.op("dve", lambda e: e.scalar_tensor_tensor(xT[:, oc, cs], self.ps[py][:], g2[:, oc:oc + 1], xT[:, oc, cs],
                                                                                  ALU.mult, ALU.add),
                                          reads=[("ps", py), ("fx", tt), "mod"], writes=[("fx", tt)])
                    tk.dma("sp", lambda e: e.dma_start(out=self.xsT_v[:, :, t0:t0 + G], in_=xT[:]),
                           reads=[("fx", tt) for tt in range(NT)], writes=["xsT"], ring="fxs", nring=1)
                    self.barrier()

    def out_proj_residual(self, wo, gated, gkey, xT, xkey, n, banks=(0, 1)):
        tk = self.tk
        g1 = self.gate(0)
        for oc in range(8):
            pb = banks[oc % len(banks)]
            for k in range(8):
                tk.op("pe", lambda e: e.matmul(self.ps[pb][:, :n], wo[:, k, oc * 128:(oc + 1) * 128], gated[:, k, :n],
                                               start=(k == 0), stop=(k == 7)),
                      reads=["wo", gkey], writes=[("ps", pb)], signal=(k == 7))
            tk.op("dve", lambda e: e.scalar_tensor_tensor(xT[:, oc, :n], self.ps[pb][:, :n], g1[:, oc:oc + 1], xT[:, oc, :n],
                                                          ALU.mult, ALU.add),
                  reads=[("ps", pb), xkey, "mod"], writes=[xkey])

    def load_w(self, dst, src_v, key, ncol, piece=1024):
        for c0 in range(0, ncol, piece):
            c1 = min(ncol, c0 + piece)
            self.tk.dma("pool", lambda e: e.dma_start(out=dst[:, :, c0:c1], in_=src_v[:, :, c0:c1]),
                        writes=[key], ring="wload", nring=4)

    def phase_gla(self, l):
        tk, nc, T = self.tk, self.nc, self.T
        j = l // 2
        TG, NS = 256, 2
        with ExitStack() as st:
            w_in = self.sb(st, "g_win", [128, 8, 3088], BF16)
            w_out = self.sb(st, "g_wout", [128, 8, D], BF16)
            xT = self.sb(st, "g_xT", [128, 8, TG], F32)
            bufA = self.sb(st, "g_bufA", [128, 8, TG], F32)
            sq = self.sb(st, "g_sq", [128, 8, TG], BF16)
            hT = self.sb(st, "g_hT", [128, 8, TG], BF16)
            rstd = self.sb(st, "g_rstd", [128, 512], F32)
            qkT = self.sb(st, "g_qkT", [128, 8, TG], F32)
            rT = self.sb(st, "g_rT", [128, 8, TG], BF16)
            glow = self.sb(st, "g_glow", [32, TG], F32)
            v = self.sb(st, "g_v", [128, NS, D], BF16)
            ktok = self.sb(st, "g_ktok", [128, NS, 512], F32)
            az = self.sb(st, "g_az", [128, 512], F32)
            zs = self.sb(st, "g_zs", [128, 512], F32)
            la = self.sb(st, "g_la", [128, 512], F32)
            Eq = self.sb(st, "g_Eq", [128, 512], F32)
            Ek = self.sb(st, "g_Ek", [128, 512], F32)
            Eend = self.sb(st, "g_Eend", [128, 512], F32)
            kend = self.sb(st, "g_kend", [128, 512], BF16)
            qd = self.sb(st, "g_qd", [128, 4, 128], BF16)
            ki = self.sb(st, "g_ki", [128, 4, 128], BF16)
            am = self.sb(st, "g_am", [128, 4, 128], BF16)
            S = self.sb(st, "g_S", [128, 4, 256], F32)
            Sbf = self.sb(st, "g_Sbf", [128, 4, 256], BF16)
            rso = self.sb(st, "g_rso", [128, 4, TG], F32)
            tmpo = self.sb(st, "g_tmpo", [128, TG], F32)
            ps = self.ps
            self.load_w(w_in, self.wt("gla_w_in", j).rearrange("(k p) f -> p k f", p=128), "win", 3088, 772)
            self.load_w(w_out, self.wt("gla_w_out", j).rearrange("(k p) f -> p k f", p=128), "wo", D, 512)
            tk.op("dve", lambda e: e.memset(S[:], 0.0), writes=["S"])
            tk.op("dve", lambda e: e.memset(Sbf[:], 0.0), writes=["Sbf"])
            tk.op("dve", lambda e: e.memset(glow[:], 1.0), writes=["glow"])
            ow, _ = SP["wgu"]
            wgu = self.spt[0:17, ow + j * 512:ow + (j + 1) * 512]
            oo, _ = SP["onorm"]
            onorm = self.spt[:, oo + j * 2:oo + j * 2 + 2]
            tri = self.c32[:, C_TRI:C_TRI + 128]
            rev = self.c32[:, C_REV:C_REV + 128]
            ones = self.cbf[:, C_ONES:C_ONES + 128]
            maskb = self.c32[:, C_MASK:C_MASK + 128].unsqueeze(1).broadcast_to([128, 4, 128])
            oT = bufA
            gated = hT
            ev = 0
            for ti in range(T // TG):
                t0 = ti * TG
                tk.dma("sp", lambda e: e.dma_start(out=xT[:], in_=self.xsT_v[:, :, t0:t0 + TG]),
                       reads=["xsT"], writes=["gx"], ring="gxl", nring=1)
                self.norm_h(0, xT[:], "gx", TG, sq, bufA, rstd, [hT[:, c, :] for c in range(8)], ["gh"] * 8, 7)
                for oc in range(8):
                    pb = oc % 2
                    for k in range(8):
                        tk.op("pe", lambda e: e.matmul(ps[pb][:, :TG], w_in[:, k, oc * 128:(oc + 1) * 128], hT[:, k, :],
                                                       start=(k == 0), stop=(k == 7)),
                              reads=["win", "gh"], writes=[("ps", pb)], signal=(k == 7))
                    if oc % 2 == 0:
                        tk.op("act", lambda e: e.activation(out=qkT[:, oc, :], in_=ps[pb][:, :TG], func=AF.Copy),
                              reads=[("ps", pb)], writes=[("qk", oc)])
                    else:
                        tk.op("dve", lambda e: e.tensor_copy(qkT[:, oc, :], ps[pb][:, :TG]),
                              reads=[("ps", pb)], writes=[("qk", oc)])
                for oc in range(8):
                    pb = 2 + oc % 2
                    for k in range(8):
                        tk.op("pe", lambda e: e.matmul(ps[pb][:, :TG], w_in[:, k, 2048 + oc * 128:2048 + (oc + 1) * 128], hT[:, k, :],
                                                       start=(k == 0), stop=(k == 7)),
                              reads=["win", "gh"], writes=[("ps", pb)], signal=(k == 7))
                    tk.op("act", lambda e: e.activation(out=rT[:, oc, :], in_=ps[pb][:, :TG], func=AF.Silu),
                          reads=[("ps", pb)], writes=[("rT", oc)])
                for k in range(8):
                    tk.op("pe", lambda e: e.matmul(ps[7][0:16, :TG], w_in[:, k, 3072:3088], hT[:, k, :],
                                                   start=(k == 0), stop=(k == 7)),
                          reads=["win", "gh"], writes=[("ps", 7)], signal=(k == 7))
                tk.op("dve", lambda e: e.tensor_copy(glow[0:16, :], ps[7][0:16, :TG]), reads=[("ps", 7)], writes=["glow"])
                for s in range(NS):
                    tok = slice(s * 128, (s + 1) * 128)
                    for half in range(2):
                        pb = half
                        for k in range(8):
                            tk.op("pe", lambda e: e.matmul(ps[pb][:], hT[:, k, tok], w_in[:, k, 1024 + half * 512:1536 + half * 512],
                                                           start=(k == 0), stop=(k == 7)),
                                  reads=["win", "gh"], writes=[("ps", pb)], signal=(k == 7))
                        if half == 0:
                            tk.op("act", lambda e: e.activation(out=v[:, s, 0:512], in_=ps[pb][:], func=AF.Copy),
                                  reads=[("ps", pb)], writes=[("v", s, 0)])
                        else:
                            tk.op("dve", lambda e: e.tensor_copy(v[:, s, 512:1024], ps[pb][:]),
                                  reads=[("ps", pb)], writes=[("v", s, 1)])
                    pb = 2 + s % 2
                    for k in range(8):
                        tk.op("pe", lambda e: e.matmul(ps[pb][:], hT[:, k, tok], w_in[:, k, 512:1024],
                                                       start=(k == 0), stop=(k == 7)),
                              reads=["win", "gh"], writes=[("ps", pb)], signal=(k == 7))
                    tk.op("dve", lambda e: e.tensor_copy(ktok[:, s, :], ps[pb][:]), reads=[("ps", pb)], writes=[("ktok", s)])
                for s in range(NS):
                    tok = slice(s * 128, (s + 1) * 128)
                    tk.op("pe", lambda e: e.matmul(ps[4][:], glow[0:17, tok], wgu, start=True, stop=True),
                          reads=["glow", "spt"], writes=[("ps", 4)])
                    tk.op("act", lambda e: e.activation(out=zs[:], in_=ps[4][:], func=AF.Copy), reads=[("ps", 4)], writes=["zs"])
                    tk.op("dve", lambda e: e.scalar_tensor_tensor(az[:], zs[:], -1.0, zs[:], ALU.mult, ALU.min),
                          reads=["zs"], writes=["az"])
                    tk.op("act", lambda e: e.activation(out=az[:], in_=az[:], func=AF.Exp),
                          reads=["az"], writes=["az"])
                    tk.op("act", lambda e: e.activation(out=az[:], in_=az[:], func=AF.Ln, bias=1.0, scale=1.0),
                          reads=["az"], writes=["az"])
                    tk.op("dve", lambda e: e.scalar_tensor_tensor(la[:], zs[:], 0.0, az[:], ALU.min, ALU.subtract),
                          reads=["zs", "az"], writes=["la"])
                    for h in range(4):
                        tk.op("pe", lambda e: e.matmul(ps[5][:, h * 128:(h + 1) * 128], la[:, h * 128:(h + 1) * 128], tri,
                                                       start=True, stop=True),
                              reads=["la", "c32"], writes=[("ps", 5)], signal=(h == 3))
                    tk.op("act", lambda e: e.activation(out=Eq[:], in_=ps[5][:], func=AF.Exp), reads=[("ps", 5)], writes=["Eq"])
                    tk.op("act", lambda e: e.activation(out=Ek[:], in_=ps[5][:], func=AF.Exp, scale=-1.0),
                          reads=[("ps", 5)], writes=["Ek"])
                    tk.op("pe", lambda e: e.matmul(ps[4][:], rev, la[:], start=True, stop=True),
                          reads=["la", "c32"], writes=[("ps", 4)])
                    tk.op("act", lambda e: e.activation(out=Eend[:], in_=ps[4][:], func=AF.Exp), reads=[("ps", 4)], writes=["Eend"])
                    tk.op("dve", lambda e: e.tensor_tensor(kend[:], ktok[:, s, :], Eend[:], ALU.mult),
                          reads=[("ktok", s), "Eend"], writes=["kend"])
                    tk.op("dve", lambda e: e.scalar_tensor_tensor(qd[:], qkT[:, 0:4, tok], 128.0 ** -0.5,
                                                                  Eq[:].rearrange("p (h t) -> p h t", h=4), ALU.mult, ALU.mult),
                          reads=[("qk", c) for c in range(4)] + ["Eq"], writes=["qd"])
                    tk.op("dve", lambda e: e.tensor_tensor(ki[:], qkT[:, 4:8, tok], Ek[:].rearrange("p (h t) -> p h t", h=4), ALU.mult),
                          reads=[("qk", c) for c in range(4, 8)] + ["Ek"], writes=["ki"])
                    for h in range(4):
                        tk.op("pe", lambda e: e.matmul(ps[6][:, h * 128:(h + 1) * 128], ki[:, h, :], qd[:, h, :], start=True, stop=True),
                              reads=["ki", "qd"], writes=[("ps", 6)], signal=(h == 3))
                    tk.op("dve", lambda e: e.tensor_tensor(am[:], ps[6][:].rearrange("p (h t) -> p h t", h=4), maskb, ALU.mult),
                          reads=[("ps", 6), "c32"], writes=["am"])
                    for c in range(8):
                        h, jj = c // 2, c % 2
                        pb = c // 4
                        col = slice((c % 4) * 128, (c % 4 + 1) * 128)
                        tk.op("pe", lambda e: e.matmul(ps[pb][:, col], v[:, s, c * 128:(c + 1) * 128], am[:, h, :], start=True, stop=False),
                              reads=[("v", s, c // 4), "am"], writes=[("ps", pb)], signal=False)
                        tk.op("pe", lambda e: e.matmul(ps[pb][:, col], Sbf[:, h, jj * 128:(jj + 1) * 128], qd[:, h, :], start=False, stop=True),
                              reads=["Sbf", "qd"], writes=[("ps", pb)], signal=(c % 4 == 3))
                    tk.op("act", lambda e: e.activation(out=oT[:, 0:4, tok], in_=ps[0][:].rearrange("p (c t) -> p c t", c=4), func=AF.Copy),
                          reads=[("ps", 0)], writes=["oT"])
                    tk.op("dve", lambda e: e.tensor_copy(oT[:, 4:8, tok], ps[1][:].rearrange("p (c t) -> p c t", c=4)),
                          reads=[("ps", 1)], writes=["oT"])
                    for h in range(4):
                        pb = 2 + h // 2
                        col = slice((h % 2) * 256, (h % 2 + 1) * 256)
                        tk.op("pe", lambda e: e.matmul(ps[pb][:, col], kend[:, h * 128:(h + 1) * 128], v[:, s, h * 256:(h + 1) * 256],
                                                       start=True, stop=True),
                              reads=["kend", ("v", s, h // 2)], writes=[("ps", pb)], signal=(h % 2 == 1))
                    for h in range(4):
                        pb = 2 + h // 2
                        col = slice((h % 2) * 256, (h % 2 + 1) * 256)
                        tk.op("dve", lambda e: e.scalar_tensor_tensor(S[:, h, :], S[:, h, :], Eq[:, h * 128 + 127:h * 128 + 128], ps[pb][:, col],
                                                                      ALU.mult, ALU.add),
                              reads=["S", "Eq", ("ps", pb)], writes=["S"])
                    tk.op("act", lambda e: e.activation(out=Sbf[:], in_=S[:], func=AF.Copy), reads=["S"], writes=["Sbf"])
                tk.op("act", lambda e: e.activation(out=sq[:], in_=oT[:], func=AF.Square), reads=["oT"], writes=["sq"])
                for h in range(4):
                    pb = 6 + h // 2
                    col = slice((h % 2) * TG, (h % 2 + 1) * TG)
                    for jj in range(2):
                        tk.op("pe", lambda e: e.matmul(ps[pb][:, col], ones, sq[:, 2 * h + jj, :], start=(jj == 0), stop=(jj == 1)),
                              reads=["sq", "cbf"], writes=[("ps", pb)], signal=(jj == 1 and h % 2 == 1))
                for hb in range(2):
                    tk.op("act", lambda e: e.activation(out=rso[:, 2 * hb:2 * hb + 2, :],
                                                        in_=ps[6 + hb][:, :2 * TG].rearrange("p (h t) -> p h t", h=2),
                                                        func=AF.Sqrt, bias=EPS, scale=1.0 / 256),
                          reads=[("ps", 6 + hb)], writes=["rso"])
                tk.op("dve", lambda e: e.reciprocal(rso[:], rso[:]), reads=["rso"], writes=["rso"])
                for c in range(8):
                    h, jj = c // 2, c % 2
                    tk.op("dve", lambda e: e.scalar_tensor_tensor(tmpo[:], oT[:, c, :], onorm[:, jj:jj + 1], rso[:, h, :], ALU.mult, ALU.mult),
                          reads=["oT", "rso", "spt"], writes=["tmpo"])
                    tk.op("dve", lambda e: e.tensor_tensor(gated[:, c, :], tmpo[:], rT[:, c, :], ALU.mult),
                          reads=["tmpo", ("rT", c), "gh"], writes=["gh"])
                self.out_proj_residual(w_out, gated, "gh", xT, "gx", TG, banks=(0, 1))
                tk.dma("sp", lambda e: e.dma_start(out=self.xsT_v[:, :, t0:t0 + TG], in_=xT[:]),
                       reads=["gx"], writes=["xsT"], ring="gxs", nring=1)
            self.barrier()

    def phase_fox(self, l):
        self.phase_fox_proj(l)
        self.phase_fox_attn(l)
        self.phase_fox_out(l)

    def phase_fox_proj(self, l):
        tk, nc, T = self.tk, self.nc, self.T
        j = l // 2
        TF = 512
        with ExitStack() as st:
            w_in = self.sb(st, "p_win", [128, 8, 4112], BF16)
            xT = self.sb(st, "p_xT", [128, 8, TF], F32)
            xr = self.sb(st, "p_xr", [128, 8, TF], F32)
            sq = self.sb(st, "p_sq", [128, 8, TF], BF16)
            hT = self.sb(st, "p_hT", [128, 8, TF], BF16)
            rstd = self.sb(st, "p_rstd", [128, 512], F32)
            qf = self.sb(st, "p_qf", [128, 4, TF], F32)
            qsq = self.sb(st, "p_qsq", [128, 4, TF], BF16)
            rs = self.sb(st, "p_rs", [128, 4, TF], F32)
            qn = [self.sb(st, f"p_qn{i}", [128, 4, TF], BF16) for i in range(2)]
            vt = self.sb(st, "p_vt", [128, 4, D], BF16)
            sg = self.sb(st, "p_sg", [128, 8, TF], BF16)
            zs = self.sb(st, "p_zs", [128, 16], F32)
            az = self.sb(st, "p_az", [128, 16], F32)
            lf = self.sb(st, "p_lf", [128, 16], F32)
            carry = self.sb(st, "p_carry", [128, 16], F32)
            ps = self.ps
            self.load_w(w_in, self.wt("fox_w_in", j).rearrange("(k p) f -> p k f", p=128), "win", 4112, 1028)
            tk.op("dve", lambda e: e.memset(carry[:], 0.0), writes=["carry"])
            ob, _ = SP["bf"]
            bfB = self.sb(st, "p_bfB", [128, 16], F32)
            tk.op("pe", lambda e: e.matmul(self.ps[6][:, 0:16], self.c32[0:1, C_ONES:C_ONES + 128],
                                           self.spt[0:1, ob + j * 16:ob + (j + 1) * 16], start=True, stop=True),
                  reads=["c32", "spt"], writes=[("ps", 6)])
            tk.op("dve", lambda e: e.tensor_copy(bfB[:], self.ps[6][:, 0:16]), reads=[("ps", 6)], writes=["bfB"])
            bd = self.cbf[:, C_BD:C_BD + 128]
            cm = self.c32[:, C_MASK:C_MASK + 128]
            ones32 = self.c32[:, C_ONES:C_ONES + 128]
            ob, _ = SP["bf"]
            bfr = self.spt[0:1, ob + j * 16:ob + (j + 1) * 16]
            oq, _ = SP["qn"]
            okn, _ = SP["kn"]
            qs_v = self.qs.rearrange("(c p) t -> p c t", p=128)
            ks_v = self.ks.rearrange("(c p) t -> p c t", p=128)
            gs_v = self.gs.rearrange("(c p) t -> p c t", p=128)
            vs_v = self.vs.rearrange("(n p) d -> p n d", p=128)
            gi = 0
            for ti in range(T // TF):
                t0 = ti * TF
                tk.dma("sp", lambda e: e.dma_start(out=xT[:], in_=self.xsT_v[:, :, t0:t0 + TF]),
                       reads=["xsT"], writes=["px"], ring="pxl", nring=1)
                self.norm_h(0, xT[:], "px", TF, sq, xr, rstd, [hT[:, c, :] for c in range(8)], ["ph"] * 8, 7)
                for grp in range(4):
                    isk = grp >= 2
                    base = (1024 if isk else 0) + (grp % 2) * 512
                    gain = self.spt[:, (okn if isk else oq) + j:(okn if isk else oq) + j + 1]
                    for c in range(4):
                        for k in range(8):
                            tk.op("pe", lambda e: e.matmul(ps[c][:], w_in[:, k, base + c * 128:base + (c + 1) * 128], hT[:, k, :],
                                                           start=(k == 0), stop=(k == 7)),
                                  reads=["win", "ph"], writes=[("ps", c)], signal=(k == 7))
                        tk.op("act", lambda e: e.activation(out=qf[:, c, :], in_=ps[c][:], func=AF.Copy),
                              reads=[("ps", c)], writes=["qf"])
                    tk.op("dve", lambda e: e.tensor_tensor(qsq[:], qf[:], qf[:], ALU.mult), reads=["qf"], writes=["qsq"])
                    for c in range(4):
                        tk.op("pe", lambda e: e.matmul(ps[4 + c][:], bd, qsq[:, c, :], start=True, stop=True),
                              reads=["qsq", "cbf"], writes=[("ps", 4 + c)])
                        tk.op("act", lambda e: e.activation(out=rs[:, c, :], in_=ps[4 + c][:], func=AF.Sqrt, bias=EPS, scale=1.0 / 64),
                              reads=[("ps", 4 + c)], writes=["rs"])
                    tk.op("dve", lambda e: e.reciprocal(rs[:], rs[:]), reads=["rs"], writes=["rs"])
                    qb_ = qn[gi % 2]
                    qkey = ("qn", gi % 2)
                    gi += 1
                    tk.op("dve", lambda e: e.scalar_tensor_tensor(qb_[:], qf[:], gain, rs[:], ALU.mult, ALU.mult),
                          reads=["qf", "rs", "spt"], writes=[qkey])
                    dst = (ks_v if isk else qs_v)[:, (grp % 2) * 4:(grp % 2) * 4 + 4, t0:t0 + TF]
                    tk.dma("sp", lambda e: e.dma_start(out=dst, in_=qb_[:]), reads=[qkey], writes=["ks" if isk else "qs"],
                           ring="pqs", nring=2)
                for oc in range(8):
                    pb = oc % 4
                    for k in range(8):
                        tk.op("pe", lambda e: e.matmul(ps[pb][:], w_in[:, k, 3072 + oc * 128:3072 + (oc + 1) * 128], hT[:, k, :],
                                                       start=(k == 0), stop=(k == 7)),
                              reads=["win", "ph"], writes=[("ps", pb)], signal=(k == 7))
                    tk.op("act", lambda e: e.activation(out=sg[:, oc, :], in_=ps[pb][:], func=AF.Sigmoid),
                          reads=[("ps", pb)], writes=["sg"])
                tk.dma("sp", lambda e: e.dma_start(out=gs_v[:, :, t0:t0 + TF], in_=sg[:]), reads=["sg"], writes=["gs"],
                       ring="pgs", nring=1)
                for s in range(4):
                    tok = slice(s * 128, (s + 1) * 128)
                    for half in range(2):
                        pb = 4 + half
                        for k in range(8):
                            tk.op("pe", lambda e: e.matmul(ps[pb][:], hT[:, k, tok], w_in[:, k, 2048 + half * 512:2560 + half * 512],
                                                           start=(k == 0), stop=(k == 7)),
                                  reads=["win", "ph"], writes=[("ps", pb)], signal=(k == 7))
                        if half == 0:
                            tk.op("act", lambda e: e.activation(out=vt[:, s, 0:512], in_=ps[pb][:], func=AF.Copy),
                                  reads=[("ps", pb)], writes=["vt"])
                        else:
                            tk.op("dve", lambda e: e.tensor_copy(vt[:, s, 512:1024], ps[pb][:]), reads=[("ps", pb)], writes=["vt"])
                    for k in range(8):
                        tk.op("pe", lambda e: e.matmul(ps[6][:, 0:16], hT[:, k, tok], w_in[:, k, 4096:4112], start=(k == 0), stop=(k == 7)),
                              reads=["win", "ph"], writes=[("ps", 6)], signal=(k == 7))
                    tk.op("dve", lambda e: e.tensor_tensor(zs[:], ps[6][:, 0:16], bfB[:], ALU.add), reads=[("ps", 6), "bfB"], writes=["pzs"])
                    tk.op("dve", lambda e: e.scalar_tensor_tensor(az[:], zs[:], -1.0, zs[:], ALU.mult, ALU.min), reads=["pzs"], writes=["paz"])
                    tk.op("act", lambda e: e.activation(out=az[:], in_=az[:], func=AF.Exp), reads=["paz"], writes=["paz"])
                    tk.op("act", lambda e: e.activation(out=az[:], in_=az[:], func=AF.Ln, bias=1.0, scale=1.0), reads=["paz"], writes=["paz"])
                    tk.op("dve", lambda e: e.scalar_tensor_tensor(lf[:], zs[:], 0.0, az[:], ALU.min, ALU.subtract),
                          reads=["pzs", "paz"], writes=["lf"])
                    gt = ti * 4 + s
                    tk.op("pe", lambda e: e.matmul(ps[6][:, 16:32], cm, lf[:], start=True, stop=True),
                          reads=["lf", "c32"], writes=[("ps", 6)], signal=False)
                    tk.op("pe", lambda e: e.matmul(ps[6][:, 32:48], ones32, lf[:], start=True, stop=True),
                          reads=["lf", "c32"], writes=[("ps", 6)])
                    tk.op("dve", lambda e: e.tensor_tensor(self.Fk[:, gt, :], ps[6][:, 16:32], carry[:], ALU.add),
                          reads=[("ps", 6), "carry"], writes=["Fk"])
                    tk.op("dve", lambda e: e.tensor_tensor(carry[:], ps[6][:, 32:48], carry[:], ALU.add),
                          reads=[("ps", 6), "carry"], writes=["carry"])
                    if s == 1:
                        tk.op("dve", lambda e: e.tensor_copy(self.Fref[:, ti, :], carry[:]), reads=["carry"], writes=["Fref"])
                tk.dma("sp", lambda e: e.dma_start(out=vs_v[:, ti * 4:ti * 4 + 4, :], in_=vt[:]), reads=["vt"], writes=["vs"],
                       ring="pvs", nring=1)
            self.barrier()

    def phase_fox_attn(self, l):
        tk, nc, T = self.tk, self.nc, self.T
        NQB, NKT = T // 512, T // 128
        with ExitStack() as st:
            QhT = [self.sb(st, f"a_q{i}", [64, T], BF16) for i in range(2)]
            KhT = [self.sb(st, f"a_k{i}", [64, T], BF16) for i in range(2)]
            Vh = [self.sb(st, f"a_v{i}", [128, NKT, 65], BF16) for i in range(2)]
            Gh = [self.sb(st, f"a_g{i}", [64, T], BF16) for i in range(2)]
            bias = self.sb(st, "a_bias", [128, NQB, NKT, 16], F32)
            Pt = [self.sb(st, f"a_P{i}", [128, 512], BF16) for i in range(4)]
            rl = self.sb(st, "a_rl", [65, 512], F32)
            bcs = self.sb(st, "a_bcs", [64, 512], F32)
            t1 = self.sb(st, "a_t1", [64, 512], F32)
            og = [self.sb(st, f"a_og{i}", [64, 512], BF16) for i in range(2)]
            ps = self.ps
            cmb = self.cbf[:, C_MASK:C_MASK + 128]
            vs_v = self.vs.rearrange("(n p) d -> p n d", p=128)
            for i in range(2):
                tk.op("dve", lambda e: e.memset(Vh[i][:], 1.0), writes=[("Vh", i)])
            for qb in range(NQB):
                nk = 4 * qb + 4
                tk.op("dve", lambda e: e.tensor_tensor(bias[:, qb, 0:nk, :],
                                                       self.Fref[:, qb, :].unsqueeze(1).broadcast_to([128, nk, 16]),
                                                       self.Fk[:, 0:nk, :], ALU.subtract),
                      reads=["Fref", "Fk"], writes=["bias"])
            L = 3
            items = [(h, qb, kt) for h in range(16) for qb in range(NQB) for kt in range(4 * qb + 4)]
            head_start = {}
            for i, (h, qb, kt) in enumerate(items):
                head_start.setdefault(h, i)

            def loads(h):
                b = h % 2
                hs = slice(h * 64, (h + 1) * 64)
                q = "pool"
                tk.dma(q, lambda e: e.dma_start(out=QhT[b][:], in_=self.qs[hs, :]), reads=["qs"], writes=[("Qh", b)], ring="aq", nring=2)
                tk.dma(q, lambda e: e.dma_start(out=KhT[b][:], in_=self.ks[hs, :]), reads=["ks"], writes=[("Kh", b)], ring="ak", nring=2)
                for n0 in range(0, NKT, 8):
                    n1 = min(NKT, n0 + 8)
                    tk.dma(q, lambda e: e.dma_start(out=Vh[b][:, n0:n1, 0:64], in_=vs_v[:, n0:n1, hs]), reads=["vs"], writes=[("Vh", b)],
                           ring="av", nring=8)
                tk.dma(q, lambda e: e.dma_start(out=Gh[b][:], in_=self.gs[hs, :]), reads=["gs"], writes=[("Gh", b)], ring="ag", nring=2)

            later = []

            def tick():
                for ent in later:
                    ent[0] -= 1
                while later and later[0][0] <= 0:
                    later.pop(0)[1]()

            def emit_pv(i):
                h, qb, kt = items[i]
                b = h % 2
                nk = 4 * qb + 4
                blk = h * NQB + qb
                po = 4 + blk % 2
                pb = i % 4
                P = Pt[pb]
                c0 = max(0, kt - 4 * qb) * 128
                tk.op("pe", lambda e: e.matmul(ps[po][0:65, c0:512], Vh[b][:, kt, :], P[:, c0:512],
                                               start=(kt == 0), stop=(kt == nk - 1)),
                      reads=[("Vh", b), ("P", pb)], writes=[("ps", po)], signal=(kt == nk - 1))
                if kt != nk - 1:
                    return
                hs = slice(h * 64, (h + 1) * 64)
                ob_ = og[blk % 2]
                okey = ("og", blk % 2)
                tk.op("dve", lambda e: e.reciprocal(rl[64:65, :], ps[po][64:65, :]), reads=[("ps", po)], writes=["rl"])

                def epi():
                    tk.op("pe", lambda e: e.matmul(ps[6][0:64, :], self.c32[64:65, C_ONES:C_ONES + 64], rl[64:65, :], start=True, stop=True),
                          reads=["rl", "c32"], writes=[("ps", 6)])
                    tk.op("act", lambda e: e.activation(out=bcs[:], in_=ps[6][0:64, :], func=AF.Copy), reads=[("ps", 6)], writes=["bcs"])
                    tk.op("dve", lambda e: e.tensor_tensor(t1[:], ps[po][0:64, :], bcs[:], ALU.mult),
                          reads=[("ps", po), "bcs"], writes=["t1"])
                    tk.op("dve", lambda e: e.tensor_tensor(ob_[:], t1[:], Gh[b][:, qb * 512:(qb + 1) * 512], ALU.mult),
                          reads=["t1", ("Gh", b)], writes=[okey])
                    tk.dma("sp", lambda e: e.dma_start(out=self.os_[hs, qb * 512:(qb + 1) * 512], in_=ob_[:]),
                           reads=[okey], writes=["os"], ring="aos", nring=2)
                later.append([2, epi])

            loads(0)
            pend = []
            for i, (h, qb, kt) in enumerate(items):
                b = h % 2
                c0 = max(0, kt - 4 * qb) * 128
                pb = i % 4
                P = Pt[pb]
                tk.op("pe", lambda e: e.matmul(ps[pb][:, c0:512], KhT[b][:, kt * 128:(kt + 1) * 128],
                                               QhT[b][:, qb * 512 + c0:(qb + 1) * 512], start=True, stop=True),
                      reads=[("Kh", b), ("Qh", b)], writes=[("ps", pb)])
                tk.op("act", lambda e: e.activation(out=P[:, c0:512], in_=ps[pb][:, c0:512], func=AF.Exp,
                                                    bias=bias[:, qb, kt, h:h + 1], scale=0.125),
                      reads=[("ps", pb), "bias"], writes=[("P", pb)])
                if kt - 4 * qb >= 0:
                    tk.op("dve", lambda e: e.tensor_tensor(P[:, c0:c0 + 128], P[:, c0:c0 + 128], cmb, ALU.mult),
                          reads=[("P", pb), "cbf"], writes=[("P", pb)])
                tick()
                pend.append(i)
                if len(pend) > L:
                    emit_pv(pend.pop(0))
                if i == head_start[h] + L + 2 and h + 1 < 16:
                    loads(h + 1)
            while pend:
                emit_pv(pend.pop(0))
                tick()
            while later:
                later.pop(0)[1]()
            self.barrier()

    def phase_fox_out(self, l):
        tk, nc, T = self.tk, self.nc, self.T
        j = l // 2
        TF = 512
        with ExitStack() as st:
            w_out = self.sb(st, "o_wout", [128, 8, D], BF16)
            xT = [self.sb(st, f"o_xT{i}", [128, 8, TF], F32) for i in range(2)]
            gT = [self.sb(st, f"o_gT{i}", [128, 8, TF], BF16) for i in range(2)]
            self.load_w(w_out, self.wt("fox_w_out", j).rearrange("(k p) f -> p k f", p=128), "wo", D, 512)
            os_v = self.os_.rearrange("(c p) t -> p c t", p=128)
            for ti in range(T // TF):
                t0 = ti * TF
                b = ti % 2
                tk.dma("sp", lambda e: e.dma_start(out=xT[b][:], in_=self.xsT_v[:, :, t0:t0 + TF]),
                       reads=["xsT"], writes=[("ox", b)], ring="oxl", nring=2)
                tk.dma("sp", lambda e: e.dma_start(out=gT[b][:], in_=os_v[:, :, t0:t0 + TF]),
                       reads=["os"], writes=[("og", b)], ring="ogl", nring=2)
                self.out_proj_residual(w_out, gT[b], ("og", b), xT[b], ("ox", b), TF, banks=(0, 1, 2, 3))
                tk.dma("sp", lambda e: e.dma_start(out=self.xsT_v[:, :, t0:t0 + TF], in_=xT[b][:]),
                       reads=[("ox", b)], writes=["xsT"], ring="oxs", nring=2)
            self.barrier()

    def build_all(self, layers=(0, 1, 2, 3)):
        with ExitStack() as st:
            self.setup(st)
            self.phase_in()
            for l in layers:
                self.phase_mod(l)
                if l % 2 == 0:
                    self.phase_gla(l)
                    self.phase_ffn(l, moe=False)
                else:
                    self.phase_fox(l)
                    self.phase_ffn(l, moe=True)
            self.phase_out()
            self.tk.drain("sp")
        return self.nc


from concourse.bass_utils import run_bass_kernel_spmd

SEQ = 4096
BATCH = 4
N_CORES = 4
REAL = {4: [0, 1, 2, 3], 8: [0, 1, 4, 5]}[N_CORES]
_PROG = {}


def _program():
    if "k" not in _PROG:
        k = K(SEQ)
        k.build_all((0, 1, 2, 3))
        _PROG["k"] = k
    return _PROG["k"]


def kernel(**inputs):
    inp = {n: np.asarray(v) for n, v in inputs.items()}
    k = _program()
    consts = make_consts()
    in_maps = []
    for c in range(N_CORES):
        if c in REAL:
            b = REAL.index(c)
            d = {"x": np.ascontiguousarray(inp["x"][b], dtype=np.float32), "consts": consts, "sp": pack_small(inp, b)}
            for n in k.w:
                base, j = n.rsplit("_", 1)
                d[n] = np.ascontiguousarray(inp[base][int(j)], dtype=np.float32)
        else:
            d = {n: np.zeros_like(v) for n, v in in_maps[0].items()}
            d["consts"] = consts
        in_maps.append(d)
    res = run_bass_kernel_spmd(k.nc, in_maps, core_ids=list(range(N_CORES)))
    out = np.stack([np.asarray(res.results[c]["out"], dtype=np.float32) for c in REAL], 0)
    return out
```

```python
from contextlib import ExitStack


class Tok:
    __slots__ = ("sem", "count", "eng")

    def __init__(self, sem=None, count=None, eng=None):
        self.sem = sem
        self.count = count
        self.eng = eng


class Tracker:
    ROLL = 30000

    def __init__(self, nc, stack: ExitStack):
        self.nc = nc
        self.stack = stack
        self.engs = {"pe": nc.tensor, "act": nc.scalar, "dve": nc.vector,
                     "pool": nc.gpsimd, "sp": nc.sync}
        self.sem = {}
        self.cnt = {}
        self.nsem = 0
        for e in self.engs:
            self._new_sem(e)
        self.waited = {e: {} for e in self.engs}
        self.last_w = {}
        self.readers = {}
        self.pending = {e: [] for e in self.engs}
        self.rings = {}
        self.n_ins = 0

    def _alloc(self, name):
        self.nsem += 1
        return self.stack.enter_context(self.nc.semaphore(f"{name}_{self.nsem}"))

    def _new_sem(self, e):
        self.sem[e] = self._alloc(f"e_{e}")
        self.cnt[e] = 0

    def _wait(self, eng, tok):
        if tok is None:
            return
        if tok.eng == eng and eng == "pe":
            return
        assert tok.count is not None, "dependency on an unsignaled instruction"
        w = self.waited[eng]
        k = id(tok.sem)
        if w.get(k, 0) >= tok.count:
            return
        self.engs[eng].wait_ge(tok.sem, tok.count)
        w[k] = tok.count

    def _deps(self, reads, writes):
        deps = []
        for b in reads:
            t = self.last_w.get(b)
            if t is not None:
                deps.append(t)
        for b in writes:
            t = self.last_w.get(b)
            if t is not None:
                deps.append(t)
            deps.extend(self.readers.get(b, ()))
        return deps

    def _record(self, tok, reads, writes):
        for b in reads:
            self.readers.setdefault(b, []).append(tok)
        for b in writes:
            self.last_w[b] = tok
            self.readers[b] = []

    def op(self, eng, fn, reads=(), writes=(), signal=True):
        for t in self._deps(reads, writes):
            self._wait(eng, t)
        ins = fn(self.engs[eng])
        self.n_ins += 1
        if signal:
            if self.cnt[eng] >= self.ROLL:
                self._new_sem(eng)
            self.cnt[eng] += 1
            ins.then_inc(self.sem[eng], 1)
            tok = Tok(self.sem[eng], self.cnt[eng], eng)
            for p in self.pending[eng]:
                p.sem, p.count = tok.sem, tok.count
            self.pending[eng] = []
        else:
            tok = Tok(None, None, eng)
            self.pending[eng].append(tok)
        self._record(tok, reads, writes)
        return tok

    def dma(self, eng, fn, reads=(), writes=(), ring="d", nring=4):
        r = self.rings.get(ring)
        if r is None:
            r = self.rings[ring] = {"sems": [self._alloc(f"r_{ring}") for _ in range(nring)],
                                    "cnt": [0] * nring, "i": 0}
        i = r["i"]
        r["i"] = (i + 1) % len(r["sems"])
        sem = r["sems"][i]
        if r["cnt"][i] > 0:
            self._wait(eng, Tok(sem, r["cnt"][i], "dma"))
        for t in self._deps(reads, writes):
            self._wait(eng, t)
        ins = fn(self.engs[eng])
        self.n_ins += 1
        r["cnt"][i] += 16
        ins.then_inc(sem, 16)
        tok = Tok(sem, r["cnt"][i], "dma")
        self._record(tok, reads, writes)
        return tok

    def drain(self, eng="sp"):
        for r in self.rings.values():
            for sem, c in zip(r["sems"], r["cnt"]):
                if c:
                    self._wait(eng, Tok(sem, c, "dma"))
        for e in self.engs:
            assert not self.pending[e], f"unsignaled tail on {e}"
            if self.cnt[e]:
                self._wait(eng, Tok(self.sem[e], self.cnt[e], e))


import numpy as np
from contextlib import ExitStack
import concourse.bass as bass
import concourse.mybir as mybir

F32 = mybir.dt.float32
BF16 = mybir.dt.bfloat16
AF = mybir.ActivationFunctionType
ALU = mybir.AluOpType
AX = mybir.AxisListType

D = 1024
KC = 8
DFF = 3584
NE = 8
EPS = 1e-6

C_ID, C_MASK, C_TRI, C_REV, C_BD, C_ONES, C_SEL = 0, 128, 256, 386, 514, 642, 770
NCONST = 898


def make_consts():
    c = np.zeros((128, NCONST), np.float32)
    s = np.arange(128)[:, None]
    t = np.arange(128)[None, :]
    c[:, C_ID:C_ID + 128] = (s == t)
    c[:, C_MASK:C_MASK + 128] = (s <= t)
    c[:, C_TRI:C_TRI + 128] = (s <= t) / 16.0
    c[:, C_TRI + 128] = 1.0 / 16.0
    c[:, C_REV:C_REV + 128] = (s > t) / 16.0
    c[:, C_BD:C_BD + 128] = (s // 64 == t // 64)
    c[:, C_ONES:C_ONES + 128] = 1.0
    c[127, C_SEL:C_SEL + 128] = 1.0
    return c


SP = {}
_o = 0
for _n, _w in [("cT", 8), ("adab", 4 * 48), ("ngain", 4 * 2 * 8), ("wgu", 2 * 512), ("onorm", 2 * 2),
               ("bf", 2 * 16), ("qn", 2), ("kn", 2), ("wr", 2 * 8 * 8), ("br", 2 * 8)]:
    SP[_n] = (_o, _w)
    _o += _w
NSP = _o


def pack_small(inp, b):
    sp = np.zeros((128, NSP), np.float32)

    def put(name, arr):
        o, w = SP[name]
        arr = np.asarray(arr, np.float32).reshape(arr.shape[0], -1)
        assert arr.shape[1] == w, (name, arr.shape, w)
        sp[:arr.shape[0], o:o + w] = arr

    fm = lambda v, n: np.asarray(v, np.float32).reshape(n, 128).T
    put("cT", fm(inp["c"][b], 8))
    put("adab", np.stack([fm(inp["ada_b"][l], 48) for l in range(4)], 1))
    put("ngain", np.stack([np.stack([fm(inp["norm_gain"][l, s], 8) for s in range(2)], 1) for l in range(4)], 1))
    wgu = np.zeros((32, 2, 512), np.float32)
    for j in range(2):
        wgu[:16, j] = inp["gla_w_gate_up"][j]
        wgu[16, j] = inp["gla_b_gate"][j]
    put("wgu", wgu)
    put("onorm", np.stack([fm(inp["gla_o_norm"][j], 2) for j in range(2)], 1))
    put("bf", np.asarray(inp["fox_b_f"], np.float32).reshape(1, 32))
    put("qn", np.stack([np.tile(inp["fox_q_norm"][j], 2) for j in range(2)], 1))
    put("kn", np.stack([np.tile(inp["fox_k_norm"][j], 2) for j in range(2)], 1))
    put("wr", np.stack([np.asarray(inp["moe_w_router"][j]).reshape(8, 128, 8).transpose(1, 0, 2) for j in range(2)], 1))
    put("br", np.asarray(inp["moe_b_router"], np.float32).reshape(1, 16))
    return sp


class K:
    def __init__(self, T, dbg=()):
        self.T = T
        self.G = min(2048, T)
        self.dbg = set(dbg)
        nc = self.nc = bass.Bass("TRN2", target_bir_lowering=False)
        dt = nc.dram_tensor
        self.x = dt("x", [T, D], F32, kind="ExternalInput").ap()
        self.consts_d = dt("consts", [128, NCONST], F32, kind="ExternalInput").ap()
        self.sp_d = dt("sp", [128, NSP], F32, kind="ExternalInput").ap()
        self.w = {}
        self.wshape = {"ada_w": [D, 6 * D], "gla_w_in": [D, 3088], "gla_w_out": [D, D],
                       "fox_w_in": [D, 4112], "fox_w_out": [D, D],
                       "ffn_w_gate": [D, DFF], "ffn_w_up": [D, DFF], "ffn_w_down": [DFF, D],
                       "moe_w_gate": [NE, D, DFF], "moe_w_up": [NE, D, DFF], "moe_w_down": [NE, DFF, D]}
        self.out = dt("out", [T, D], F32, kind="ExternalOutput").ap()
        self.xsT = dt("xsT", [D, T], F32).ap()
        self.xsT_v = self.xsT.rearrange("(c p) t -> p c t", p=128)
        self.qs = dt("qs", [D, T], BF16).ap()
        self.ks = dt("ks", [D, T], BF16).ap()
        self.vs = dt("vs", [T, D], BF16).ap()
        self.gs = dt("gs", [D, T], BF16).ap()
        self.os_ = dt("os", [D, T], BF16).ap()
        self.dbg_out = {}

    def wt(self, name, j):
        key = f"{name}_{j}"
        if key not in self.w:
            self.w[key] = self.nc.dram_tensor(key, self.wshape[name], F32, kind="ExternalInput").ap()
        return self.w[key]

    def sb(self, st, name, shape, dtype):
        self.uid = getattr(self, "uid", 0) + 1
        return st.enter_context(self.nc.sbuf_tensor(f"{name}_u{self.uid}", shape, dtype))

    def setup(self, st):
        nc = self.nc
        self.tk = tk = Tracker(nc, st)
        self.c32 = self.sb(st, "c32", [128, NCONST], F32)
        self.spt = self.sb(st, "spt", [128, NSP], F32)
        self.cbf = self.sb(st, "cbf", [128, NCONST], BF16)
        self.mod = self.sb(st, "mod", [128, 48], F32)
        self.A = self.sb(st, "modA", [128, 2, 8], F32)
        self.ps = [st.enter_context(nc.psum_tensor(f"ps{i}", [128, 512], F32)) for i in range(8)]
        self.Fk = self.sb(st, "Fk", [128, self.T // 128, 16], F32)
        self.Fref = self.sb(st, "Fref", [128, max(1, self.T // 512), 16], F32)
        self.bfb = self.sb(st, "bfb", [1, 32], BF16)
        tk.dma("sp", lambda e: e.dma_start(out=self.c32[:], in_=self.consts_d), writes=["c32"], ring="c")
        tk.dma("sp", lambda e: e.dma_start(out=self.spt[:], in_=self.sp_d), writes=["spt"], ring="c")
        tk.op("dve", lambda e: e.tensor_copy(self.cbf[:], self.c32[:]), reads=["c32"], writes=["cbf"])
        ob, _ = SP["bf"]
        tk.op("dve", lambda e: e.tensor_copy(self.bfb[:], self.spt[0:1, ob:ob + 32]), reads=["spt"], writes=["bfb"])

    def spv(self, name):
        o, w = SP[name]
        return self.spt[:, o:o + w]

    def barrier(self):
        tk = self.tk
        tk.drain("sp")
        self.nc.all_engine_barrier()
        tk.last_w.clear()
        tk.readers.clear()

    def phase_in(self):
        tk, nc = self.tk, self.nc
        with ExitStack() as st:
            xin = [self.sb(st, f"xin{i}", [128, D], F32) for i in range(2)]
            xT = [self.sb(st, f"xTi{i}", [128, 8, 128], F32) for i in range(2)]
            ident = self.c32[:, C_ID:C_ID + 128]
            for tt in range(self.T // 128):
                b = tt % 2
                tk.dma("sp", lambda e: e.dma_start(out=xin[b][:], in_=self.x[tt * 128:(tt + 1) * 128, :]),
                       writes=[("xin", b)], ring="xin", nring=2)
                for half in range(2):
                    pb = (tt * 2 + half) % 8
                    for j in range(4):
                        c = half * 4 + j
                        tk.op("pe", lambda e: e.transpose(self.ps[pb][:, j * 128:(j + 1) * 128],
                                                          xin[b][:, c * 128:(c + 1) * 128], ident),
                              reads=[("xin", b), "c32"], writes=[("ps", pb)], signal=(j == 3))
                    src = self.ps[pb][:].rearrange("p (c t) -> p c t", c=4)
                    dst = xT[b][:, half * 4:(half + 1) * 4, :]
                    if half == 0:
                        tk.op("dve", lambda e: e.tensor_copy(dst, src), reads=[("ps", pb)], writes=[("xTi", b, half)])
                    else:
                        tk.op("act", lambda e: e.activation(out=dst, in_=src, func=AF.Copy),
                              reads=[("ps", pb)], writes=[("xTi", b, half)])
                tk.dma("sp", lambda e: e.dma_start(out=self.xsT_v[:, :, tt * 128:(tt + 1) * 128], in_=xT[b][:]),
                       reads=[("xTi", b, 0), ("xTi", b, 1)], writes=["xsT"], ring="xst", nring=2)
            self.barrier()

    def phase_out(self):
        tk, nc = self.tk, self.nc
        with ExitStack() as st:
            xin = [self.sb(st, f"xo{i}", [128, D], F32) for i in range(2)]
            xT = [self.sb(st, f"xTo{i}", [128, 8, 128], F32) for i in range(2)]
            ident = self.c32[:, C_ID:C_ID + 128]
            for tt in range(self.T // 128):
                b = tt % 2
                tk.dma("sp", lambda e: e.dma_start(out=xT[b][:], in_=self.xsT_v[:, :, tt * 128:(tt + 1) * 128]),
                       reads=["xsT"], writes=[("xTo", b)], ring="xld", nring=2)
                for half in range(2):
                    pb = (tt * 2 + half) % 8
                    for j in range(4):
                        c = half * 4 + j
                        tk.op("pe", lambda e: e.transpose(self.ps[pb][:, j * 128:(j + 1) * 128], xT[b][:, c, :], ident),
                              reads=[("xTo", b), "c32"], writes=[("ps", pb)], signal=(j == 3))
                    dst = xin[b][:, half * 512:(half + 1) * 512]
                    if half == 0:
                        tk.op("dve", lambda e: e.tensor_copy(dst, self.ps[pb][:]), reads=[("ps", pb)], writes=[("xo", b, half)])
                    else:
                        tk.op("act", lambda e: e.activation(out=dst, in_=self.ps[pb][:], func=AF.Copy),
                              reads=[("ps", pb)], writes=[("xo", b, half)])
                tk.dma("sp", lambda e: e.dma_start(out=self.out[tt * 128:(tt + 1) * 128, :], in_=xin[b][:]),
                       reads=[("xo", b, 0), ("xo", b, 1)], writes=[("out", tt)], ring="ost", nring=2)
            self.barrier()

    def phase_mod(self, l):
        tk, nc = self.tk, self.nc
        with ExitStack() as st:
            cond = self.sb(st, "cond", [128, 8], BF16)
            wa = [self.sb(st, f"wa{i}", [128, 8, 1536], BF16) for i in range(2)]
            tk.op("act", lambda e: e.activation(out=cond[:], in_=self.spv("cT"), func=AF.Silu),
                  reads=["spt"], writes=["cond"])
            wv = self.wt("ada_w", l).rearrange("(k p) j -> p k j", p=128)
            pm = self.ps[0]
            for q in range(4):
                b = q % 2
                tk.dma("pool", lambda e: e.dma_start(out=wa[b][:], in_=wv[:, :, q * 1536:(q + 1) * 1536]),
                       writes=[("wa", b)], ring="wa", nring=2)
                for jc in range(12):
                    col = q * 12 + jc
                    for k in range(8):
                        tk.op("pe", lambda e: e.matmul(pm[:, col:col + 1], wa[b][:, k, jc * 128:(jc + 1) * 128],
                                                       cond[:, k:k + 1], start=(k == 0), stop=(k == 7)),
                              reads=[("wa", b), "cond"], writes=[("ps", 0)], signal=(k == 7 and jc == 11))
            o, _ = SP["adab"]
            tk.op("dve", lambda e: e.tensor_tensor(self.mod[:], pm[:, 0:48], self.spt[:, o + l * 48:o + (l + 1) * 48], ALU.add),
                  reads=[("ps", 0), "spt"], writes=["mod"])
            og, _ = SP["ngain"]
            for s in range(2):
                sc = self.mod[:, 8:16] if s == 0 else self.mod[:, 32:40]
                gain = self.spt[:, og + (l * 2 + s) * 8:og + (l * 2 + s + 1) * 8]
                tk.op("dve", lambda e: e.scalar_tensor_tensor(self.A[:, s, :], sc, 1.0, gain, ALU.add, ALU.mult),
                      reads=["mod", "spt"], writes=["modA"])
            self.barrier()

    def shift(self, s):
        return self.mod[:, 0:8] if s == 0 else self.mod[:, 24:32]

    def gate(self, s):
        return self.mod[:, 16:24] if s == 0 else self.mod[:, 40:48]

    def norm_h(self, s, xT, xkey, n, sq, xr, rstd, outs, okeys, pbank, h32=None):
        tk = self.tk
        ones = self.cbf[:, C_ONES:C_ONES + 128]
        tk.op("act", lambda e: e.activation(out=sq[:, :, :n], in_=xT, func=AF.Square),
              reads=[xkey], writes=["sq"])
        for c in range(8):
            tk.op("pe", lambda e: e.matmul(self.ps[pbank][:, :n], ones, sq[:, c, :n], start=(c == 0), stop=(c == 7)),
                  reads=["sq", "cbf"], writes=[("ps", pbank)], signal=(c == 7))
        tk.op("act", lambda e: e.activation(out=rstd[:, :n], in_=self.ps[pbank][:, :n], func=AF.Sqrt, bias=EPS, scale=1.0 / D),
              reads=[("ps", pbank)], writes=["rstd"])
        tk.op("dve", lambda e: e.reciprocal(rstd[:, :n], rstd[:, :n]), reads=["rstd"], writes=["rstd"])
        rb = rstd[:, :n].unsqueeze(1).broadcast_to([128, 8, n])
        tk.op("dve", lambda e: e.tensor_tensor(xr[:, :, :n], xT, rb, ALU.mult),
              reads=[xkey, "rstd"], writes=["xr"])
        sh = self.shift(s)
        for c in range(8):
            dst = outs[c] if h32 is None else h32[:, c, :n]
            tk.op("act", lambda e: e.activation(out=dst, in_=xr[:, c, :n], func=AF.Identity,
                                                bias=sh[:, c:c + 1], scale=self.A[:, s, c:c + 1]),
                  reads=["xr", "mod", "modA"], writes=[okeys[c]] if h32 is None else [("h32", c)])
            if h32 is not None:
                tk.op("dve", lambda e: e.tensor_copy(outs[c], h32[:, c, :n]), reads=[("h32", c)], writes=[okeys[c]])

    def phase_ffn(self, l, moe):
        tk, nc, G = self.tk, self.nc, self.G
        j = l // 2
        NT = G // 512
        with ExitStack() as st:
            xT = self.sb(st, "f_xT", [128, 8, G], F32)
            hT = self.sb(st, "f_hT", [128, 8, G], BF16)
            wg = [self.sb(st, f"f_wg{i}", [128, 8, 512], BF16) for i in range(2)]
            wu = [self.sb(st, f"f_wu{i}", [128, 8, 512], BF16) for i in range(2)]
            wd = [self.sb(st, f"f_wd{i}", [128, 4, D], BF16) for i in range(2)]
            if moe:
                comb = self.sb(st, "f_comb", [128, G // 128, 8], F32)
                wr = self.spv("wr").rearrange("p (j k e) -> p j k e", j=2, k=8)
                obr, _ = SP["br"]
            g2 = self.gate(1)
            nexp = NE if moe else 1
            ident = self.c32[:, C_ID:C_ID + 128]
            ones32 = self.c32[:, C_ONES:C_ONES + 128]
            wi = 0
            for grp in range(self.T // G):
                t0 = grp * G
                tk.dma("sp", lambda e: e.dma_start(out=xT[:], in_=self.xsT_v[:, :, t0:t0 + G]),
                       reads=["xsT"], writes=[("fx", tt) for tt in range(NT)], ring="fxl", nring=1)
                with ExitStack() as st2:
                    sq = self.sb(st2, "f_sq", [128, 8, 512], BF16)
                    xr = self.sb(st2, "f_xr", [128, 8, 512], F32)
                    rstd = self.sb(st2, "f_rstd", [128, 512], F32)
                    if moe:
                        lg = self.sb(st2, "f_lg", [128, 8], F32)
                        mx = self.sb(st2, "f_mx", [128, 8], F32)
                        tmp8 = self.sb(st2, "f_tmp8", [128, 8], F32)
                        msk = self.sb(st2, "f_msk", [128, 8], F32)
                        nl1 = self.sb(st2, "f_nl1", [128, 1], F32)
                        den = self.sb(st2, "f_den", [128, 1], F32)
                        h32 = xr
                    for tt in range(NT):
                        cs = slice(tt * 512, (tt + 1) * 512)
                        self.norm_h(1, xT[:, :, cs], ("fx", tt), 512, sq, xr, rstd,
                                    [hT[:, c, cs] for c in range(8)], [("fh", tt)] * 8, 7,
                                    h32=(h32 if moe else None))
                        if moe:
                            for s4 in range(4):
                                si = tt * 4 + s4
                                for k in range(8):
                                    tk.op("pe", lambda e: e.matmul(self.ps[6][:, 0:8], h32[:, k, s4 * 128:(s4 + 1) * 128],
                                                                   wr[:, j, k, :], start=(k == 0), stop=False),
                                          reads=[("h32", k), "spt"], writes=[("ps", 6)], signal=False)
                                tk.op("pe", lambda e: e.matmul(self.ps[6][:, 0:8], ones32[0:1, :],
                                                               self.spt[0:1, obr + j * 8:obr + (j + 1) * 8], start=False, stop=True),
                                      reads=["c32", "spt"], writes=[("ps", 6)])
                                tk.op("dve", lambda e: e.tensor_copy(lg[:], self.ps[6][:, 0:8]), reads=[("ps", 6)], writes=["lg"])
                                tk.op("dve", lambda e: e.max(mx[:], lg[:]), reads=["lg"], writes=["mx"])
                                tk.op("dve", lambda e: e.tensor_scalar(msk[:], lg[:], mx[:, 1:2], None, ALU.is_ge),
                                      reads=["lg", "mx"], writes=["msk"])
                                tk.op("dve", lambda e: e.tensor_scalar(nl1[:], mx[:, 0:1], -1.0, None, ALU.mult),
                                      reads=["mx"], writes=["nl1"])
                                tk.op("act", lambda e: e.activation(out=tmp8[:], in_=lg[:], func=AF.Exp, bias=nl1[:, 0:1], scale=1.0),
                                      reads=["lg", "nl1"], writes=["tmp8"])
                                tk.op("dve", lambda e: e.tensor_tensor(tmp8[:], tmp8[:], msk[:], ALU.mult),
                                      reads=["tmp8", "msk"], writes=["tmp8"])
                                tk.op("dve", lambda e: e.reduce_sum(den[:], tmp8[:], AX.X), reads=["tmp8"], writes=["den"])
                                tk.op("dve", lambda e: e.reciprocal(den[:], den[:]), reads=["den"], writes=["den"])
                                tk.op("dve", lambda e: e.tensor_scalar(comb[:, si, :], tmp8[:], den[:, 0:1], None, ALU.mult),
                                      reads=["tmp8", "den"], writes=["comb"])
                    self.barrier()
                with ExitStack() as st3:
                    hid = self.sb(st3, "f_hid", [128, 4, G], BF16)
                    sg = [self.sb(st3, f"f_sg{i}", [128, 512], F32) for i in range(2)]
                    if moe:
                        combB = [self.sb(st3, f"f_combB{i}", [128, G], F32) for i in range(2)]
                        dg = [self.sb(st3, f"f_dg{i}", [128, 128], F32) for i in range(2)]
                    for ex in range(nexp):
                        if moe:
                            cb = combB[ex % 2]
                            ckey = ("combB", ex % 2)
                            for si in range(G // 128):
                                dgt = dg[si % 2]
                                tk.op("dve", lambda e: e.tensor_scalar(dgt[:], ident, comb[:, si, ex:ex + 1], None, ALU.mult),
                                      reads=["comb", "c32"], writes=[("dg", si % 2)])
                                pb = 6 + si % 2
                                tk.op("pe", lambda e: e.matmul(self.ps[pb][:, 0:128], ones32, dgt[:], start=True, stop=True),
                                      reads=[("dg", si % 2), "c32"], writes=[("ps", pb)])
                                tk.op("act", lambda e: e.activation(out=cb[:, si * 128:(si + 1) * 128], in_=self.ps[pb][:, 0:128], func=AF.Copy),
                                      reads=[("ps", pb)], writes=[(ckey, si)])
                            Wg = self.wt("moe_w_gate", j)[ex]
                            Wu = self.wt("moe_w_up", j)[ex]
                            Wd = self.wt("moe_w_down", j)[ex]
                        else:
                            Wg = self.wt("ffn_w_gate", j)
                            Wu = self.wt("ffn_w_up", j)
                            Wd = self.wt("ffn_w_down", j)
                        Wg = Wg.rearrange("(k p) f -> p k f", p=128)
                        Wu = Wu.rearrange("(k p) f -> p k f", p=128)
                        Wd = Wd.rearrange("(k p) o -> p k o", p=128)
                        for fc in range(DFF // 512):
                            b = wi % 2
                            wi += 1
                            fs = slice(fc * 512, (fc + 1) * 512)
                            tk.dma("pool", lambda e: e.dma_start(out=wg[b][:], in_=Wg[:, :, fs]), writes=[("wg", b)], ring="wg", nring=2)
                            tk.dma("pool", lambda e: e.dma_start(out=wu[b][:], in_=Wu[:, :, fs]), writes=[("wu", b)], ring="wu", nring=2)
                            tk.dma("pool", lambda e: e.dma_start(out=wd[b][:], in_=Wd[:, fc * 4:(fc + 1) * 4, :]), writes=[("wd", b)], ring="wd", nring=2)
                            it = 0
                            for ft in range(4):
                                for tt in range(NT):
                                    cs = slice(tt * 512, (tt + 1) * 512)
                                    pg, pu = (0, 1) if it % 2 == 0 else (2, 3)
                                    sgt = sg[it % 2]
                                    skey = ("sg", it % 2)
                                    it += 1
                                    for k in range(8):
                                        tk.op("pe", lambda e: e.matmul(self.ps[pg][:], wg[b][:, k, ft * 128:(ft + 1) * 128], hT[:, k, cs],
                                                                       start=(k == 0), stop=(k == 7)),
                                              reads=[("wg", b), ("fh", tt)], writes=[("ps", pg)], signal=(k == 7))
                                    for k in range(8):
                                        tk.op("pe", lambda e: e.matmul(self.ps[pu][:], wu[b][:, k, ft * 128:(ft + 1) * 128], hT[:, k, cs],
                                                                       start=(k == 0), stop=(k == 7)),
                                              reads=[("wu", b), ("fh", tt)], writes=[("ps", pu)], signal=(k == 7))
                                    tk.op("act", lambda e: e.activation(out=sgt[:], in_=self.ps[pg][:], func=AF.Silu),
                                          reads=[("ps", pg)], writes=[skey])
                                    if moe:
                                        tk.op("dve", lambda e: e.tensor_tensor(sgt[:], sgt[:], self.ps[pu][:], ALU.mult),
                                              reads=[skey, ("ps", pu)], writes=[skey])
                                        tk.op("dve", lambda e: e.tensor_tensor(hid[:, ft, cs], sgt[:], cb[:, cs], ALU.mult),
                                              reads=[skey] + [(ckey, tt * 4 + q4) for q4 in range(4)], writes=[("hid", ft, tt)])
                                    else:
                                        tk.op("dve", lambda e: e.tensor_tensor(hid[:, ft, cs], sgt[:], self.ps[pu][:], ALU.mult),
                                              reads=[skey, ("ps", pu)], writes=[("hid", ft, tt)])
                            it = 0
                            for oc in range(8):
                                for tt in range(NT):
                                    cs = slice(tt * 512, (tt + 1) * 512)
                                    py = 4 + it % 2
                                    it += 1
                                    for ft in range(4):
                                        tk.op("pe", lambda e: e.matmul(self.ps[py][:], wd[b][:, ft, oc * 128:(oc + 1) * 128], hid[:, ft, cs],
                                                                       start=(ft == 0), stop=(ft == 3)),
                                              reads=[("wd", b), ("hid", ft, tt)], writes=[("ps", py)], signal=(ft == 3))
                                    tk.op("dve", lambda e: e.scalar_tensor_tensor(xT[:, oc, cs], self.ps[py][:], g2[:, oc:oc + 1], xT[:, oc, cs],
                                                                                  ALU.mult, ALU.add),
                                          reads=[("ps", py), ("fx", tt), "mod"], writes=[("fx", tt)])
                    tk.dma("sp", lambda e: e.dma_start(out=self.xsT_v[:, :, t0:t0 + G], in_=xT[:]),
                           reads=[("fx", tt) for tt in range(NT)], writes=["xsT"], ring="fxs", nring=1)
                    self.barrier()

    def out_proj_residual(self, wo, gated, gkey, xT, xkey, n, banks=(0, 1)):
        tk = self.tk
        g1 = self.gate(0)
        for oc in range(8):
            pb = banks[oc % len(banks)]
            for k in range(8):
                tk.op("pe", lambda e: e.matmul(self.ps[pb][:, :n], wo[:, k, oc * 128:(oc + 1) * 128], gated[:, k, :n],
                                               start=(k == 0), stop=(k == 7)),
                      reads=["wo", gkey], writes=[("ps", pb)], signal=(k == 7))
            tk.op("dve", lambda e: e.scalar_tensor_tensor(xT[:, oc, :n], self.ps[pb][:, :n], g1[:, oc:oc + 1], xT[:, oc, :n],
                                                          ALU.mult, ALU.add),
                  reads=[("ps", pb), xkey, "mod"], writes=[xkey])

    def load_w(self, dst, src_v, key, ncol, piece=1024):
        for c0 in range(0, ncol, piece):
            c1 = min(ncol, c0 + piece)
            self.tk.dma("pool", lambda e: e.dma_start(out=dst[:, :, c0:c1], in_=src_v[:, :, c0:c1]),
                        writes=[key], ring="wload", nring=4)

    def phase_gla(self, l):
        tk, nc, T = self.tk, self.nc, self.T
        j = l // 2
        TG, NS = 256, 2
        with ExitStack() as st:
            w_in = self.sb(st, "g_win", [128, 8, 3088], BF16)
            w_out = self.sb(st, "g_wout", [128, 8, D], BF16)
            xT = self.sb(st, "g_xT", [128, 8, TG], F32)
            bufA = self.sb(st, "g_bufA", [128, 8, TG], F32)
            sq = self.sb(st, "g_sq", [128, 8, TG], BF16)
            hT = self.sb(st, "g_hT", [128, 8, TG], BF16)
            rstd = self.sb(st, "g_rstd", [128, 512], F32)
            qkT = self.sb(st, "g_qkT", [128, 8, TG], F32)
            rT = self.sb(st, "g_rT", [128, 8, TG], BF16)
            glow = self.sb(st, "g_glow", [32, TG], F32)
            v = self.sb(st, "g_v", [128, NS, D], BF16)
            ktok = self.sb(st, "g_ktok", [128, NS, 512], F32)
            az_ = [self.sb(st, f"g_az{i}", [128, 512], F32) for i in range(NS)]
            zs_ = [self.sb(st, f"g_zs{i}", [128, 512], F32) for i in range(NS)]
            la_ = [self.sb(st, f"g_la{i}", [128, 512], F32) for i in range(NS)]
            Eq_ = [self.sb(st, f"g_Eq{i}", [128, 512], F32) for i in range(NS)]
            Ek_ = [self.sb(st, f"g_Ek{i}", [128, 512], F32) for i in range(NS)]
            Eend_ = [self.sb(st, f"g_Eend{i}", [128, 512], F32) for i in range(NS)]
            kend = self.sb(st, "g_kend", [128, 512], BF16)
            qd = self.sb(st, "g_qd", [128, 4, 128], BF16)
            ki = self.sb(st, "g_ki", [128, 4, 128], BF16)
            am = self.sb(st, "g_am", [128, 4, 128], BF16)
            S = self.sb(st, "g_S", [128, 4, 256], F32)
            Sbf = self.sb(st, "g_Sbf", [128, 4, 256], BF16)
            rso = self.sb(st, "g_rso", [128, 4, TG], F32)
            tmpo = self.sb(st, "g_tmpo", [128, TG], F32)
            ps = self.ps
            self.load_w(w_in, self.wt("gla_w_in", j).rearrange("(k p) f -> p k f", p=128), "win", 3088, 772)
            self.load_w(w_out, self.wt("gla_w_out", j).rearrange("(k p) f -> p k f", p=128), "wo", D, 512)
            tk.op("dve", lambda e: e.memset(S[:], 0.0), writes=["S"])
            tk.op("dve", lambda e: e.memset(Sbf[:], 0.0), writes=["Sbf"])
            tk.op("dve", lambda e: e.memset(glow[:], 1.0), writes=["glow"])
            ow, _ = SP["wgu"]
            wgu = self.spt[0:17, ow + j * 512:ow + (j + 1) * 512]
            oo, _ = SP["onorm"]
            onorm = self.spt[:, oo + j * 2:oo + j * 2 + 2]
            tri = self.c32[:, C_TRI:C_TRI + 128]
            rev = self.c32[:, C_REV:C_REV + 128]
            ones = self.cbf[:, C_ONES:C_ONES + 128]
            maskb = self.c32[:, C_MASK:C_MASK + 128].unsqueeze(1).broadcast_to([128, 4, 128])
            oT = bufA
            gated = hT
            ev = 0
            for ti in range(T // TG):
                t0 = ti * TG
                tk.dma("sp", lambda e: e.dma_start(out=xT[:], in_=self.xsT_v[:, :, t0:t0 + TG]),
                       reads=["xsT"], writes=["gx"], ring="gxl", nring=1)
                self.norm_h(0, xT[:], "gx", TG, sq, bufA, rstd, [hT[:, c, :] for c in range(8)], ["gh"] * 8, 7)
                for k in range(8):
                    tk.op("pe", lambda e: e.matmul(ps[7][0:16, :TG], w_in[:, k, 3072:3088], hT[:, k, :],
                                                   start=(k == 0), stop=(k == 7)),
                          reads=["win", "gh"], writes=[("ps", 7)], signal=(k == 7))
                tk.op("dve", lambda e: e.tensor_copy(glow[0:16, :], ps[7][0:16, :TG]), reads=[("ps", 7)], writes=["glow"])
                groups = []

                def g_qk(oc):
                    pb = oc % 2
                    for k in range(8):
                        tk.op("pe", lambda e: e.matmul(ps[pb][:, :TG], w_in[:, k, oc * 128:(oc + 1) * 128], hT[:, k, :],
                                                       start=(k == 0), stop=(k == 7)),
                              reads=["win", "gh"], writes=[("ps", pb)], signal=(k == 7))
                    if oc % 2 == 0:
                        tk.op("act", lambda e: e.activation(out=qkT[:, oc, :], in_=ps[pb][:, :TG], func=AF.Copy),
                              reads=[("ps", pb)], writes=[("qk", oc)])
                    else:
                        tk.op("dve", lambda e: e.tensor_copy(qkT[:, oc, :], ps[pb][:, :TG]),
                              reads=[("ps", pb)], writes=[("qk", oc)])

                def g_r(oc):
                    pb = 2 + oc % 2
                    for k in range(8):
                        tk.op("pe", lambda e: e.matmul(ps[pb][:, :TG], w_in[:, k, 2048 + oc * 128:2048 + (oc + 1) * 128], hT[:, k, :],
                                                       start=(k == 0), stop=(k == 7)),
                              reads=["win", "gh"], writes=[("ps", pb)], signal=(k == 7))
                    tk.op("act", lambda e: e.activation(out=rT[:, oc, :], in_=ps[pb][:, :TG], func=AF.Silu),
                          reads=[("ps", pb)], writes=[("rT", oc)])

                def g_v(s, half):
                    tok = slice(s * 128, (s + 1) * 128)
                    pb = half
                    for k in range(8):
                        tk.op("pe", lambda e: e.matmul(ps[pb][:], hT[:, k, tok], w_in[:, k, 1024 + half * 512:1536 + half * 512],
                                                       start=(k == 0), stop=(k == 7)),
                              reads=["win", "gh"], writes=[("ps", pb)], signal=(k == 7))
                    if half == 0:
                        tk.op("act", lambda e: e.activation(out=v[:, s, 0:512], in_=ps[pb][:], func=AF.Copy),
                              reads=[("ps", pb)], writes=[("v", s, 0)])
                    else:
                        tk.op("dve", lambda e: e.tensor_copy(v[:, s, 512:1024], ps[pb][:]),
                              reads=[("ps", pb)], writes=[("v", s, 1)])

                def g_kt(s):
                    tok = slice(s * 128, (s + 1) * 128)
                    pb = 2 + s % 2
                    for k in range(8):
                        tk.op("pe", lambda e: e.matmul(ps[pb][:], hT[:, k, tok], w_in[:, k, 512:1024],
                                                       start=(k == 0), stop=(k == 7)),
                              reads=["win", "gh"], writes=[("ps", pb)], signal=(k == 7))
                    tk.op("dve", lambda e: e.tensor_copy(ktok[:, s, :], ps[pb][:]), reads=[("ps", pb)], writes=[("ktok", s)])

                for oc in range(8):
                    groups.append(lambda oc=oc: g_qk(oc))
                for s in range(NS):
                    groups.append(lambda s=s: g_v(s, 0))
                    groups.append(lambda s=s: g_v(s, 1))
                    groups.append(lambda s=s: g_kt(s))
                for oc in range(8):
                    groups.append(lambda oc=oc: g_r(oc))
                chain = []
                for s in range(NS):
                    tok = slice(s * 128, (s + 1) * 128)
                    zs, az, la, Eq, Ek, Eend = zs_[s], az_[s], la_[s], Eq_[s], Ek_[s], Eend_[s]
                    chain += [
                        lambda s=s, tok=tok: tk.op("pe", lambda e: e.matmul(ps[4][:], glow[0:17, tok], wgu, start=True, stop=True),
                                                   reads=["glow", "spt"], writes=[("ps", 4)]),
                        lambda s=s, zs=zs: tk.op("act", lambda e: e.activation(out=zs[:], in_=ps[4][:], func=AF.Copy),
                                                 reads=[("ps", 4)], writes=[("zs", s)]),
                        lambda s=s, zs=zs, az=az: tk.op("dve", lambda e: e.scalar_tensor_tensor(az[:], zs[:], -1.0, zs[:], ALU.mult, ALU.min),
                                                        reads=[("zs", s)], writes=[("az", s)]),
                        lambda s=s, az=az: tk.op("act", lambda e: e.activation(out=az[:], in_=az[:], func=AF.Exp),
                                                 reads=[("az", s)], writes=[("az", s)]),
                        lambda s=s, az=az: tk.op("act", lambda e: e.activation(out=az[:], in_=az[:], func=AF.Ln, bias=1.0, scale=1.0),
                                                 reads=[("az", s)], writes=[("az", s)]),
                        lambda s=s, zs=zs, az=az, la=la: tk.op("dve", lambda e: e.scalar_tensor_tensor(la[:], zs[:], 0.0, az[:], ALU.min, ALU.subtract),
                                                               reads=[("zs", s), ("az", s)], writes=[("la", s)]),
                        lambda s=s, la=la: [tk.op("pe", lambda e: e.matmul(ps[5][:, h * 128:(h + 1) * 128], la[:, h * 128:(h + 1) * 128], tri,
                                                                            start=True, stop=True),
                                                  reads=[("la", s), "c32"], writes=[("ps", 5)], signal=(h == 3)) for h in range(4)],
                        lambda s=s, Eq=Eq: tk.op("act", lambda e: e.activation(out=Eq[:], in_=ps[5][:], func=AF.Exp),
                                                 reads=[("ps", 5)], writes=[("Eq", s)]),
                        lambda s=s, Ek=Ek: tk.op("act", lambda e: e.activation(out=Ek[:], in_=ps[5][:], func=AF.Exp, scale=-1.0),
                                                 reads=[("ps", 5)], writes=[("Ek", s)]),
                        lambda s=s, la=la: tk.op("pe", lambda e: e.matmul(ps[4][:], rev, la[:], start=True, stop=True),
                                                 reads=[("la", s), "c32"], writes=[("ps", 4)]),
                        lambda s=s, Eend=Eend: tk.op("act", lambda e: e.activation(out=Eend[:], in_=ps[4][:], func=AF.Exp),
                                                     reads=[("ps", 4)], writes=[("Eend", s)]),
                    ]
                for i in range(max(len(groups), len(chain))):
                    if i < len(groups):
                        groups[i]()
                    if i < len(chain):
                        chain[i]()
                for s in range(NS):
                    tok = slice(s * 128, (s + 1) * 128)
                    Eq, Ek, Eend = Eq_[s], Ek_[s], Eend_[s]
                    tk.op("dve", lambda e: e.tensor_tensor(kend[:], ktok[:, s, :], Eend[:], ALU.mult),
                          reads=[("ktok", s), ("Eend", s)], writes=["kend"])
                    tk.op("dve", lambda e: e.scalar_tensor_tensor(qd[:], qkT[:, 0:4, tok], 128.0 ** -0.5,
                                                                  Eq[:].rearrange("p (h t) -> p h t", h=4), ALU.mult, ALU.mult),
                          reads=[("qk", c) for c in range(4)] + [("Eq", s)], writes=["qd"])
                    tk.op("dve", lambda e: e.tensor_tensor(ki[:], qkT[:, 4:8, tok], Ek[:].rearrange("p (h t) -> p h t", h=4), ALU.mult),
                          reads=[("qk", c) for c in range(4, 8)] + [("Ek", s)], writes=["ki"])
                    for h in range(4):
                        tk.op("pe", lambda e: e.matmul(ps[6][:, h * 128:(h + 1) * 128], ki[:, h, :], qd[:, h, :], start=True, stop=True),
                              reads=["ki", "qd"], writes=[("ps", 6)], signal=(h == 3))
                    tk.op("dve", lambda e: e.tensor_tensor(am[:], ps[6][:].rearrange("p (h t) -> p h t", h=4), maskb, ALU.mult),
                          reads=[("ps", 6), "c32"], writes=["am"])
                    for c in range(8):
                        h, jj = c // 2, c % 2
                        pb = c // 4
                        col = slice((c % 4) * 128, (c % 4 + 1) * 128)
                        tk.op("pe", lambda e: e.matmul(ps[pb][:, col], v[:, s, c * 128:(c + 1) * 128], am[:, h, :], start=True, stop=False),
                              reads=[("v", s, c // 4), "am"], writes=[("ps", pb)], signal=False)
                        tk.op("pe", lambda e: e.matmul(ps[pb][:, col], Sbf[:, h, jj * 128:(jj + 1) * 128], qd[:, h, :], start=False, stop=True),
                              reads=["Sbf", "qd"], writes=[("ps", pb)], signal=(c % 4 == 3))
                    tk.op("act", lambda e: e.activation(out=oT[:, 0:4, tok], in_=ps[0][:].rearrange("p (c t) -> p c t", c=4), func=AF.Copy),
                          reads=[("ps", 0)], writes=["oT"])
                    tk.op("dve", lambda e: e.tensor_copy(oT[:, 4:8, tok], ps[1][:].rearrange("p (c t) -> p c t", c=4)),
                          reads=[("ps", 1)], writes=["oT"])
                    for h in range(4):
                        pb = 2 + h // 2
                        col = slice((h % 2) * 256, (h % 2 + 1) * 256)
                        tk.op("pe", lambda e: e.matmul(ps[pb][:, col], kend[:, h * 128:(h + 1) * 128], v[:, s, h * 256:(h + 1) * 256],
                                                       start=True, stop=True),
                              reads=["kend", ("v", s, h // 2)], writes=[("ps", pb)], signal=(h % 2 == 1))
                    for h in range(4):
                        pb = 2 + h // 2
                        col = slice((h % 2) * 256, (h % 2 + 1) * 256)
                        tk.op("dve", lambda e: e.scalar_tensor_tensor(S[:, h, :], S[:, h, :], Eq[:, h * 128 + 127:h * 128 + 128], ps[pb][:, col],
                                                                      ALU.mult, ALU.add),
                              reads=["S", ("Eq", s), ("ps", pb)], writes=["S"])
                    tk.op("act", lambda e: e.activation(out=Sbf[:], in_=S[:], func=AF.Copy), reads=["S"], writes=["Sbf"])
                tk.op("act", lambda e: e.activation(out=sq[:], in_=oT[:], func=AF.Square), reads=["oT"], writes=["sq"])
                for h in range(4):
                    pb = 6 + h // 2
                    col = slice((h % 2) * TG, (h % 2 + 1) * TG)
                    for jj in range(2):
                        tk.op("pe", lambda e: e.matmul(ps[pb][:, col], ones, sq[:, 2 * h + jj, :], start=(jj == 0), stop=(jj == 1)),
                              reads=["sq", "cbf"], writes=[("ps", pb)], signal=(jj == 1 and h % 2 == 1))
                for hb in range(2):
                    tk.op("act", lambda e: e.activation(out=rso[:, 2 * hb:2 * hb + 2, :],
                                                        in_=ps[6 + hb][:, :2 * TG].rearrange("p (h t) -> p h t", h=2),
                                                        func=AF.Sqrt, bias=EPS, scale=1.0 / 256),
                          reads=[("ps", 6 + hb)], writes=["rso"])
                tk.op("dve", lambda e: e.reciprocal(rso[:], rso[:]), reads=["rso"], writes=["rso"])
                for c in range(8):
                    h, jj = c // 2, c % 2
                    tk.op("dve", lambda e: e.scalar_tensor_tensor(tmpo[:], oT[:, c, :], onorm[:, jj:jj + 1], rso[:, h, :], ALU.mult, ALU.mult),
                          reads=["oT", "rso", "spt"], writes=["tmpo"])
                    tk.op("dve", lambda e: e.tensor_tensor(gated[:, c, :], tmpo[:], rT[:, c, :], ALU.mult),
                          reads=["tmpo", ("rT", c), "gh"], writes=["gh"])
                self.out_proj_residual(w_out, gated, "gh", xT, "gx", TG, banks=(0, 1))
                tk.dma("sp", lambda e: e.dma_start(out=self.xsT_v[:, :, t0:t0 + TG], in_=xT[:]),
                       reads=["gx"], writes=["xsT"], ring="gxs", nring=1)
            self.barrier()

    def phase_fox(self, l):
        self.phase_fox_proj(l)
        self.phase_fox_attn(l)
        self.phase_fox_out(l)

    def phase_fox_proj(self, l):
        tk, nc, T = self.tk, self.nc, self.T
        j = l // 2
        TF = 512
        with ExitStack() as st:
            w_in = self.sb(st, "p_win", [128, 8, 4112], BF16)
            xT = self.sb(st, "p_xT", [128, 8, TF], F32)
            xr = self.sb(st, "p_xr", [128, 8, TF], F32)
            sq = self.sb(st, "p_sq", [128, 8, TF], BF16)
            hT = self.sb(st, "p_hT", [128, 8, TF], BF16)
            rstd = self.sb(st, "p_rstd", [128, 512], F32)
            qf = self.sb(st, "p_qf", [128, 4, TF], F32)
            qsq = self.sb(st, "p_qsq", [128, 4, TF], BF16)
            rs = self.sb(st, "p_rs", [128, 4, TF], F32)
            qn = [self.sb(st, f"p_qn{i}", [128, 4, TF], BF16) for i in range(2)]
            vt = self.sb(st, "p_vt", [128, 4, D], BF16)
            sg = self.sb(st, "p_sg", [128, 8, TF], BF16)
            zs = self.sb(st, "p_zs", [128, 16], F32)
            az = self.sb(st, "p_az", [128, 16], F32)
            lf = self.sb(st, "p_lf", [128, 16], F32)
            carry = self.sb(st, "p_carry", [128, 16], F32)
            ps = self.ps
            self.load_w(w_in, self.wt("fox_w_in", j).rearrange("(k p) f -> p k f", p=128), "win", 4112, 1028)
            tk.op("dve", lambda e: e.memset(carry[:], 0.0), writes=["carry"])
            ob, _ = SP["bf"]
            bfB = self.sb(st, "p_bfB", [128, 16], F32)
            tk.op("pe", lambda e: e.matmul(self.ps[6][:, 0:16], self.c32[0:1, C_ONES:C_ONES + 128],
                                           self.spt[0:1, ob + j * 16:ob + (j + 1) * 16], start=True, stop=True),
                  reads=["c32", "spt"], writes=[("ps", 6)])
            tk.op("dve", lambda e: e.tensor_copy(bfB[:], self.ps[6][:, 0:16]), reads=[("ps", 6)], writes=["bfB"])
            bd = self.cbf[:, C_BD:C_BD + 128]
            cm = self.c32[:, C_MASK:C_MASK + 128]
            ones32 = self.c32[:, C_ONES:C_ONES + 128]
            ob, _ = SP["bf"]
            bfr = self.spt[0:1, ob + j * 16:ob + (j + 1) * 16]
            oq, _ = SP["qn"]
            okn, _ = SP["kn"]
            qs_v = self.qs.rearrange("(c p) t -> p c t", p=128)
            ks_v = self.ks.rearrange("(c p) t -> p c t", p=128)
            gs_v = self.gs.rearrange("(c p) t -> p c t", p=128)
            vs_v = self.vs.rearrange("(n p) d -> p n d", p=128)
            gi = 0
            for ti in range(T // TF):
                t0 = ti * TF
                tk.dma("sp", lambda e: e.dma_start(out=xT[:], in_=self.xsT_v[:, :, t0:t0 + TF]),
                       reads=["xsT"], writes=["px"], ring="pxl", nring=1)
                self.norm_h(0, xT[:], "px", TF, sq, xr, rstd, [hT[:, c, :] for c in range(8)], ["ph"] * 8, 7)
                for grp in range(4):
                    isk = grp >= 2
                    base = (1024 if isk else 0) + (grp % 2) * 512
                    gain = self.spt[:, (okn if isk else oq) + j:(okn if isk else oq) + j + 1]
                    for c in range(4):
                        for k in range(8):
                            tk.op("pe", lambda e: e.matmul(ps[c][:], w_in[:, k, base + c * 128:base + (c + 1) * 128], hT[:, k, :],
                                                           start=(k == 0), stop=(k == 7)),
                                  reads=["win", "ph"], writes=[("ps", c)], signal=(k == 7))
                        tk.op("act", lambda e: e.activation(out=qf[:, c, :], in_=ps[c][:], func=AF.Copy),
                              reads=[("ps", c)], writes=["qf"])
                    tk.op("dve", lambda e: e.tensor_tensor(qsq[:], qf[:], qf[:], ALU.mult), reads=["qf"], writes=["qsq"])
                    for c in range(4):
                        tk.op("pe", lambda e: e.matmul(ps[4 + c][:], bd, qsq[:, c, :], start=True, stop=True),
                              reads=["qsq", "cbf"], writes=[("ps", 4 + c)])
                        tk.op("act", lambda e: e.activation(out=rs[:, c, :], in_=ps[4 + c][:], func=AF.Ln, bias=EPS, scale=1.0 / 64),
                              reads=[("ps", 4 + c)], writes=["rs"])
                    tk.op("act", lambda e: e.activation(out=rs[:], in_=rs[:], func=AF.Exp, scale=-0.5), reads=["rs"], writes=["rs"])
                    qb_ = qn[gi % 2]
                    qkey = ("qn", gi % 2)
                    gi += 1
                    tk.op("dve", lambda e: e.scalar_tensor_tensor(qb_[:], qf[:], gain, rs[:], ALU.mult, ALU.mult),
                          reads=["qf", "rs", "spt"], writes=[qkey])
                    dst = (ks_v if isk else qs_v)[:, (grp % 2) * 4:(grp % 2) * 4 + 4, t0:t0 + TF]
                    tk.dma("sp", lambda e: e.dma_start(out=dst, in_=qb_[:]), reads=[qkey], writes=["ks" if isk else "qs"],
                           ring="pqs", nring=2)
                for oc in range(8):
                    pb = oc % 4
                    for k in range(8):
                        tk.op("pe", lambda e: e.matmul(ps[pb][:], w_in[:, k, 3072 + oc * 128:3072 + (oc + 1) * 128], hT[:, k, :],
                                                       start=(k == 0), stop=(k == 7)),
                              reads=["win", "ph"], writes=[("ps", pb)], signal=(k == 7))
                    tk.op("act", lambda e: e.activation(out=sg[:, oc, :], in_=ps[pb][:], func=AF.Sigmoid),
                          reads=[("ps", pb)], writes=["sg"])
                tk.dma("sp", lambda e: e.dma_start(out=gs_v[:, :, t0:t0 + TF], in_=sg[:]), reads=["sg"], writes=["gs"],
                       ring="pgs", nring=1)
                for s in range(4):
                    tok = slice(s * 128, (s + 1) * 128)
                    for half in range(2):
                        pb = 4 + half
                        for k in range(8):
                            tk.op("pe", lambda e: e.matmul(ps[pb][:], hT[:, k, tok], w_in[:, k, 2048 + half * 512:2560 + half * 512],
                                                           start=(k == 0), stop=(k == 7)),
                                  reads=["win", "ph"], writes=[("ps", pb)], signal=(k == 7))
                        if half == 0:
                            tk.op("act", lambda e: e.activation(out=vt[:, s, 0:512], in_=ps[pb][:], func=AF.Copy),
                                  reads=[("ps", pb)], writes=["vt"])
                        else:
                            tk.op("dve", lambda e: e.tensor_copy(vt[:, s, 512:1024], ps[pb][:]), reads=[("ps", pb)], writes=["vt"])
                    for k in range(8):
                        tk.op("pe", lambda e: e.matmul(ps[6][:, 0:16], hT[:, k, tok], w_in[:, k, 4096:4112], start=(k == 0), stop=(k == 7)),
                              reads=["win", "ph"], writes=[("ps", 6)], signal=(k == 7))
                    tk.op("dve", lambda e: e.tensor_tensor(zs[:], ps[6][:, 0:16], bfB[:], ALU.add), reads=[("ps", 6), "bfB"], writes=["pzs"])
                    tk.op("dve", lambda e: e.scalar_tensor_tensor(az[:], zs[:], -1.0, zs[:], ALU.mult, ALU.min), reads=["pzs"], writes=["paz"])
                    tk.op("act", lambda e: e.activation(out=az[:], in_=az[:], func=AF.Exp), reads=["paz"], writes=["paz"])
                    tk.op("act", lambda e: e.activation(out=az[:], in_=az[:], func=AF.Ln, bias=1.0, scale=1.0), reads=["paz"], writes=["paz"])
                    tk.op("dve", lambda e: e.scalar_tensor_tensor(lf[:], zs[:], 0.0, az[:], ALU.min, ALU.subtract),
                          reads=["pzs", "paz"], writes=["lf"])
                    gt = ti * 4 + s
                    tk.op("pe", lambda e: e.matmul(ps[6][:, 16:32], cm, lf[:], start=True, stop=True),
                          reads=["lf", "c32"], writes=[("ps", 6)], signal=False)
                    tk.op("pe", lambda e: e.matmul(ps[6][:, 32:48], ones32, lf[:], start=True, stop=True),
                          reads=["lf", "c32"], writes=[("ps", 6)])
                    tk.op("dve", lambda e: e.tensor_tensor(self.Fk[:, gt, :], ps[6][:, 16:32], carry[:], ALU.add),
                          reads=[("ps", 6), "carry"], writes=["Fk"])
                    tk.op("dve", lambda e: e.tensor_tensor(carry[:], ps[6][:, 32:48], carry[:], ALU.add),
                          reads=[("ps", 6), "carry"], writes=["carry"])
                    if s == 1:
                        tk.op("dve", lambda e: e.tensor_copy(self.Fref[:, ti, :], carry[:]), reads=["carry"], writes=["Fref"])
                tk.dma("sp", lambda e: e.dma_start(out=vs_v[:, ti * 4:ti * 4 + 4, :], in_=vt[:]), reads=["vt"], writes=["vs"],
                       ring="pvs", nring=1)
            self.barrier()

    def phase_fox_attn(self, l):
        tk, nc, T = self.tk, self.nc, self.T
        NQB, NKT = T // 512, T // 128
        with ExitStack() as st:
            QhT = [self.sb(st, f"a_q{i}", [128, T], BF16) for i in range(2)]
            KhT = [self.sb(st, f"a_k{i}", [128, T], BF16) for i in range(2)]
            Vh = [self.sb(st, f"a_v{i}", [128, NKT, 128], BF16) for i in range(2)]
            Gh = [self.sb(st, f"a_g{i}", [64, T], BF16) for i in range(2)]
            bias = self.sb(st, "a_bias", [128, NQB, NKT, 16], F32)
            Pt = [self.sb(st, f"a_P{i}", [128, 512], BF16) for i in range(4)]
            rl = self.sb(st, "a_rl", [65, 512], F32)
            bcs = self.sb(st, "a_bcs", [64, 512], F32)
            t1 = self.sb(st, "a_t1", [64, 512], F32)
            og = [self.sb(st, f"a_og{i}", [64, 512], BF16) for i in range(2)]
            ps = self.ps
            cmb = self.cbf[:, C_MASK:C_MASK + 128]
            vs_v = self.vs.rearrange("(n p) d -> p n d", p=128)
            for i in range(2):
                tk.op("dve", lambda e: e.memset(Vh[i][:], 1.0), writes=[("Vh", i)])
                tk.op("dve", lambda e: e.memset(QhT[i][64:128, :], 0.0), writes=[("Qh", i)])
                tk.op("dve", lambda e: e.memset(KhT[i][64:128, :], 0.0), writes=[("Kh", i)])
            for qb in range(NQB):
                nk = 4 * qb + 4
                tk.op("dve", lambda e: e.tensor_tensor(bias[:, qb, 0:nk, :],
                                                       self.Fref[:, qb, :].unsqueeze(1).broadcast_to([128, nk, 16]),
                                                       self.Fk[:, 0:nk, :], ALU.subtract),
                      reads=["Fref", "Fk"], writes=["bias"])
            L = 3
            items = [(h, qb, kt) for h in range(16) for qb in range(NQB) for kt in range(4 * qb + 4)]
            head_start = {}
            for i, (h, qb, kt) in enumerate(items):
                head_start.setdefault(h, i)

            def loads(h):
                b = h % 2
                hs = slice(h * 64, (h + 1) * 64)
                q = "pool"
                tk.dma(q, lambda e: e.dma_start(out=QhT[b][0:64, :], in_=self.qs[hs, :]), reads=["qs"], writes=[("Qh", b)], ring="aq", nring=2)
                tk.dma(q, lambda e: e.dma_start(out=KhT[b][0:64, :], in_=self.ks[hs, :]), reads=["ks"], writes=[("Kh", b)], ring="ak", nring=2)
                for n0 in range(0, NKT, 8):
                    n1 = min(NKT, n0 + 8)
                    tk.dma(q, lambda e: e.dma_start(out=Vh[b][:, n0:n1, 0:64], in_=vs_v[:, n0:n1, hs]), reads=["vs"], writes=[("Vh", b)],
                           ring="av", nring=8)
                tk.dma(q, lambda e: e.dma_start(out=Gh[b][:], in_=self.gs[hs, :]), reads=["gs"], writes=[("Gh", b)], ring="ag", nring=2)

            later = []

            def tick():
                for ent in later:
                    ent[0] -= 1
                while later and later[0][0] <= 0:
                    later.pop(0)[1]()

            def emit_pv(i):
                h, qb, kt = items[i]
                b = h % 2
                nk = 4 * qb + 4
                blk = h * NQB + qb
                po = 4 + blk % 2
                pb = i % 4
                P = Pt[pb]
                c0 = max(0, kt - 4 * qb) * 128
                tk.op("pe", lambda e: e.matmul(ps[po][:, c0:512], Vh[b][:, kt, :], P[:, c0:512],
                                               start=(kt == 0), stop=(kt == nk - 1)),
                      reads=[("Vh", b), ("P", pb)], writes=[("ps", po)], signal=(kt == nk - 1))
                if kt != nk - 1:
                    return
                hs = slice(h * 64, (h + 1) * 64)
                ob_ = og[blk % 2]
                okey = ("og", blk % 2)
                tk.op("dve", lambda e: e.reciprocal(rl[64:65, :], ps[po][64:65, :]), reads=[("ps", po)], writes=["rl"])

                def epi():
                    tk.op("pe", lambda e: e.matmul(ps[6][0:64, :], self.c32[64:65, C_ONES:C_ONES + 64], rl[64:65, :], start=True, stop=True),
                          reads=["rl", "c32"], writes=[("ps", 6)])
                    tk.op("act", lambda e: e.activation(out=bcs[:], in_=ps[6][0:64, :], func=AF.Copy), reads=[("ps", 6)], writes=["bcs"])
                    tk.op("dve", lambda e: e.tensor_tensor(t1[:], ps[po][0:64, :], bcs[:], ALU.mult),
                          reads=[("ps", po), "bcs"], writes=["t1"])
                    tk.op("dve", lambda e: e.tensor_tensor(ob_[:], t1[:], Gh[b][:, qb * 512:(qb + 1) * 512], ALU.mult),
                          reads=["t1", ("Gh", b)], writes=[okey])
                    tk.dma("sp", lambda e: e.dma_start(out=self.os_[hs, qb * 512:(qb + 1) * 512], in_=ob_[:]),
                           reads=[okey], writes=["os"], ring="aos", nring=2)
                later.append([2, epi])

            loads(0)
            pend = []
            for i, (h, qb, kt) in enumerate(items):
                b = h % 2
                c0 = max(0, kt - 4 * qb) * 128
                pb = i % 4
                P = Pt[pb]
                tk.op("pe", lambda e: e.matmul(ps[pb][:, c0:512], KhT[b][:, kt * 128:(kt + 1) * 128],
                                               QhT[b][:, qb * 512 + c0:(qb + 1) * 512], start=True, stop=True),
                      reads=[("Kh", b), ("Qh", b)], writes=[("ps", pb)])
                tk.op("act", lambda e: e.activation(out=P[:, c0:512], in_=ps[pb][:, c0:512], func=AF.Exp,
                                                    bias=bias[:, qb, kt, h:h + 1], scale=0.125),
                      reads=[("ps", pb), "bias"], writes=[("P", pb)])
                if kt - 4 * qb >= 0:
                    tk.op("dve", lambda e: e.tensor_tensor(P[:, c0:c0 + 128], P[:, c0:c0 + 128], cmb, ALU.mult),
                          reads=[("P", pb), "cbf"], writes=[("P", pb)])
                tick()
                pend.append(i)
                if len(pend) > L:
                    emit_pv(pend.pop(0))
                if i == head_start[h] + L + 2 and h + 1 < 16:
                    loads(h + 1)
            while pend:
                emit_pv(pend.pop(0))
                tick()
            while later:
                later.pop(0)[1]()
            self.barrier()

    def phase_fox_out(self, l):
        tk, nc, T = self.tk, self.nc, self.T
        j = l // 2
        TF = 512
        with ExitStack() as st:
            w_out = self.sb(st, "o_wout", [128, 8, D], BF16)
            xT = [self.sb(st, f"o_xT{i}", [128, 8, TF], F32) for i in range(2)]
            gT = [self.sb(st, f"o_gT{i}", [128, 8, TF], BF16) for i in range(2)]
            self.load_w(w_out, self.wt("fox_w_out", j).rearrange("(k p) f -> p k f", p=128), "wo", D, 512)
            os_v = self.os_.rearrange("(c p) t -> p c t", p=128)
            for ti in range(T // TF):
                t0 = ti * TF
                b = ti % 2
                tk.dma("sp", lambda e: e.dma_start(out=xT[b][:], in_=self.xsT_v[:, :, t0:t0 + TF]),
                       reads=["xsT"], writes=[("ox", b)], ring="oxl", nring=2)
                tk.dma("sp", lambda e: e.dma_start(out=gT[b][:], in_=os_v[:, :, t0:t0 + TF]),
                       reads=["os"], writes=[("og", b)], ring="ogl", nring=2)
                self.out_proj_residual(w_out, gT[b], ("og", b), xT[b], ("ox", b), TF, banks=(0, 1, 2, 3))
                tk.dma("sp", lambda e: e.dma_start(out=self.xsT_v[:, :, t0:t0 + TF], in_=xT[b][:]),
                       reads=[("ox", b)], writes=["xsT"], ring="oxs", nring=2)
            self.barrier()

    def build_all(self, layers=(0, 1, 2, 3)):
        with ExitStack() as st:
            self.setup(st)
            self.phase_in()
            for l in layers:
                self.phase_mod(l)
                if l % 2 == 0:
                    self.phase_gla(l)
                    self.phase_ffn(l, moe=False)
                else:
                    self.phase_fox(l)
                    self.phase_ffn(l, moe=True)
            self.phase_out()
            self.tk.drain("sp")
        return self.nc


from concourse.bass_utils import run_bass_kernel_spmd

SEQ = 4096
BATCH = 4
N_CORES = 4
REAL = {4: [0, 1, 2, 3], 8: [0, 1, 4, 5]}[N_CORES]
_PROG = {}


def _program():
    if "k" not in _PROG:
        k = K(SEQ)
        k.build_all((0, 1, 2, 3))
        _PROG["k"] = k
    return _PROG["k"]


def kernel(**inputs):
    inp = {n: np.asarray(v) for n, v in inputs.items()}
    k = _program()
    consts = make_consts()
    in_maps = []
    for c in range(N_CORES):
        if c in REAL:
            b = REAL.index(c)
            d = {"x": np.ascontiguousarray(inp["x"][b], dtype=np.float32), "consts": consts, "sp": pack_small(inp, b)}
            for n in k.w:
                base, j = n.rsplit("_", 1)
                d[n] = np.ascontiguousarray(inp[base][int(j)], dtype=np.float32)
        else:
            d = {n: np.zeros_like(v) for n, v in in_maps[0].items()}
            d["consts"] = consts
        in_maps.append(d)
    res = run_bass_kernel_spmd(k.nc, in_maps, core_ids=list(range(N_CORES)))
    out = np.stack([np.asarray(res.results[c]["out"], dtype=np.float32) for c in REAL], 0)
    return out
```

```python
from contextlib import ExitStack


class Tok:
    __slots__ = ("sem", "count", "eng")

    def __init__(self, sem=None, count=None, eng=None):
        self.sem = sem
        self.count = count
        self.eng = eng


class Tracker:
    ROLL = 30000

    def __init__(self, nc, stack: ExitStack):
        self.nc = nc
        self.stack = stack
        self.engs = {"pe": nc.tensor, "act": nc.scalar, "dve": nc.vector,
                     "pool": nc.gpsimd, "sp": nc.sync}
        self.sem = {}
        self.cnt = {}
        self.nsem = 0
        for e in self.engs:
            self._new_sem(e)
        self.waited = {e: {} for e in self.engs}
        self.last_w = {}
        self.readers = {}
        self.pending = {e: [] for e in self.engs}
        self.rings = {}
        self.n_ins = 0

    def _alloc(self, name):
        self.nsem += 1
        return self.stack.enter_context(self.nc.semaphore(f"{name}_{self.nsem}"))

    def _new_sem(self, e):
        self.sem[e] = self._alloc(f"e_{e}")
        self.cnt[e] = 0

    def _wait(self, eng, tok):
        if tok is None:
            return
        if tok.eng == eng and eng == "pe":
            return
        assert tok.count is not None, "dependency on an unsignaled instruction"
        w = self.waited[eng]
        k = id(tok.sem)
        if w.get(k, 0) >= tok.count:
            return
        self.engs[eng].wait_ge(tok.sem, tok.count)
        w[k] = tok.count

    def _deps(self, reads, writes):
        deps = []
        for b in reads:
            t = self.last_w.get(b)
            if t is not None:
                deps.append(t)
        for b in writes:
            t = self.last_w.get(b)
            if t is not None:
                deps.append(t)
            deps.extend(self.readers.get(b, ()))
        return deps

    def _record(self, tok, reads, writes):
        for b in reads:
            self.readers.setdefault(b, []).append(tok)
        for b in writes:
            self.last_w[b] = tok
            self.readers[b] = []

    def op(self, eng, fn, reads=(), writes=(), signal=True):
        for t in self._deps(reads, writes):
            self._wait(eng, t)
        ins = fn(self.engs[eng])
        self.n_ins += 1
        if signal:
            if self.cnt[eng] >= self.ROLL:
                self._new_sem(eng)
            self.cnt[eng] += 1
            ins.then_inc(self.sem[eng], 1)
            tok = Tok(self.sem[eng], self.cnt[eng], eng)
            for p in self.pending[eng]:
                p.sem, p.count = tok.sem, tok.count
            self.pending[eng] = []
        else:
            tok = Tok(None, None, eng)
            self.pending[eng].append(tok)
        self._record(tok, reads, writes)
        return tok

    def dma(self, eng, fn, reads=(), writes=(), ring="d", nring=4):
        r = self.rings.get(ring)
        if r is None:
            r = self.rings[ring] = {"sems": [self._alloc(f"r_{ring}") for _ in range(nring)],
                                    "cnt": [0] * nring, "i": 0}
        i = r["i"]
        r["i"] = (i + 1) % len(r["sems"])
        sem = r["sems"][i]
        if r["cnt"][i] > 0:
            self._wait(eng, Tok(sem, r["cnt"][i], "dma"))
        for t in self._deps(reads, writes):
            self._wait(eng, t)
        ins = fn(self.engs[eng])
        self.n_ins += 1
        r["cnt"][i] += 16
        ins.then_inc(sem, 16)
        tok = Tok(sem, r["cnt"][i], "dma")
        self._record(tok, reads, writes)
        return tok

    def drain(self, eng="sp"):
        for r in self.rings.values():
            for sem, c in zip(r["sems"], r["cnt"]):
                if c:
                    self._wait(eng, Tok(sem, c, "dma"))
        for e in self.engs:
            assert not self.pending[e], f"unsignaled tail on {e}"
            if self.cnt[e]:
                self._wait(eng, Tok(self.sem[e], self.cnt[e], e))


import numpy as np
from contextlib import ExitStack
import concourse.bass as bass
import concourse.mybir as mybir

F32 = mybir.dt.float32
BF16 = mybir.dt.bfloat16
AF = mybir.ActivationFunctionType
ALU = mybir.AluOpType
AX = mybir.AxisListType

D = 1024
KC = 8
DFF = 3584
NE = 8
EPS = 1e-6

C_ID, C_MASK, C_TRI, C_REV, C_BD, C_ONES, C_SEL = 0, 128, 256, 386, 514, 642, 770
NCONST = 898


def make_consts():
    c = np.zeros((128, NCONST), np.float32)
    s = np.arange(128)[:, None]
    t = np.arange(128)[None, :]
    c[:, C_ID:C_ID + 128] = (s == t)
    c[:, C_MASK:C_MASK + 128] = (s <= t)
    c[:, C_TRI:C_TRI + 128] = (s <= t) / 16.0
    c[:, C_TRI + 128] = 1.0 / 16.0
    c[:, C_REV:C_REV + 128] = (s > t) / 16.0
    c[:, C_BD:C_BD + 128] = (s // 64 == t // 64)
    c[:, C_ONES:C_ONES + 128] = 1.0
    c[127, C_SEL:C_SEL + 128] = 1.0
    return c


SP = {}
_o = 0
for _n, _w in [("cT", 8), ("adab", 4 * 48), ("ngain", 4 * 2 * 8), ("wgu", 2 * 512), ("onorm", 2 * 2),
               ("bf", 2 * 16), ("qn", 2), ("kn", 2), ("wr", 2 * 8 * 8), ("br", 2 * 8)]:
    SP[_n] = (_o, _w)
    _o += _w
NSP = _o


def pack_small(inp, b):
    sp = np.zeros((128, NSP), np.float32)

    def put(name, arr):
        o, w = SP[name]
        arr = np.asarray(arr, np.float32).reshape(arr.shape[0], -1)
        assert arr.shape[1] == w, (name, arr.shape, w)
        sp[:arr.shape[0], o:o + w] = arr

    fm = lambda v, n: np.asarray(v, np.float32).reshape(n, 128).T
    put("cT", fm(inp["c"][b], 8))
    put("adab", np.stack([fm(inp["ada_b"][l], 48) for l in range(4)], 1))
    put("ngain", np.stack([np.stack([fm(inp["norm_gain"][l, s], 8) for s in range(2)], 1) for l in range(4)], 1))
    wgu = np.zeros((32, 2, 512), np.float32)
    for j in range(2):
        wgu[:16, j] = inp["gla_w_gate_up"][j]
        wgu[16, j] = inp["gla_b_gate"][j]
    put("wgu", wgu)
    put("onorm", np.stack([fm(inp["gla_o_norm"][j], 2) for j in range(2)], 1))
    put("bf", np.asarray(inp["fox_b_f"], np.float32).reshape(1, 32))
    put("qn", np.stack([np.tile(inp["fox_q_norm"][j], 2) for j in range(2)], 1))
    put("kn", np.stack([np.tile(inp["fox_k_norm"][j], 2) for j in range(2)], 1))
    put("wr", np.stack([np.asarray(inp["moe_w_router"][j]).reshape(8, 128, 8).transpose(1, 0, 2) for j in range(2)], 1))
    put("br", np.asarray(inp["moe_b_router"], np.float32).reshape(1, 16))
    return sp


class K:
    def __init__(self, T, dbg=(), G=None, split_last=False):
        self.T = T
        self.G = G or min(2048, T)
        self.split_last = split_last
        self.dbg = set(dbg)
        nc = self.nc = bass.Bass("TRN2", target_bir_lowering=False)
        dt = nc.dram_tensor
        self.x = dt("x", [T, D], F32, kind="ExternalInput").ap()
        self.consts_d = dt("consts", [128, NCONST], F32, kind="ExternalInput").ap()
        self.sp_d = dt("sp", [128, NSP], F32, kind="ExternalInput").ap()
        if split_last:
            self.sel_d = dt("sel", [1, 4], mybir.dt.int32, kind="ExternalInput").ap()
        self.w = {}
        self.wshape = {"ada_w": [D, 6 * D], "gla_w_in": [D, 3088], "gla_w_out": [D, D],
                       "fox_w_in": [D, 4112], "fox_w_out": [D, D],
                       "ffn_w_gate": [D, DFF], "ffn_w_up": [D, DFF], "ffn_w_down": [DFF, D],
                       "moe_w_gate": [NE, D, DFF], "moe_w_up": [NE, D, DFF], "moe_w_down": [NE, DFF, D]}
        self.out = dt("out", [T, D], F32, kind="ExternalOutput").ap()
        self.xsT = dt("xsT", [D, T], F32).ap()
        self.xsT_v = self.xsT.rearrange("(c p) t -> p c t", p=128)
        self.qs = dt("qs", [D, T], BF16).ap()
        self.ks = dt("ks", [D, T], BF16).ap()
        self.vs = dt("vs", [T, D], BF16).ap()
        self.gs = dt("gs", [D, T], BF16).ap()
        self.os_ = dt("os", [D, T], BF16).ap()
        self.dbg_out = {}

    def wt(self, name, j):
        key = f"{name}_{j}"
        if key not in self.w:
            self.w[key] = self.nc.dram_tensor(key, self.wshape[name], F32, kind="ExternalInput").ap()
        return self.w[key]

    def sb(self, st, name, shape, dtype):
        self.uid = getattr(self, "uid", 0) + 1
        return st.enter_context(self.nc.sbuf_tensor(f"{name}_u{self.uid}", shape, dtype))

    def setup(self, st):
        nc = self.nc
        self.tk = tk = Tracker(nc, st)
        self.c32 = self.sb(st, "c32", [128, NCONST], F32)
        self.spt = self.sb(st, "spt", [128, NSP], F32)
        self.cbf = self.sb(st, "cbf", [128, NCONST], BF16)
        self.mod = self.sb(st, "mod", [128, 48], F32)
        self.A = self.sb(st, "modA", [128, 2, 8], F32)
        self.ps = [st.enter_context(nc.psum_tensor(f"ps{i}", [128, 512], F32)) for i in range(8)]
        self.Fk = self.sb(st, "Fk", [128, self.T // 128, 16], F32)
        self.Fref = self.sb(st, "Fref", [128, max(1, self.T // 512), 16], F32)
        self.bfb = self.sb(st, "bfb", [1, 32], BF16)
        tk.dma("sp", lambda e: e.dma_start(out=self.c32[:], in_=self.consts_d), writes=["c32"], ring="c")
        tk.dma("sp", lambda e: e.dma_start(out=self.spt[:], in_=self.sp_d), writes=["spt"], ring="c")
        tk.op("dve", lambda e: e.tensor_copy(self.cbf[:], self.c32[:]), reads=["c32"], writes=["cbf"])
        ob, _ = SP["bf"]
        tk.op("dve", lambda e: e.tensor_copy(self.bfb[:], self.spt[0:1, ob:ob + 32]), reads=["spt"], writes=["bfb"])
        if self.split_last:
            self.sel_sb = self.sb(st, "sel_sb", [1, 4], mybir.dt.int32)
            self.r_col = st.enter_context(nc.sync.register("r_col"))
            sem = tk._alloc("selsem")
            nc.sync.dma_start(out=self.sel_sb[:], in_=self.sel_d).then_inc(sem, 16)
            nc.sync.wait_ge(sem, 16)
            nc.sync.reg_load(self.r_col, self.sel_sb[0:1, 1:2])

    def spv(self, name):
        o, w = SP[name]
        return self.spt[:, o:o + w]

    def barrier(self):
        tk = self.tk
        tk.drain("sp")
        self.nc.all_engine_barrier()
        tk.last_w.clear()
        tk.readers.clear()

    def phase_in(self):
        tk, nc = self.tk, self.nc
        with ExitStack() as st:
            xin = [self.sb(st, f"xin{i}", [128, D], F32) for i in range(2)]
            xT = [self.sb(st, f"xTi{i}", [128, 8, 128], F32) for i in range(2)]
            ident = self.c32[:, C_ID:C_ID + 128]
            for tt in range(self.T // 128):
                b = tt % 2
                tk.dma("sp", lambda e: e.dma_start(out=xin[b][:], in_=self.x[tt * 128:(tt + 1) * 128, :]),
                       writes=[("xin", b)], ring="xin", nring=2)
                for half in range(2):
                    pb = (tt * 2 + half) % 8
                    for j in range(4):
                        c = half * 4 + j
                        tk.op("pe", lambda e: e.transpose(self.ps[pb][:, j * 128:(j + 1) * 128],
                                                          xin[b][:, c * 128:(c + 1) * 128], ident),
                              reads=[("xin", b), "c32"], writes=[("ps", pb)], signal=(j == 3))
                    src = self.ps[pb][:].rearrange("p (c t) -> p c t", c=4)
                    dst = xT[b][:, half * 4:(half + 1) * 4, :]
                    if half == 0:
                        tk.op("dve", lambda e: e.tensor_copy(dst, src), reads=[("ps", pb)], writes=[("xTi", b, half)])
                    else:
                        tk.op("act", lambda e: e.activation(out=dst, in_=src, func=AF.Copy),
                              reads=[("ps", pb)], writes=[("xTi", b, half)])
                tk.dma("sp", lambda e: e.dma_start(out=self.xsT_v[:, :, tt * 128:(tt + 1) * 128], in_=xT[b][:]),
                       reads=[("xTi", b, 0), ("xTi", b, 1)], writes=["xsT"], ring="xst", nring=2)
            self.barrier()

    def phase_out(self):
        tk, nc = self.tk, self.nc
        with ExitStack() as st:
            xin = [self.sb(st, f"xo{i}", [128, D], F32) for i in range(2)]
            xT = [self.sb(st, f"xTo{i}", [128, 8, 128], F32) for i in range(2)]
            ident = self.c32[:, C_ID:C_ID + 128]
            for tt in range(self.T // 128):
                b = tt % 2
                tk.dma("sp", lambda e: e.dma_start(out=xT[b][:], in_=self.xsT_v[:, :, tt * 128:(tt + 1) * 128]),
                       reads=["xsT"], writes=[("xTo", b)], ring="xld", nring=2)
                for half in range(2):
                    pb = (tt * 2 + half) % 8
                    for j in range(4):
                        c = half * 4 + j
                        tk.op("pe", lambda e: e.transpose(self.ps[pb][:, j * 128:(j + 1) * 128], xT[b][:, c, :], ident),
                              reads=[("xTo", b), "c32"], writes=[("ps", pb)], signal=(j == 3))
                    dst = xin[b][:, half * 512:(half + 1) * 512]
                    if half == 0:
                        tk.op("dve", lambda e: e.tensor_copy(dst, self.ps[pb][:]), reads=[("ps", pb)], writes=[("xo", b, half)])
                    else:
                        tk.op("act", lambda e: e.activation(out=dst, in_=self.ps[pb][:], func=AF.Copy),
                              reads=[("ps", pb)], writes=[("xo", b, half)])
                tk.dma("sp", lambda e: e.dma_start(out=self.out[tt * 128:(tt + 1) * 128, :], in_=xin[b][:]),
                       reads=[("xo", b, 0), ("xo", b, 1)], writes=[("out", tt)], ring="ost", nring=2)
            self.barrier()

    def phase_mod(self, l):
        tk, nc = self.tk, self.nc
        with ExitStack() as st:
            cond = self.sb(st, "cond", [128, 8], BF16)
            wa = [self.sb(st, f"wa{i}", [128, 8, 1536], BF16) for i in range(2)]
            tk.op("act", lambda e: e.activation(out=cond[:], in_=self.spv("cT"), func=AF.Silu),
                  reads=["spt"], writes=["cond"])
            wv = self.wt("ada_w", l).rearrange("(k p) j -> p k j", p=128)
            pm = self.ps[0]
            for q in range(4):
                b = q % 2
                tk.dma("pool", lambda e: e.dma_start(out=wa[b][:], in_=wv[:, :, q * 1536:(q + 1) * 1536]),
                       writes=[("wa", b)], ring="wa", nring=2)
                for jc in range(12):
                    col = q * 12 + jc
                    for k in range(8):
                        tk.op("pe", lambda e: e.matmul(pm[:, col:col + 1], wa[b][:, k, jc * 128:(jc + 1) * 128],
                                                       cond[:, k:k + 1], start=(k == 0), stop=(k == 7)),
                              reads=[("wa", b), "cond"], writes=[("ps", 0)], signal=(k == 7 and jc == 11))
            o, _ = SP["adab"]
            tk.op("dve", lambda e: e.tensor_tensor(self.mod[:], pm[:, 0:48], self.spt[:, o + l * 48:o + (l + 1) * 48], ALU.add),
                  reads=[("ps", 0), "spt"], writes=["mod"])
            og, _ = SP["ngain"]
            for s in range(2):
                sc = self.mod[:, 8:16] if s == 0 else self.mod[:, 32:40]
                gain = self.spt[:, og + (l * 2 + s) * 8:og + (l * 2 + s + 1) * 8]
                tk.op("dve", lambda e: e.scalar_tensor_tensor(self.A[:, s, :], sc, 1.0, gain, ALU.add, ALU.mult),
                      reads=["mod", "spt"], writes=["modA"])
            self.barrier()

    def shift(self, s):
        return self.mod[:, 0:8] if s == 0 else self.mod[:, 24:32]

    def gate(self, s):
        return self.mod[:, 16:24] if s == 0 else self.mod[:, 40:48]

    def norm_h(self, s, xT, xkey, n, sq, xr, rstd, outs, okeys, pbank, h32=None):
        tk = self.tk
        ones = self.cbf[:, C_ONES:C_ONES + 128]
        tk.op("act", lambda e: e.activation(out=sq[:, :, :n], in_=xT, func=AF.Square),
              reads=[xkey], writes=["sq"])
        for c in range(8):
            tk.op("pe", lambda e: e.matmul(self.ps[pbank][:, :n], ones, sq[:, c, :n], start=(c == 0), stop=(c == 7)),
                  reads=["sq", "cbf"], writes=[("ps", pbank)], signal=(c == 7))
        tk.op("act", lambda e: e.activation(out=rstd[:, :n], in_=self.ps[pbank][:, :n], func=AF.Sqrt, bias=EPS, scale=1.0 / D),
              reads=[("ps", pbank)], writes=["rstd"])
        tk.op("dve", lambda e: e.reciprocal(rstd[:, :n], rstd[:, :n]), reads=["rstd"], writes=["rstd"])
        rb = rstd[:, :n].unsqueeze(1).broadcast_to([128, 8, n])
        tk.op("dve", lambda e: e.tensor_tensor(xr[:, :, :n], xT, rb, ALU.mult),
              reads=[xkey, "rstd"], writes=["xr"])
        sh = self.shift(s)
        for c in range(8):
            dst = outs[c] if h32 is None else h32[:, c, :n]
            tk.op("act", lambda e: e.activation(out=dst, in_=xr[:, c, :n], func=AF.Identity,
                                                bias=sh[:, c:c + 1], scale=self.A[:, s, c:c + 1]),
                  reads=["xr", "mod", "modA"], writes=[okeys[c]] if h32 is None else [("h32", c)])
            if h32 is not None:
                tk.op("dve", lambda e: e.tensor_copy(outs[c], h32[:, c, :n]), reads=[("h32", c)], writes=[okeys[c]])

    def phase_ffn(self, l, moe, dyn=False):
        tk, nc, G = self.tk, self.nc, self.G
        j = l // 2
        NT = G // 512
        with ExitStack() as st:
            xT = self.sb(st, "f_xT", [128, 8, G], F32)
            hT = self.sb(st, "f_hT", [128, 8, G], BF16)
            wg = [self.sb(st, f"f_wg{i}", [128, 8, 512], BF16) for i in range(2)]
            wu = [self.sb(st, f"f_wu{i}", [128, 8, 512], BF16) for i in range(2)]
            wd = [self.sb(st, f"f_wd{i}", [128, 4, D], BF16) for i in range(2)]
            if moe:
                comb = self.sb(st, "f_comb", [128, G // 128, 8], F32)
                wr = self.spv("wr").rearrange("p (j k e) -> p j k e", j=2, k=8)
                obr, _ = SP["br"]
            g2 = self.gate(1)
            nexp = NE if moe else 1
            ident = self.c32[:, C_ID:C_ID + 128]
            ones32 = self.c32[:, C_ONES:C_ONES + 128]
            wi = 0
            for grp in range(1 if dyn else self.T // G):
                t0 = grp * G
                if dyn:
                    grp_ap = bass.AP(self.xsT.tensor, self.r_col, [[self.T, 128], [128 * self.T, 8], [1, G]])
                else:
                    grp_ap = self.xsT_v[:, :, t0:t0 + G]
                tk.dma("sp", lambda e: e.dma_start(out=xT[:], in_=grp_ap),
                       reads=["xsT"], writes=[("fx", tt) for tt in range(NT)], ring="fxl", nring=1)
                with ExitStack() as st2:
                    sq = self.sb(st2, "f_sq", [128, 8, 512], BF16)
                    xr = self.sb(st2, "f_xr", [128, 8, 512], F32)
                    rstd = self.sb(st2, "f_rstd", [128, 512], F32)
                    if moe:
                        lg = self.sb(st2, "f_lg", [128, 8], F32)
                        mx = self.sb(st2, "f_mx", [128, 8], F32)
                        tmp8 = self.sb(st2, "f_tmp8", [128, 8], F32)
                        msk = self.sb(st2, "f_msk", [128, 8], F32)
                        nl1 = self.sb(st2, "f_nl1", [128, 1], F32)
                        den = self.sb(st2, "f_den", [128, 1], F32)
                        h32 = xr
                    for tt in range(NT):
                        cs = slice(tt * 512, (tt + 1) * 512)
                        self.norm_h(1, xT[:, :, cs], ("fx", tt), 512, sq, xr, rstd,
                                    [hT[:, c, cs] for c in range(8)], [("fh", tt)] * 8, 7,
                                    h32=(h32 if moe else None))
                        if moe:
                            for s4 in range(4):
                                si = tt * 4 + s4
                                for k in range(8):
                                    tk.op("pe", lambda e: e.matmul(self.ps[6][:, 0:8], h32[:, k, s4 * 128:(s4 + 1) * 128],
                                                                   wr[:, j, k, :], start=(k == 0), stop=False),
                                          reads=[("h32", k), "spt"], writes=[("ps", 6)], signal=False)
                                tk.op("pe", lambda e: e.matmul(self.ps[6][:, 0:8], ones32[0:1, :],
                                                               self.spt[0:1, obr + j * 8:obr + (j + 1) * 8], start=False, stop=True),
                                      reads=["c32", "spt"], writes=[("ps", 6)])
                                tk.op("dve", lambda e: e.tensor_copy(lg[:], self.ps[6][:, 0:8]), reads=[("ps", 6)], writes=["lg"])
                                tk.op("dve", lambda e: e.max(mx[:], lg[:]), reads=["lg"], writes=["mx"])
                                tk.op("dve", lambda e: e.tensor_scalar(msk[:], lg[:], mx[:, 1:2], None, ALU.is_ge),
                                      reads=["lg", "mx"], writes=["msk"])
                                tk.op("dve", lambda e: e.tensor_scalar(nl1[:], mx[:, 0:1], -1.0, None, ALU.mult),
                                      reads=["mx"], writes=["nl1"])
                                tk.op("act", lambda e: e.activation(out=tmp8[:], in_=lg[:], func=AF.Exp, bias=nl1[:, 0:1], scale=1.0),
                                      reads=["lg", "nl1"], writes=["tmp8"])
                                tk.op("dve", lambda e: e.tensor_tensor(tmp8[:], tmp8[:], msk[:], ALU.mult),
                                      reads=["tmp8", "msk"], writes=["tmp8"])
                                tk.op("dve", lambda e: e.reduce_sum(den[:], tmp8[:], AX.X), reads=["tmp8"], writes=["den"])
                                tk.op("dve", lambda e: e.reciprocal(den[:], den[:]), reads=["den"], writes=["den"])
                                tk.op("dve", lambda e: e.tensor_scalar(comb[:, si, :], tmp8[:], den[:, 0:1], None, ALU.mult),
                                      reads=["tmp8", "den"], writes=["comb"])
                    self.barrier()
                with ExitStack() as st3:
                    hid = self.sb(st3, "f_hid", [128, 4, G], BF16)
                    sg = [self.sb(st3, f"f_sg{i}", [128, 512], F32) for i in range(2)]
                    if moe:
                        combB = [self.sb(st3, f"f_combB{i}", [128, G], F32) for i in range(2)]
                        dg = [self.sb(st3, f"f_dg{i}", [128, 128], F32) for i in range(2)]
                    for ex in range(nexp):
                        if moe:
                            cb = combB[ex % 2]
                            ckey = ("combB", ex % 2)
                            for si in range(G // 128):
                                dgt = dg[si % 2]
                                tk.op("dve", lambda e: e.tensor_scalar(dgt[:], ident, comb[:, si, ex:ex + 1], None, ALU.mult),
                                      reads=["comb", "c32"], writes=[("dg", si % 2)])
                                pb = 6 + si % 2
                                tk.op("pe", lambda e: e.matmul(self.ps[pb][:, 0:128], ones32, dgt[:], start=True, stop=True),
                                      reads=[("dg", si % 2), "c32"], writes=[("ps", pb)])
                                tk.op("act", lambda e: e.activation(out=cb[:, si * 128:(si + 1) * 128], in_=self.ps[pb][:, 0:128], func=AF.Copy),
                                      reads=[("ps", pb)], writes=[(ckey, si)])
                            Wg = self.wt("moe_w_gate", j)[ex]
                            Wu = self.wt("moe_w_up", j)[ex]
                            Wd = self.wt("moe_w_down", j)[ex]
                        else:
                            Wg = self.wt("ffn_w_gate", j)
                            Wu = self.wt("ffn_w_up", j)
                            Wd = self.wt("ffn_w_down", j)
                        Wg = Wg.rearrange("(k p) f -> p k f", p=128)
                        Wu = Wu.rearrange("(k p) f -> p k f", p=128)
                        Wd = Wd.rearrange("(k p) o -> p k o", p=128)
                        for fc in range(DFF // 512):
                            b = wi % 2
                            wi += 1
                            fs = slice(fc * 512, (fc + 1) * 512)
                            tk.dma("pool", lambda e: e.dma_start(out=wg[b][:], in_=Wg[:, :, fs]), writes=[("wg", b)], ring="wg", nring=2)
                            tk.dma("pool", lambda e: e.dma_start(out=wu[b][:], in_=Wu[:, :, fs]), writes=[("wu", b)], ring="wu", nring=2)
                            tk.dma("pool", lambda e: e.dma_start(out=wd[b][:], in_=Wd[:, fc * 4:(fc + 1) * 4, :]), writes=[("wd", b)], ring="wd", nring=2)
                            it = 0
                            for ft in range(4):
                                for tt in range(NT):
                                    cs = slice(tt * 512, (tt + 1) * 512)
                                    pg, pu = (0, 1) if it % 2 == 0 else (2, 3)
                                    sgt = sg[it % 2]
                                    skey = ("sg", it % 2)
                                    it += 1
                                    for k in range(8):
                                        tk.op("pe", lambda e: e.matmul(self.ps[pg][:], wg[b][:, k, ft * 128:(ft + 1) * 128], hT[:, k, cs],
                                                                       start=(k == 0), stop=(k == 7)),
                                              reads=[("wg", b), ("fh", tt)], writes=[("ps", pg)], signal=(k == 7))
                                    for k in range(8):
                                        tk.op("pe", lambda e: e.matmul(self.ps[pu][:], wu[b][:, k, ft * 128:(ft + 1) * 128], hT[:, k, cs],
                                                                       start=(k == 0), stop=(k == 7)),
                                              reads=[("wu", b), ("fh", tt)], writes=[("ps", pu)], signal=(k == 7))
                                    tk.op("act", lambda e: e.activation(out=sgt[:], in_=self.ps[pg][:], func=AF.Silu),
                                          reads=[("ps", pg)], writes=[skey])
                                    if moe:
                                        tk.op("dve", lambda e: e.tensor_tensor(sgt[:], sgt[:], self.ps[pu][:], ALU.mult),
                                              reads=[skey, ("ps", pu)], writes=[skey])
                                        tk.op("dve", lambda e: e.tensor_tensor(hid[:, ft, cs], sgt[:], cb[:, cs], ALU.mult),
                                              reads=[skey] + [(ckey, tt * 4 + q4) for q4 in range(4)], writes=[("hid", ft, tt)])
                                    else:
                                        tk.op("dve", lambda e: e.tensor_tensor(hid[:, ft, cs], sgt[:], self.ps[pu][:], ALU.mult),
                                              reads=[skey, ("ps", pu)], writes=[("hid", ft, tt)])
                            it = 0
                            for oc in range(8):
                                for tt in range(NT):
                                    cs = slice(tt * 512, (tt + 1) * 512)
                                    py = 4 + it % 2
                                    it += 1
                                    for ft in range(4):
                                        tk.op("pe", lambda e: e.matmul(self.ps[py][:], wd[b][:, ft, oc * 128:(oc + 1) * 128], hid[:, ft, cs],
                                                                       start=(ft == 0), stop=(ft == 3)),
                                              reads=[("wd", b), ("hid", ft, tt)], writes=[("ps", py)], signal=(ft == 3))
                                    tk.op("dve", lambda e: e.scalar_tensor_tensor(xT[:, oc, cs], self.ps[py][:], g2[:, oc:oc + 1], xT[:, oc, cs],
                                                                                  ALU.mult, ALU.add),
                                          reads=[("ps", py), ("fx", tt), "mod"], writes=[("fx", tt)])
                    tk.dma("sp", lambda e: e.dma_start(out=grp_ap, in_=xT[:]),
                           reads=[("fx", tt) for tt in range(NT)], writes=["xsT"], ring="fxs", nring=1)
                    self.barrier()

    def out_proj_residual(self, wo, gated, gkey, xT, xkey, n, banks=(0, 1)):
        tk = self.tk
        g1 = self.gate(0)
        for oc in range(8):
            pb = banks[oc % len(banks)]
            for k in range(8):
                tk.op("pe", lambda e: e.matmul(self.ps[pb][:, :n], wo[:, k, oc * 128:(oc + 1) * 128], gated[:, k, :n],
                                               start=(k == 0), stop=(k == 7)),
                      reads=["wo", gkey], writes=[("ps", pb)], signal=(k == 7))
            tk.op("dve", lambda e: e.scalar_tensor_tensor(xT[:, oc, :n], self.ps[pb][:, :n], g1[:, oc:oc + 1], xT[:, oc, :n],
                                                          ALU.mult, ALU.add),
                  reads=[("ps", pb), xkey, "mod"], writes=[xkey])

    def load_w(self, dst, src_v, key, ncol, piece=1024):
        for c0 in range(0, ncol, piece):
            c1 = min(ncol, c0 + piece)
            self.tk.dma("pool", lambda e: e.dma_start(out=dst[:, :, c0:c1], in_=src_v[:, :, c0:c1]),
                        writes=[key], ring="wload", nring=4)

    def phase_gla(self, l):
        tk, nc, T = self.tk, self.nc, self.T
        j = l // 2
        TG, NS = 256, 2
        with ExitStack() as st:
            w_in = self.sb(st, "g_win", [128, 8, 3088], BF16)
            w_out = self.sb(st, "g_wout", [128, 8, D], BF16)
            xT = self.sb(st, "g_xT", [128, 8, TG], F32)
            bufA = self.sb(st, "g_bufA", [128, 8, TG], F32)
            sq = self.sb(st, "g_sq", [128, 8, TG], BF16)
            hT = self.sb(st, "g_hT", [128, 8, TG], BF16)
            rstd = self.sb(st, "g_rstd", [128, 512], F32)
            qkT = self.sb(st, "g_qkT", [128, 8, TG], F32)
            rT = self.sb(st, "g_rT", [128, 8, TG], BF16)
            glow = self.sb(st, "g_glow", [32, TG], F32)
            v = self.sb(st, "g_v", [128, NS, D], BF16)
            ktok = self.sb(st, "g_ktok", [128, NS, 512], F32)
            az_ = [self.sb(st, f"g_az{i}", [128, 512], F32) for i in range(NS)]
            zs_ = [self.sb(st, f"g_zs{i}", [128, 512], F32) for i in range(NS)]
            la_ = [self.sb(st, f"g_la{i}", [128, 512], F32) for i in range(NS)]
            Eq_ = [self.sb(st, f"g_Eq{i}", [128, 512], F32) for i in range(NS)]
            Ek_ = [self.sb(st, f"g_Ek{i}", [128, 512], F32) for i in range(NS)]
            Eend_ = [self.sb(st, f"g_Eend{i}", [128, 512], F32) for i in range(NS)]
            kend = self.sb(st, "g_kend", [128, 512], BF16)
            qd = self.sb(st, "g_qd", [128, 4, 128], BF16)
            ki = self.sb(st, "g_ki", [128, 4, 128], BF16)
            am = self.sb(st, "g_am", [128, 4, 128], BF16)
            S = self.sb(st, "g_S", [128, 4, 256], F32)
            Sbf = self.sb(st, "g_Sbf", [128, 4, 256], BF16)
            rso = self.sb(st, "g_rso", [128, 4, TG], F32)
            tmpo = self.sb(st, "g_tmpo", [128, TG], F32)
            ps = self.ps
            self.load_w(w_in, self.wt("gla_w_in", j).rearrange("(k p) f -> p k f", p=128), "win", 3088, 772)
            self.load_w(w_out, self.wt("gla_w_out", j).rearrange("(k p) f -> p k f", p=128), "wo", D, 512)
            tk.op("dve", lambda e: e.memset(S[:], 0.0), writes=["S"])
            tk.op("dve", lambda e: e.memset(Sbf[:], 0.0), writes=["Sbf"])
            tk.op("dve", lambda e: e.memset(glow[:], 1.0), writes=["glow"])
            ow, _ = SP["wgu"]
            wgu = self.spt[0:17, ow + j * 512:ow + (j + 1) * 512]
            oo, _ = SP["onorm"]
            onorm = self.spt[:, oo + j * 2:oo + j * 2 + 2]
            tri = self.c32[:, C_TRI:C_TRI + 128]
            rev = self.c32[:, C_REV:C_REV + 128]
            ones = self.cbf[:, C_ONES:C_ONES + 128]
            maskb = self.c32[:, C_MASK:C_MASK + 128].unsqueeze(1).broadcast_to([128, 4, 128])
            oT = bufA
            gated = hT
            ev = 0
            for ti in range(T // TG):
                t0 = ti * TG
                tk.dma("sp", lambda e: e.dma_start(out=xT[:], in_=self.xsT_v[:, :, t0:t0 + TG]),
                       reads=["xsT"], writes=["gx"], ring="gxl", nring=1)
                self.norm_h(0, xT[:], "gx", TG, sq, bufA, rstd, [hT[:, c, :] for c in range(8)], ["gh"] * 8, 7)
                for k in range(8):
                    tk.op("pe", lambda e: e.matmul(ps[7][0:16, :TG], w_in[:, k, 3072:3088], hT[:, k, :],
                                                   start=(k == 0), stop=(k == 7)),
                          reads=["win", "gh"], writes=[("ps", 7)], signal=(k == 7))
                tk.op("dve", lambda e: e.tensor_copy(glow[0:16, :], ps[7][0:16, :TG]), reads=[("ps", 7)], writes=["glow"])
                groups = []

                def g_qk(oc):
                    pb = oc % 2
                    for k in range(8):
                        tk.op("pe", lambda e: e.matmul(ps[pb][:, :TG], w_in[:, k, oc * 128:(oc + 1) * 128], hT[:, k, :],
                                                       start=(k == 0), stop=(k == 7)),
                              reads=["win", "gh"], writes=[("ps", pb)], signal=(k == 7))
                    if oc % 2 == 0:
                        tk.op("act", lambda e: e.activation(out=qkT[:, oc, :], in_=ps[pb][:, :TG], func=AF.Copy),
                              reads=[("ps", pb)], writes=[("qk", oc)])
                    else:
                        tk.op("dve", lambda e: e.tensor_copy(qkT[:, oc, :], ps[pb][:, :TG]),
                              reads=[("ps", pb)], writes=[("qk", oc)])

                def g_r(oc):
                    pb = 2 + oc % 2
                    for k in range(8):
                        tk.op("pe", lambda e: e.matmul(ps[pb][:, :TG], w_in[:, k, 2048 + oc * 128:2048 + (oc + 1) * 128], hT[:, k, :],
                                                       start=(k == 0), stop=(k == 7)),
                              reads=["win", "gh"], writes=[("ps", pb)], signal=(k == 7))
                    tk.op("act", lambda e: e.activation(out=rT[:, oc, :], in_=ps[pb][:, :TG], func=AF.Silu),
                          reads=[("ps", pb)], writes=[("rT", oc)])

                def g_v(s, half):
                    tok = slice(s * 128, (s + 1) * 128)
                    pb = half
                    for k in range(8):
                        tk.op("pe", lambda e: e.matmul(ps[pb][:], hT[:, k, tok], w_in[:, k, 1024 + half * 512:1536 + half * 512],
                                                       start=(k == 0), stop=(k == 7)),
                              reads=["win", "gh"], writes=[("ps", pb)], signal=(k == 7))
                    if half == 0:
                        tk.op("act", lambda e: e.activation(out=v[:, s, 0:512], in_=ps[pb][:], func=AF.Copy),
                              reads=[("ps", pb)], writes=[("v", s, 0)])
                    else:
                        tk.op("dve", lambda e: e.tensor_copy(v[:, s, 512:1024], ps[pb][:]),
                              reads=[("ps", pb)], writes=[("v", s, 1)])

                def g_kt(s):
                    tok = slice(s * 128, (s + 1) * 128)
                    pb = 2 + s % 2
                    for k in range(8):
                        tk.op("pe", lambda e: e.matmul(ps[pb][:], hT[:, k, tok], w_in[:, k, 512:1024],
                                                       start=(k == 0), stop=(k == 7)),
                              reads=["win", "gh"], writes=[("ps", pb)], signal=(k == 7))
                    tk.op("dve", lambda e: e.tensor_copy(ktok[:, s, :], ps[pb][:]), reads=[("ps", pb)], writes=[("ktok", s)])

                for oc in range(8):
                    groups.append(lambda oc=oc: g_qk(oc))
                for s in range(NS):
                    groups.append(lambda s=s: g_v(s, 0))
                    groups.append(lambda s=s: g_v(s, 1))
                    groups.append(lambda s=s: g_kt(s))
                for oc in range(8):
                    groups.append(lambda oc=oc: g_r(oc))
                chain = []
                for s in range(NS):
                    tok = slice(s * 128, (s + 1) * 128)
                    zs, az, la, Eq, Ek, Eend = zs_[s], az_[s], la_[s], Eq_[s], Ek_[s], Eend_[s]
                    chain += [
                        lambda s=s, tok=tok: tk.op("pe", lambda e: e.matmul(ps[4][:], glow[0:17, tok], wgu, start=True, stop=True),
                                                   reads=["glow", "spt"], writes=[("ps", 4)]),
                        lambda s=s, zs=zs: tk.op("act", lambda e: e.activation(out=zs[:], in_=ps[4][:], func=AF.Copy),
                                                 reads=[("ps", 4)], writes=[("zs", s)]),
                        lambda s=s, zs=zs, az=az: tk.op("dve", lambda e: e.scalar_tensor_tensor(az[:], zs[:], -1.0, zs[:], ALU.mult, ALU.min),
                                                        reads=[("zs", s)], writes=[("az", s)]),
                        lambda s=s, az=az: tk.op("act", lambda e: e.activation(out=az[:], in_=az[:], func=AF.Exp),
                                                 reads=[("az", s)], writes=[("az", s)]),
                        lambda s=s, az=az: tk.op("act", lambda e: e.activation(out=az[:], in_=az[:], func=AF.Ln, bias=1.0, scale=1.0),
                                                 reads=[("az", s)], writes=[("az", s)]),
                        lambda s=s, zs=zs, az=az, la=la: tk.op("dve", lambda e: e.scalar_tensor_tensor(la[:], zs[:], 0.0, az[:], ALU.min, ALU.subtract),
                                                               reads=[("zs", s), ("az", s)], writes=[("la", s)]),
                        lambda s=s, la=la: [tk.op("pe", lambda e: e.matmul(ps[5][:, h * 128:(h + 1) * 128], la[:, h * 128:(h + 1) * 128], tri,
                                                                            start=True, stop=True),
                                                  reads=[("la", s), "c32"], writes=[("ps", 5)], signal=(h == 3)) for h in range(4)],
                        lambda s=s, Eq=Eq: tk.op("act", lambda e: e.activation(out=Eq[:], in_=ps[5][:], func=AF.Exp),
                                                 reads=[("ps", 5)], writes=[("Eq", s)]),
                        lambda s=s, Ek=Ek: tk.op("act", lambda e: e.activation(out=Ek[:], in_=ps[5][:], func=AF.Exp, scale=-1.0),
                                                 reads=[("ps", 5)], writes=[("Ek", s)]),
                        lambda s=s, la=la: tk.op("pe", lambda e: e.matmul(ps[4][:], rev, la[:], start=True, stop=True),
                                                 reads=[("la", s), "c32"], writes=[("ps", 4)]),
                        lambda s=s, Eend=Eend: tk.op("act", lambda e: e.activation(out=Eend[:], in_=ps[4][:], func=AF.Exp),
                                                     reads=[("ps", 4)], writes=[("Eend", s)]),
                    ]
                for i in range(max(len(groups), len(chain))):
                    if i < len(groups):
                        groups[i]()
                    if i < len(chain):
                        chain[i]()
                for s in range(NS):
                    tok = slice(s * 128, (s + 1) * 128)
                    Eq, Ek, Eend = Eq_[s], Ek_[s], Eend_[s]
                    tk.op("dve", lambda e: e.tensor_tensor(kend[:], ktok[:, s, :], Eend[:], ALU.mult),
                          reads=[("ktok", s), ("Eend", s)], writes=["kend"])
                    tk.op("dve", lambda e: e.scalar_tensor_tensor(qd[:], qkT[:, 0:4, tok], 128.0 ** -0.5,
                                                                  Eq[:].rearrange("p (h t) -> p h t", h=4), ALU.mult, ALU.mult),
                          reads=[("qk", c) for c in range(4)] + [("Eq", s)], writes=["qd"])
                    tk.op("dve", lambda e: e.tensor_tensor(ki[:], qkT[:, 4:8, tok], Ek[:].rearrange("p (h t) -> p h t", h=4), ALU.mult),
                          reads=[("qk", c) for c in range(4, 8)] + [("Ek", s)], writes=["ki"])
                    for h in range(4):
                        tk.op("pe", lambda e: e.matmul(ps[6][:, h * 128:(h + 1) * 128], ki[:, h, :], qd[:, h, :], start=True, stop=True),
                              reads=["ki", "qd"], writes=[("ps", 6)], signal=(h == 3))
                    tk.op("dve", lambda e: e.tensor_tensor(am[:], ps[6][:].rearrange("p (h t) -> p h t", h=4), maskb, ALU.mult),
                          reads=[("ps", 6), "c32"], writes=["am"])
                    for c in range(8):
                        h, jj = c // 2, c % 2
                        pb = c // 4
                        col = slice((c % 4) * 128, (c % 4 + 1) * 128)
                        tk.op("pe", lambda e: e.matmul(ps[pb][:, col], v[:, s, c * 128:(c + 1) * 128], am[:, h, :], start=True, stop=False),
                              reads=[("v", s, c // 4), "am"], writes=[("ps", pb)], signal=False)
                        tk.op("pe", lambda e: e.matmul(ps[pb][:, col], Sbf[:, h, jj * 128:(jj + 1) * 128], qd[:, h, :], start=False, stop=True),
                              reads=["Sbf", "qd"], writes=[("ps", pb)], signal=(c % 4 == 3))
                    tk.op("act", lambda e: e.activation(out=oT[:, 0:4, tok], in_=ps[0][:].rearrange("p (c t) -> p c t", c=4), func=AF.Copy),
                          reads=[("ps", 0)], writes=["oT"])
                    tk.op("dve", lambda e: e.tensor_copy(oT[:, 4:8, tok], ps[1][:].rearrange("p (c t) -> p c t", c=4)),
                          reads=[("ps", 1)], writes=["oT"])
                    for h in range(4):
                        pb = 2 + h // 2
                        col = slice((h % 2) * 256, (h % 2 + 1) * 256)
                        tk.op("pe", lambda e: e.matmul(ps[pb][:, col], kend[:, h * 128:(h + 1) * 128], v[:, s, h * 256:(h + 1) * 256],
                                                       start=True, stop=True),
                              reads=["kend", ("v", s, h // 2)], writes=[("ps", pb)], signal=(h % 2 == 1))
                    for h in range(4):
                        pb = 2 + h // 2
                        col = slice((h % 2) * 256, (h % 2 + 1) * 256)
                        tk.op("dve", lambda e: e.scalar_tensor_tensor(S[:, h, :], S[:, h, :], Eq[:, h * 128 + 127:h * 128 + 128], ps[pb][:, col],
                                                                      ALU.mult, ALU.add),
                              reads=["S", ("Eq", s), ("ps", pb)], writes=["S"])
                    tk.op("act", lambda e: e.activation(out=Sbf[:], in_=S[:], func=AF.Copy), reads=["S"], writes=["Sbf"])
                tk.op("act", lambda e: e.activation(out=sq[:], in_=oT[:], func=AF.Square), reads=["oT"], writes=["sq"])
                for h in range(4):
                    pb = 6 + h // 2
                    col = slice((h % 2) * TG, (h % 2 + 1) * TG)
                    for jj in range(2):
                        tk.op("pe", lambda e: e.matmul(ps[pb][:, col], ones, sq[:, 2 * h + jj, :], start=(jj == 0), stop=(jj == 1)),
                              reads=["sq", "cbf"], writes=[("ps", pb)], signal=(jj == 1 and h % 2 == 1))
                for hb in range(2):
                    tk.op("act", lambda e: e.activation(out=rso[:, 2 * hb:2 * hb + 2, :],
                                                        in_=ps[6 + hb][:, :2 * TG].rearrange("p (h t) -> p h t", h=2),
                                                        func=AF.Sqrt, bias=EPS, scale=1.0 / 256),
                          reads=[("ps", 6 + hb)], writes=["rso"])
                tk.op("dve", lambda e: e.reciprocal(rso[:], rso[:]), reads=["rso"], writes=["rso"])
                for c in range(8):
                    h, jj = c // 2, c % 2
                    tk.op("dve", lambda e: e.scalar_tensor_tensor(tmpo[:], oT[:, c, :], onorm[:, jj:jj + 1], rso[:, h, :], ALU.mult, ALU.mult),
                          reads=["oT", "rso", "spt"], writes=["tmpo"])
                    tk.op("dve", lambda e: e.tensor_tensor(gated[:, c, :], tmpo[:], rT[:, c, :], ALU.mult),
                          reads=["tmpo", ("rT", c), "gh"], writes=["gh"])
                self.out_proj_residual(w_out, gated, "gh", xT, "gx", TG, banks=(0, 1))
                tk.dma("sp", lambda e: e.dma_start(out=self.xsT_v[:, :, t0:t0 + TG], in_=xT[:]),
                       reads=["gx"], writes=["xsT"], ring="gxs", nring=1)
            self.barrier()

    def phase_fox(self, l):
        self.phase_fox_proj(l)
        self.phase_fox_attn(l)
        self.phase_fox_out(l)

    def phase_fox_proj(self, l):
        tk, nc, T = self.tk, self.nc, self.T
        j = l // 2
        TF = 512
        with ExitStack() as st:
            w_in = self.sb(st, "p_win", [128, 8, 4112], BF16)
            xT = self.sb(st, "p_xT", [128, 8, TF], F32)
            xr = self.sb(st, "p_xr", [128, 8, TF], F32)
            sq = self.sb(st, "p_sq", [128, 8, TF], BF16)
            hT = self.sb(st, "p_hT", [128, 8, TF], BF16)
            rstd = self.sb(st, "p_rstd", [128, 512], F32)
            qf = self.sb(st, "p_qf", [128, 4, TF], F32)
            qsq = self.sb(st, "p_qsq", [128, 4, TF], BF16)
            rs = self.sb(st, "p_rs", [128, 4, TF], F32)
            qn = [self.sb(st, f"p_qn{i}", [128, 4, TF], BF16) for i in range(2)]
            vt = self.sb(st, "p_vt", [128, 4, D], BF16)
            sg = self.sb(st, "p_sg", [128, 8, TF], BF16)
            zs = self.sb(st, "p_zs", [128, 16], F32)
            az = self.sb(st, "p_az", [128, 16], F32)
            lf = self.sb(st, "p_lf", [128, 16], F32)
            carry = self.sb(st, "p_carry", [128, 16], F32)
            ps = self.ps
            self.load_w(w_in, self.wt("fox_w_in", j).rearrange("(k p) f -> p k f", p=128), "win", 4112, 1028)
            tk.op("dve", lambda e: e.memset(carry[:], 0.0), writes=["carry"])
            ob, _ = SP["bf"]
            bfB = self.sb(st, "p_bfB", [128, 16], F32)
            tk.op("pe", lambda e: e.matmul(self.ps[6][:, 0:16], self.c32[0:1, C_ONES:C_ONES + 128],
                                           self.spt[0:1, ob + j * 16:ob + (j + 1) * 16], start=True, stop=True),
                  reads=["c32", "spt"], writes=[("ps", 6)])
            tk.op("dve", lambda e: e.tensor_copy(bfB[:], self.ps[6][:, 0:16]), reads=[("ps", 6)], writes=["bfB"])
            bd = self.cbf[:, C_BD:C_BD + 128]
            cm = self.c32[:, C_MASK:C_MASK + 128]
            ones32 = self.c32[:, C_ONES:C_ONES + 128]
            ob, _ = SP["bf"]
            bfr = self.spt[0:1, ob + j * 16:ob + (j + 1) * 16]
            oq, _ = SP["qn"]
            okn, _ = SP["kn"]
            qs_v = self.qs.rearrange("(c p) t -> p c t", p=128)
            ks_v = self.ks.rearrange("(c p) t -> p c t", p=128)
            gs_v = self.gs.rearrange("(c p) t -> p c t", p=128)
            vs_v = self.vs.rearrange("(n p) d -> p n d", p=128)
            gi = 0
            for ti in range(T // TF):
                t0 = ti * TF
                tk.dma("sp", lambda e: e.dma_start(out=xT[:], in_=self.xsT_v[:, :, t0:t0 + TF]),
                       reads=["xsT"], writes=["px"], ring="pxl", nring=1)
                self.norm_h(0, xT[:], "px", TF, sq, xr, rstd, [hT[:, c, :] for c in range(8)], ["ph"] * 8, 7)
                for grp in range(4):
                    isk = grp >= 2
                    base = (1024 if isk else 0) + (grp % 2) * 512
                    gain = self.spt[:, (okn if isk else oq) + j:(okn if isk else oq) + j + 1]
                    for c in range(4):
                        for k in range(8):
                            tk.op("pe", lambda e: e.matmul(ps[c][:], w_in[:, k, base + c * 128:base + (c + 1) * 128], hT[:, k, :],
                                                           start=(k == 0), stop=(k == 7)),
                                  reads=["win", "ph"], writes=[("ps", c)], signal=(k == 7))
                        tk.op("act", lambda e: e.activation(out=qf[:, c, :], in_=ps[c][:], func=AF.Copy),
                              reads=[("ps", c)], writes=["qf"])
                    tk.op("dve", lambda e: e.tensor_tensor(qsq[:], qf[:], qf[:], ALU.mult), reads=["qf"], writes=["qsq"])
                    for c in range(4):
                        tk.op("pe", lambda e: e.matmul(ps[4 + c][:], bd, qsq[:, c, :], start=True, stop=True),
                              reads=["qsq", "cbf"], writes=[("ps", 4 + c)])
                        tk.op("act", lambda e: e.activation(out=rs[:, c, :], in_=ps[4 + c][:], func=AF.Ln, bias=EPS, scale=1.0 / 64),
                              reads=[("ps", 4 + c)], writes=["rs"])
                    tk.op("act", lambda e: e.activation(out=rs[:], in_=rs[:], func=AF.Exp, scale=-0.5), reads=["rs"], writes=["rs"])
                    qb_ = qn[gi % 2]
                    qkey = ("qn", gi % 2)
                    gi += 1
                    tk.op("dve", lambda e: e.scalar_tensor_tensor(qb_[:], qf[:], gain, rs[:], ALU.mult, ALU.mult),
                          reads=["qf", "rs", "spt"], writes=[qkey])
                    dst = (ks_v if isk else qs_v)[:, (grp % 2) * 4:(grp % 2) * 4 + 4, t0:t0 + TF]
                    tk.dma("sp", lambda e: e.dma_start(out=dst, in_=qb_[:]), reads=[qkey], writes=["ks" if isk else "qs"],
                           ring="pqs", nring=2)
                for oc in range(8):
                    pb = oc % 4
                    for k in range(8):
                        tk.op("pe", lambda e: e.matmul(ps[pb][:], w_in[:, k, 3072 + oc * 128:3072 + (oc + 1) * 128], hT[:, k, :],
                                                       start=(k == 0), stop=(k == 7)),
                              reads=["win", "ph"], writes=[("ps", pb)], signal=(k == 7))
                    tk.op("act", lambda e: e.activation(out=sg[:, oc, :], in_=ps[pb][:], func=AF.Sigmoid),
                          reads=[("ps", pb)], writes=["sg"])
                tk.dma("sp", lambda e: e.dma_start(out=gs_v[:, :, t0:t0 + TF], in_=sg[:]), reads=["sg"], writes=["gs"],
                       ring="pgs", nring=1)
                for s in range(4):
                    tok = slice(s * 128, (s + 1) * 128)
                    for half in range(2):
                        pb = 4 + half
                        for k in range(8):
                            tk.op("pe", lambda e: e.matmul(ps[pb][:], hT[:, k, tok], w_in[:, k, 2048 + half * 512:2560 + half * 512],
                                                           start=(k == 0), stop=(k == 7)),
                                  reads=["win", "ph"], writes=[("ps", pb)], signal=(k == 7))
                        if half == 0:
                            tk.op("act", lambda e: e.activation(out=vt[:, s, 0:512], in_=ps[pb][:], func=AF.Copy),
                                  reads=[("ps", pb)], writes=["vt"])
                        else:
                            tk.op("dve", lambda e: e.tensor_copy(vt[:, s, 512:1024], ps[pb][:]), reads=[("ps", pb)], writes=["vt"])
                    for k in range(8):
                        tk.op("pe", lambda e: e.matmul(ps[6][:, 0:16], hT[:, k, tok], w_in[:, k, 4096:4112], start=(k == 0), stop=(k == 7)),
                              reads=["win", "ph"], writes=[("ps", 6)], signal=(k == 7))
                    tk.op("dve", lambda e: e.tensor_tensor(zs[:], ps[6][:, 0:16], bfB[:], ALU.add), reads=[("ps", 6), "bfB"], writes=["pzs"])
                    tk.op("dve", lambda e: e.scalar_tensor_tensor(az[:], zs[:], -1.0, zs[:], ALU.mult, ALU.min), reads=["pzs"], writes=["paz"])
                    tk.op("act", lambda e: e.activation(out=az[:], in_=az[:], func=AF.Exp), reads=["paz"], writes=["paz"])
                    tk.op("act", lambda e: e.activation(out=az[:], in_=az[:], func=AF.Ln, bias=1.0, scale=1.0), reads=["paz"], writes=["paz"])
                    tk.op("dve", lambda e: e.scalar_tensor_tensor(lf[:], zs[:], 0.0, az[:], ALU.min, ALU.subtract),
                          reads=["pzs", "paz"], writes=["lf"])
                    gt = ti * 4 + s
                    tk.op("pe", lambda e: e.matmul(ps[6][:, 16:32], cm, lf[:], start=True, stop=True),
                          reads=["lf", "c32"], writes=[("ps", 6)], signal=False)
                    tk.op("pe", lambda e: e.matmul(ps[6][:, 32:48], ones32, lf[:], start=True, stop=True),
                          reads=["lf", "c32"], writes=[("ps", 6)])
                    tk.op("dve", lambda e: e.tensor_tensor(self.Fk[:, gt, :], ps[6][:, 16:32], carry[:], ALU.add),
                          reads=[("ps", 6), "carry"], writes=["Fk"])
                    tk.op("dve", lambda e: e.tensor_tensor(carry[:], ps[6][:, 32:48], carry[:], ALU.add),
                          reads=[("ps", 6), "carry"], writes=["carry"])
                    if s == 1:
                        tk.op("dve", lambda e: e.tensor_copy(self.Fref[:, ti, :], carry[:]), reads=["carry"], writes=["Fref"])
                tk.dma("sp", lambda e: e.dma_start(out=vs_v[:, ti * 4:ti * 4 + 4, :], in_=vt[:]), reads=["vt"], writes=["vs"],
                       ring="pvs", nring=1)
            self.barrier()

    def phase_fox_attn(self, l):
        tk, nc, T = self.tk, self.nc, self.T
        NQB, NKT = T // 512, T // 128
        with ExitStack() as st:
            QhT = [self.sb(st, f"a_q{i}", [128, T], BF16) for i in range(2)]
            KhT = [self.sb(st, f"a_k{i}", [128, T], BF16) for i in range(2)]
            Vh = [self.sb(st, f"a_v{i}", [128, NKT, 128], BF16) for i in range(2)]
            Gh = [self.sb(st, f"a_g{i}", [64, T], BF16) for i in range(2)]
            bias = self.sb(st, "a_bias", [128, NQB, NKT, 16], F32)
            Pt = [self.sb(st, f"a_P{i}", [128, 512], BF16) for i in range(4)]
            rl = self.sb(st, "a_rl", [65, 512], F32)
            bcs = self.sb(st, "a_bcs", [64, 512], F32)
            t1 = self.sb(st, "a_t1", [64, 512], F32)
            og = [self.sb(st, f"a_og{i}", [64, 512], BF16) for i in range(2)]
            ps = self.ps
            cmb = self.cbf[:, C_MASK:C_MASK + 128]
            vs_v = self.vs.rearrange("(n p) d -> p n d", p=128)
            for i in range(2):
                tk.op("dve", lambda e: e.memset(Vh[i][:], 1.0), writes=[("Vh", i)])
                tk.op("dve", lambda e: e.memset(QhT[i][64:128, :], 0.0), writes=[("Qh", i)])
                tk.op("dve", lambda e: e.memset(KhT[i][64:128, :], 0.0), writes=[("Kh", i)])
            for qb in range(NQB):
                nk = 4 * qb + 4
                tk.op("dve", lambda e: e.tensor_tensor(bias[:, qb, 0:nk, :],
                                                       self.Fref[:, qb, :].unsqueeze(1).broadcast_to([128, nk, 16]),
                                                       self.Fk[:, 0:nk, :], ALU.subtract),
                      reads=["Fref", "Fk"], writes=["bias"])
            L = 3
            items = [(h, qb, kt) for h in range(16) for qb in range(NQB) for kt in range(4 * qb + 4)]
            head_start = {}
            for i, (h, qb, kt) in enumerate(items):
                head_start.setdefault(h, i)

            def loads(h):
                b = h % 2
                hs = slice(h * 64, (h + 1) * 64)
                q = "pool"
                tk.dma(q, lambda e: e.dma_start(out=QhT[b][0:64, :], in_=self.qs[hs, :]), reads=["qs"], writes=[("Qh", b)], ring="aq", nring=2)
                tk.dma(q, lambda e: e.dma_start(out=KhT[b][0:64, :], in_=self.ks[hs, :]), reads=["ks"], writes=[("Kh", b)], ring="ak", nring=2)
                for n0 in range(0, NKT, 8):
                    n1 = min(NKT, n0 + 8)
                    tk.dma(q, lambda e: e.dma_start(out=Vh[b][:, n0:n1, 0:64], in_=vs_v[:, n0:n1, hs]), reads=["vs"], writes=[("Vh", b)],
                           ring="av", nring=8)
                tk.dma(q, lambda e: e.dma_start(out=Gh[b][:], in_=self.gs[hs, :]), reads=["gs"], writes=[("Gh", b)], ring="ag", nring=2)

            later = []

            def tick():
                for ent in later:
                    ent[0] -= 1
                while later and later[0][0] <= 0:
                    later.pop(0)[1]()

            def emit_pv(i):
                h, qb, kt = items[i]
                b = h % 2
                nk = 4 * qb + 4
                blk = h * NQB + qb
                po = 4 + blk % 2
                pb = i % 4
                P = Pt[pb]
                c0 = max(0, kt - 4 * qb) * 128
                tk.op("pe", lambda e: e.matmul(ps[po][:, c0:512], Vh[b][:, kt, :], P[:, c0:512],
                                               start=(kt == 0), stop=(kt == nk - 1)),
                      reads=[("Vh", b), ("P", pb)], writes=[("ps", po)], signal=(kt == nk - 1))
                if kt != nk - 1:
                    return
                hs = slice(h * 64, (h + 1) * 64)
                ob_ = og[blk % 2]
                okey = ("og", blk % 2)
                tk.op("dve", lambda e: e.reciprocal(rl[64:65, :], ps[po][64:65, :]), reads=[("ps", po)], writes=["rl"])

                def epi():
                    tk.op("pe", lambda e: e.matmul(ps[6][0:64, :], self.c32[64:65, C_ONES:C_ONES + 64], rl[64:65, :], start=True, stop=True),
                          reads=["rl", "c32"], writes=[("ps", 6)])
                    tk.op("act", lambda e: e.activation(out=bcs[:], in_=ps[6][0:64, :], func=AF.Copy), reads=[("ps", 6)], writes=["bcs"])
                    tk.op("dve", lambda e: e.tensor_tensor(t1[:], ps[po][0:64, :], bcs[:], ALU.mult),
                          reads=[("ps", po), "bcs"], writes=["t1"])
                    tk.op("dve", lambda e: e.tensor_tensor(ob_[:], t1[:], Gh[b][:, qb * 512:(qb + 1) * 512], ALU.mult),
                          reads=["t1", ("Gh", b)], writes=[okey])
                    tk.dma("sp", lambda e: e.dma_start(out=self.os_[hs, qb * 512:(qb + 1) * 512], in_=ob_[:]),
                           reads=[okey], writes=["os"], ring="aos", nring=2)
                later.append([2, epi])

            loads(0)
            pend = []
            for i, (h, qb, kt) in enumerate(items):
                b = h % 2
                c0 = max(0, kt - 4 * qb) * 128
                pb = i % 4
                P = Pt[pb]
                tk.op("pe", lambda e: e.matmul(ps[pb][:, c0:512], KhT[b][:, kt * 128:(kt + 1) * 128],
                                               QhT[b][:, qb * 512 + c0:(qb + 1) * 512], start=True, stop=True),
                      reads=[("Kh", b), ("Qh", b)], writes=[("ps", pb)])
                tk.op("act", lambda e: e.activation(out=P[:, c0:512], in_=ps[pb][:, c0:512], func=AF.Exp,
                                                    bias=bias[:, qb, kt, h:h + 1], scale=0.125),
                      reads=[("ps", pb), "bias"], writes=[("P", pb)])
                if kt - 4 * qb >= 0:
                    tk.op("dve", lambda e: e.tensor_tensor(P[:, c0:c0 + 128], P[:, c0:c0 + 128], cmb, ALU.mult),
                          reads=[("P", pb), "cbf"], writes=[("P", pb)])
                tick()
                pend.append(i)
                if len(pend) > L:
                    emit_pv(pend.pop(0))
                if i == head_start[h] + L + 2 and h + 1 < 16:
                    loads(h + 1)
            while pend:
                emit_pv(pend.pop(0))
                tick()
            while later:
                later.pop(0)[1]()
            self.barrier()

    def phase_fox_out(self, l):
        tk, nc, T = self.tk, self.nc, self.T
        j = l // 2
        TF = 512
        with ExitStack() as st:
            w_out = self.sb(st, "o_wout", [128, 8, D], BF16)
            xT = [self.sb(st, f"o_xT{i}", [128, 8, TF], F32) for i in range(2)]
            gT = [self.sb(st, f"o_gT{i}", [128, 8, TF], BF16) for i in range(2)]
            self.load_w(w_out, self.wt("fox_w_out", j).rearrange("(k p) f -> p k f", p=128), "wo", D, 512)
            os_v = self.os_.rearrange("(c p) t -> p c t", p=128)
            for ti in range(T // TF):
                t0 = ti * TF
                b = ti % 2
                tk.dma("sp", lambda e: e.dma_start(out=xT[b][:], in_=self.xsT_v[:, :, t0:t0 + TF]),
                       reads=["xsT"], writes=[("ox", b)], ring="oxl", nring=2)
                tk.dma("sp", lambda e: e.dma_start(out=gT[b][:], in_=os_v[:, :, t0:t0 + TF]),
                       reads=["os"], writes=[("og", b)], ring="ogl", nring=2)
                self.out_proj_residual(w_out, gT[b], ("og", b), xT[b], ("ox", b), TF, banks=(0, 1, 2, 3))
                tk.dma("sp", lambda e: e.dma_start(out=self.xsT_v[:, :, t0:t0 + TF], in_=xT[b][:]),
                       reads=[("ox", b)], writes=["xsT"], ring="oxs", nring=2)
            self.barrier()

    def build_all(self, layers=(0, 1, 2, 3)):
        with ExitStack() as st:
            self.setup(st)
            self.phase_in()
            for l in layers:
                self.phase_mod(l)
                if l % 2 == 0:
                    self.phase_gla(l)
                    self.phase_ffn(l, moe=False)
                else:
                    self.phase_fox(l)
                    self.phase_ffn(l, moe=True, dyn=(self.split_last and l == layers[-1]))
            self.phase_out()
            self.tk.drain("sp")
        return self.nc


from concourse.bass_utils import run_bass_kernel_spmd

SEQ = 4096
BATCH = 4
N_CORES = 8
_PROG = {}


def _program():
    if "k" not in _PROG:
        k = K(SEQ, split_last=True)
        k.build_all((0, 1, 2, 3))
        _PROG["k"] = k
    return _PROG["k"]


def kernel(**inputs):
    inp = {n: np.asarray(v) for n, v in inputs.items()}
    k = _program()
    consts = make_consts()
    G = k.G
    in_maps = []
    for c in range(N_CORES):
        b, half = c % BATCH, c // BATCH
        if half == 0:
            d = {"x": np.ascontiguousarray(inp["x"][b], dtype=np.float32), "consts": consts, "sp": pack_small(inp, b)}
            for n in k.w:
                base, j = n.rsplit("_", 1)
                d[n] = np.ascontiguousarray(inp[base][int(j)], dtype=np.float32)
        else:
            d = dict(in_maps[b])
        d["sel"] = np.array([[half, half * G, 0, 0]], np.int32)
        in_maps.append(d)
    res = run_bass_kernel_spmd(k.nc, in_maps, core_ids=list(range(N_CORES)))
    out = np.stack([np.concatenate([np.asarray(res.results[b]["out"], dtype=np.float32)[:G],
                                    np.asarray(res.results[b + BATCH]["out"], dtype=np.float32)[G:]], 0)
                    for b in range(BATCH)], 0)
    return out
```

```python
from contextlib import ExitStack


class Tok:
    __slots__ = ("sem", "count", "eng")

    def __init__(self, sem=None, count=None, eng=None):
        self.sem = sem
        self.count = count
        self.eng = eng


class Tracker:
    ROLL = 30000

    def __init__(self, nc, stack: ExitStack):
        self.nc = nc
        self.stack = stack
        self.engs = {"pe": nc.tensor, "act": nc.scalar, "dve": nc.vector,
                     "pool": nc.gpsimd, "sp": nc.sync}
        self.sem = {}
        self.cnt = {}
        self.nsem = 0
        for e in self.engs:
            self._new_sem(e)
        self.waited = {e: {} for e in self.engs}
        self.last_w = {}
        self.readers = {}
        self.pending = {e: [] for e in self.engs}
        self.rings = {}
        self.n_ins = 0

    def _alloc(self, name):
        self.nsem += 1
        return self.stack.enter_context(self.nc.semaphore(f"{name}_{self.nsem}"))

    def _new_sem(self, e):
        self.sem[e] = self._alloc(f"e_{e}")
        self.cnt[e] = 0

    def _wait(self, eng, tok):
        if tok is None:
            return
        if tok.eng == eng and eng == "pe":
            return
        assert tok.count is not None, "dependency on an unsignaled instruction"
        w = self.waited[eng]
        k = id(tok.sem)
        if w.get(k, 0) >= tok.count:
            return
        self.engs[eng].wait_ge(tok.sem, tok.count)
        w[k] = tok.count

    def _deps(self, reads, writes):
        deps = []
        for b in reads:
            t = self.last_w.get(b)
            if t is not None:
                deps.append(t)
        for b in writes:
            t = self.last_w.get(b)
            if t is not None:
                deps.append(t)
            deps.extend(self.readers.get(b, ()))
        return deps

    def _record(self, tok, reads, writes):
        for b in reads:
            self.readers.setdefault(b, []).append(tok)
        for b in writes:
            self.last_w[b] = tok
            self.readers[b] = []

    def op(self, eng, fn, reads=(), writes=(), signal=True):
        for t in self._deps(reads, writes):
            self._wait(eng, t)
        ins = fn(self.engs[eng])
        self.n_ins += 1
        if signal:
            if self.cnt[eng] >= self.ROLL:
                self._new_sem(eng)
            self.cnt[eng] += 1
            ins.then_inc(self.sem[eng], 1)
            tok = Tok(self.sem[eng], self.cnt[eng], eng)
            for p in self.pending[eng]:
                p.sem, p.count = tok.sem, tok.count
            self.pending[eng] = []
        else:
            tok = Tok(None, None, eng)
            self.pending[eng].append(tok)
        self._record(tok, reads, writes)
        return tok

    def dma(self, eng, fn, reads=(), writes=(), ring="d", nring=4):
        r = self.rings.get(ring)
        if r is None:
            r = self.rings[ring] = {"sems": [self._alloc(f"r_{ring}") for _ in range(nring)],
                                    "cnt": [0] * nring, "i": 0}
        i = r["i"]
        r["i"] = (i + 1) % len(r["sems"])
        sem = r["sems"][i]
        if r["cnt"][i] > 0:
            self._wait(eng, Tok(sem, r["cnt"][i], "dma"))
        for t in self._deps(reads, writes):
            self._wait(eng, t)
        ins = fn(self.engs[eng])
        self.n_ins += 1
        r["cnt"][i] += 16
        ins.then_inc(sem, 16)
        tok = Tok(sem, r["cnt"][i], "dma")
        self._record(tok, reads, writes)
        return tok

    def drain(self, eng="sp"):
        for r in self.rings.values():
            for sem, c in zip(r["sems"], r["cnt"]):
                if c:
                    self._wait(eng, Tok(sem, c, "dma"))
        for e in self.engs:
            assert not self.pending[e], f"unsignaled tail on {e}"
            if self.cnt[e]:
                self._wait(eng, Tok(self.sem[e], self.cnt[e], e))


import numpy as np
from contextlib import ExitStack
import concourse.bass as bass
import concourse.mybir as mybir

F32 = mybir.dt.float32
BF16 = mybir.dt.bfloat16
AF = mybir.ActivationFunctionType
ALU = mybir.AluOpType
AX = mybir.AxisListType

D = 1024
KC = 8
DFF = 3584
NE = 8
EPS = 1e-6

C_ID, C_MASK, C_TRI, C_REV, C_BD, C_ONES, C_SEL = 0, 128, 256, 386, 514, 642, 770
NCONST = 898


def make_consts():
    c = np.zeros((128, NCONST), np.float32)
    s = np.arange(128)[:, None]
    t = np.arange(128)[None, :]
    c[:, C_ID:C_ID + 128] = (s == t)
    c[:, C_MASK:C_MASK + 128] = (s <= t)
    c[:, C_TRI:C_TRI + 128] = (s <= t) / 16.0
    c[:, C_TRI + 128] = 1.0 / 16.0
    c[:, C_REV:C_REV + 128] = (s > t) / 16.0
    c[:, C_BD:C_BD + 128] = (s // 64 == t // 64)
    c[:, C_ONES:C_ONES + 128] = 1.0
    c[127, C_SEL:C_SEL + 128] = 1.0
    return c


SP = {}
_o = 0
for _n, _w in [("cT", 8), ("adab", 4 * 48), ("ngain", 4 * 2 * 8), ("wgu", 2 * 512), ("onorm", 2 * 2),
               ("bf", 2 * 16), ("qn", 2), ("kn", 2), ("wr", 2 * 8 * 8), ("br", 2 * 8)]:
    SP[_n] = (_o, _w)
    _o += _w
NSP = _o


def pack_small(inp, b):
    sp = np.zeros((128, NSP), np.float32)

    def put(name, arr):
        o, w = SP[name]
        arr = np.asarray(arr, np.float32).reshape(arr.shape[0], -1)
        assert arr.shape[1] == w, (name, arr.shape, w)
        sp[:arr.shape[0], o:o + w] = arr

    fm = lambda v, n: np.asarray(v, np.float32).reshape(n, 128).T
    put("cT", fm(inp["c"][b], 8))
    put("adab", np.stack([fm(inp["ada_b"][l], 48) for l in range(4)], 1))
    put("ngain", np.stack([np.stack([fm(inp["norm_gain"][l, s], 8) for s in range(2)], 1) for l in range(4)], 1))
    wgu = np.zeros((32, 2, 512), np.float32)
    for j in range(2):
        wgu[:16, j] = inp["gla_w_gate_up"][j]
        wgu[16, j] = inp["gla_b_gate"][j]
    put("wgu", wgu)
    put("onorm", np.stack([fm(inp["gla_o_norm"][j], 2) for j in range(2)], 1))
    put("bf", np.asarray(inp["fox_b_f"], np.float32).reshape(1, 32))
    put("qn", np.stack([np.tile(inp["fox_q_norm"][j], 2) for j in range(2)], 1))
    put("kn", np.stack([np.tile(inp["fox_k_norm"][j], 2) for j in range(2)], 1))
    put("wr", np.stack([np.asarray(inp["moe_w_router"][j]).reshape(8, 128, 8).transpose(1, 0, 2) for j in range(2)], 1))
    put("br", np.asarray(inp["moe_b_router"], np.float32).reshape(1, 16))
    return sp


class K:
    def __init__(self, T, dbg=(), G=None, split_last=False):
        self.T = T
        self.G = G or min(2048, T)
        self.split_last = split_last
        self.dbg = set(dbg)
        nc = self.nc = bass.Bass("TRN2", target_bir_lowering=False)
        dt = nc.dram_tensor
        self.x = dt("x", [T, D], F32, kind="ExternalInput").ap()
        self.consts_d = dt("consts", [128, NCONST], F32, kind="ExternalInput").ap()
        self.sp_d = dt("sp", [128, NSP], F32, kind="ExternalInput").ap()
        if split_last:
            self.sel_d = dt("sel", [1, 4], mybir.dt.int32, kind="ExternalInput").ap()
        self.w = {}
        self.wshape = {"ada_w": [D, 6 * D], "gla_w_in": [D, 3088], "gla_w_out": [D, D],
                       "fox_w_in": [D, 4112], "fox_w_out": [D, D],
                       "ffn_w_gate": [D, DFF], "ffn_w_up": [D, DFF], "ffn_w_down": [DFF, D],
                       "moe_w_gate": [NE, D, DFF], "moe_w_up": [NE, D, DFF], "moe_w_down": [NE, DFF, D]}
        self.out = dt("out", [T, D], F32, kind="ExternalOutput").ap()
        self.xsT = dt("xsT", [D, T], F32).ap()
        self.xsT_v = self.xsT.rearrange("(c p) t -> p c t", p=128)
        self.qs = dt("qs", [D, T], BF16).ap()
        self.ks = dt("ks", [D, T], BF16).ap()
        self.vs = dt("vs", [T, D], BF16).ap()
        self.gs = dt("gs", [D, T], BF16).ap()
        self.os_ = dt("os", [D, T], BF16).ap()
        self.dbg_out = {}

    def wt(self, name, j):
        key = f"{name}_{j}"
        if key not in self.w:
            self.w[key] = self.nc.dram_tensor(key, self.wshape[name], F32, kind="ExternalInput").ap()
        return self.w[key]

    def sb(self, st, name, shape, dtype):
        self.uid = getattr(self, "uid", 0) + 1
        return st.enter_context(self.nc.sbuf_tensor(f"{name}_u{self.uid}", shape, dtype))

    def setup(self, st):
        nc = self.nc
        self.tk = tk = Tracker(nc, st)
        self.c32 = self.sb(st, "c32", [128, NCONST], F32)
        self.spt = self.sb(st, "spt", [128, NSP], F32)
        self.cbf = self.sb(st, "cbf", [128, NCONST], BF16)
        self.modL = self.sb(st, "mod", [128, 4, 48], F32)
        self.AL = self.sb(st, "modA", [128, 4, 2, 8], F32)
        self.cur = 0
        self.ps = [st.enter_context(nc.psum_tensor(f"ps{i}", [128, 512], F32)) for i in range(8)]
        self.Fk = self.sb(st, "Fk", [128, self.T // 128, 16], F32)
        self.Fref = self.sb(st, "Fref", [128, max(1, self.T // 512), 16], F32)
        self.bfb = self.sb(st, "bfb", [1, 32], BF16)
        tk.dma("sp", lambda e: e.dma_start(out=self.c32[:], in_=self.consts_d), writes=["c32"], ring="c")
        tk.dma("sp", lambda e: e.dma_start(out=self.spt[:], in_=self.sp_d), writes=["spt"], ring="c")
        tk.op("dve", lambda e: e.tensor_copy(self.cbf[:], self.c32[:]), reads=["c32"], writes=["cbf"])
        ob, _ = SP["bf"]
        tk.op("dve", lambda e: e.tensor_copy(self.bfb[:], self.spt[0:1, ob:ob + 32]), reads=["spt"], writes=["bfb"])
        if self.split_last:
            self.sel_sb = self.sb(st, "sel_sb", [1, 4], mybir.dt.int32)
            self.r_col = st.enter_context(nc.sync.register("r_col"))
            sem = tk._alloc("selsem")
            nc.sync.dma_start(out=self.sel_sb[:], in_=self.sel_d).then_inc(sem, 16)
            nc.sync.wait_ge(sem, 16)
            nc.sync.reg_load(self.r_col, self.sel_sb[0:1, 1:2])

    def spv(self, name):
        o, w = SP[name]
        return self.spt[:, o:o + w]

    def barrier(self):
        tk = self.tk
        tk.drain("sp")
        self.nc.all_engine_barrier()
        tk.last_w.clear()
        tk.readers.clear()

    def phase_in(self, barrier=True, body=None):
        tk, nc = self.tk, self.nc
        with ExitStack() as st:
            if body is not None:
                body(st)
            xin = [self.sb(st, f"xin{i}", [128, D], F32) for i in range(2)]
            xT = [self.sb(st, f"xTi{i}", [128, 8, 128], F32) for i in range(2)]
            ident = self.c32[:, C_ID:C_ID + 128]
            for tt in range(self.T // 128):
                b = tt % 2
                tk.dma("sp", lambda e: e.dma_start(out=xin[b][:], in_=self.x[tt * 128:(tt + 1) * 128, :]),
                       writes=[("xin", b)], ring="xin", nring=2)
                for half in range(2):
                    pb = (tt * 2 + half) % 6
                    for j in range(4):
                        c = half * 4 + j
                        tk.op("pe", lambda e: e.transpose(self.ps[pb][:, j * 128:(j + 1) * 128],
                                                          xin[b][:, c * 128:(c + 1) * 128], ident),
                              reads=[("xin", b), "c32"], writes=[("ps", pb)], signal=(j == 3))
                    src = self.ps[pb][:].rearrange("p (c t) -> p c t", c=4)
                    dst = xT[b][:, half * 4:(half + 1) * 4, :]
                    if half == 0:
                        tk.op("dve", lambda e: e.tensor_copy(dst, src), reads=[("ps", pb)], writes=[("xTi", b, half)])
                    else:
                        tk.op("act", lambda e: e.activation(out=dst, in_=src, func=AF.Copy),
                              reads=[("ps", pb)], writes=[("xTi", b, half)])
                tk.dma("sp", lambda e: e.dma_start(out=self.xsT_v[:, :, tt * 128:(tt + 1) * 128], in_=xT[b][:]),
                       reads=[("xTi", b, 0), ("xTi", b, 1)], writes=["xsT"], ring="xst", nring=2)
            if barrier:
                self.barrier()

    def phase_out(self):
        tk, nc = self.tk, self.nc
        with ExitStack() as st:
            xin = [self.sb(st, f"xo{i}", [128, D], F32) for i in range(2)]
            xT = [self.sb(st, f"xTo{i}", [128, 8, 128], F32) for i in range(2)]
            ident = self.c32[:, C_ID:C_ID + 128]
            for tt in range(self.T // 128):
                b = tt % 2
                tk.dma("sp", lambda e: e.dma_start(out=xT[b][:], in_=self.xsT_v[:, :, tt * 128:(tt + 1) * 128]),
                       reads=["xsT"], writes=[("xTo", b)], ring="xld", nring=2)
                for half in range(2):
                    pb = (tt * 2 + half) % 8
                    for j in range(4):
                        c = half * 4 + j
                        tk.op("pe", lambda e: e.transpose(self.ps[pb][:, j * 128:(j + 1) * 128], xT[b][:, c, :], ident),
                              reads=[("xTo", b), "c32"], writes=[("ps", pb)], signal=(j == 3))
                    dst = xin[b][:, half * 512:(half + 1) * 512]
                    if half == 0:
                        tk.op("dve", lambda e: e.tensor_copy(dst, self.ps[pb][:]), reads=[("ps", pb)], writes=[("xo", b, half)])
                    else:
                        tk.op("act", lambda e: e.activation(out=dst, in_=self.ps[pb][:], func=AF.Copy),
                              reads=[("ps", pb)], writes=[("xo", b, half)])
                tk.dma("sp", lambda e: e.dma_start(out=self.out[tt * 128:(tt + 1) * 128, :], in_=xin[b][:]),
                       reads=[("xo", b, 0), ("xo", b, 1)], writes=[("out", tt)], ring="ost", nring=2)
            self.barrier()

    def phase_mod(self, l):
        tk, nc = self.tk, self.nc
        self.cur = l
        if True:
            cond, wa = self.mod_bufs
            wv = self.wt("ada_w", l).rearrange("(k p) j -> p k j", p=128)
            pm = self.ps[7]
            for q in range(4):
                b = q % 2
                tk.dma("pool", lambda e: e.dma_start(out=wa[b][:], in_=wv[:, :, q * 1536:(q + 1) * 1536]),
                       writes=[("wa", b)], ring="wa", nring=2)
                for jc in range(12):
                    col = q * 12 + jc
                    for k in range(8):
                        tk.op("pe", lambda e: e.matmul(pm[:, col:col + 1], wa[b][:, k, jc * 128:(jc + 1) * 128],
                                                       cond[:, k:k + 1], start=(k == 0), stop=(k == 7)),
                              reads=[("wa", b), "cond"], writes=[("ps", 7)], signal=(k == 7 and jc == 11))
            o, _ = SP["adab"]
            tk.op("dve", lambda e: e.tensor_tensor(self.mod[:], pm[:, 0:48], self.spt[:, o + l * 48:o + (l + 1) * 48], ALU.add),
                  reads=[("ps", 7), "spt"], writes=["mod"])
            og, _ = SP["ngain"]
            for s in range(2):
                sc = self.mod[:, 8:16] if s == 0 else self.mod[:, 32:40]
                gain = self.spt[:, og + (l * 2 + s) * 8:og + (l * 2 + s + 1) * 8]
                tk.op("dve", lambda e: e.scalar_tensor_tensor(self.A[:, s, :], sc, 1.0, gain, ALU.add, ALU.mult),
                      reads=["mod", "spt"], writes=["modA"])

    @property
    def mod(self):
        return self.modL[:, self.cur, :]

    @property
    def A(self):
        return self.AL[:, self.cur, :, :]

    def shift(self, s):
        return self.mod[:, 0:8] if s == 0 else self.mod[:, 24:32]

    def gate(self, s):
        return self.mod[:, 16:24] if s == 0 else self.mod[:, 40:48]

    def norm_h(self, s, xT, xkey, n, sq, xr, rstd, outs, okeys, pbank, h32=None):
        tk = self.tk
        ones = self.cbf[:, C_ONES:C_ONES + 128]
        tk.op("act", lambda e: e.activation(out=sq[:, :, :n], in_=xT, func=AF.Square),
              reads=[xkey], writes=["sq"])
        for c in range(8):
            tk.op("pe", lambda e: e.matmul(self.ps[pbank][:, :n], ones, sq[:, c, :n], start=(c == 0), stop=(c == 7)),
                  reads=["sq", "cbf"], writes=[("ps", pbank)], signal=(c == 7))
        tk.op("act", lambda e: e.activation(out=rstd[:, :n], in_=self.ps[pbank][:, :n], func=AF.Sqrt, bias=EPS, scale=1.0 / D),
              reads=[("ps", pbank)], writes=["rstd"])
        tk.op("dve", lambda e: e.reciprocal(rstd[:, :n], rstd[:, :n]), reads=["rstd"], writes=["rstd"])
        rb = rstd[:, :n].unsqueeze(1).broadcast_to([128, 8, n])
        tk.op("dve", lambda e: e.tensor_tensor(xr[:, :, :n], xT, rb, ALU.mult),
              reads=[xkey, "rstd"], writes=["xr"])
        sh = self.shift(s)
        for c in range(8):
            dst = outs[c] if h32 is None else h32[:, c, :n]
            tk.op("act", lambda e: e.activation(out=dst, in_=xr[:, c, :n], func=AF.Identity,
                                                bias=sh[:, c:c + 1], scale=self.A[:, s, c:c + 1]),
                  reads=["xr", "mod", "modA"], writes=[okeys[c]] if h32 is None else [("h32", c)])
            if h32 is not None:
                tk.op("dve", lambda e: e.tensor_copy(outs[c], h32[:, c, :n]), reads=[("h32", c)], writes=[okeys[c]])

    def phase_ffn(self, l, moe, dyn=False):
        tk, nc, G = self.tk, self.nc, self.G
        j = l // 2
        NT = G // 512
        with ExitStack() as st:
            xT = self.sb(st, "f_xT", [128, 8, G], F32)
            hT = self.sb(st, "f_hT", [128, 8, G], BF16)
            wg = [self.sb(st, f"f_wg{i}", [128, 8, 512], BF16) for i in range(2)]
            wu = [self.sb(st, f"f_wu{i}", [128, 8, 512], BF16) for i in range(2)]
            wd = [self.sb(st, f"f_wd{i}", [128, 4, D], BF16) for i in range(2)]
            if moe:
                comb = self.sb(st, "f_comb", [128, G // 128, 8], F32)
                wr = self.spv("wr").rearrange("p (j k e) -> p j k e", j=2, k=8)
                obr, _ = SP["br"]
            g2 = self.gate(1)
            nexp = NE if moe else 1
            ident = self.c32[:, C_ID:C_ID + 128]
            ones32 = self.c32[:, C_ONES:C_ONES + 128]
            wi = 0
            for grp in range(1 if dyn else self.T // G):
                t0 = grp * G
                if dyn:
                    grp_ap = bass.AP(self.xsT.tensor, self.r_col, [[self.T, 128], [128 * self.T, 8], [1, G]])
                else:
                    grp_ap = self.xsT_v[:, :, t0:t0 + G]
                tk.dma("sp", lambda e: e.dma_start(out=xT[:], in_=grp_ap),
                       reads=["xsT"], writes=[("fx", tt) for tt in range(NT)], ring="fxl", nring=1)
                with ExitStack() as st2:
                    sq = self.sb(st2, "f_sq", [128, 8, 512], BF16)
                    xr = self.sb(st2, "f_xr", [128, 8, 512], F32)
                    rstd = self.sb(st2, "f_rstd", [128, 512], F32)
                    if moe:
                        lg = self.sb(st2, "f_lg", [128, 8], F32)
                        mx = self.sb(st2, "f_mx", [128, 8], F32)
                        tmp8 = self.sb(st2, "f_tmp8", [128, 8], F32)
                        msk = self.sb(st2, "f_msk", [128, 8], F32)
                        nl1 = self.sb(st2, "f_nl1", [128, 1], F32)
                        den = self.sb(st2, "f_den", [128, 1], F32)
                        h32 = xr
                    for tt in range(NT):
                        cs = slice(tt * 512, (tt + 1) * 512)
                        self.norm_h(1, xT[:, :, cs], ("fx", tt), 512, sq, xr, rstd,
                                    [hT[:, c, cs] for c in range(8)], [("fh", tt)] * 8, 7,
                                    h32=(h32 if moe else None))
                        if moe:
                            for s4 in range(4):
                                si = tt * 4 + s4
                                for k in range(8):
                                    tk.op("pe", lambda e: e.matmul(self.ps[6][:, 0:8], h32[:, k, s4 * 128:(s4 + 1) * 128],
                                                                   wr[:, j, k, :], start=(k == 0), stop=False),
                                          reads=[("h32", k), "spt"], writes=[("ps", 6)], signal=False)
                                tk.op("pe", lambda e: e.matmul(self.ps[6][:, 0:8], ones32[0:1, :],
                                                               self.spt[0:1, obr + j * 8:obr + (j + 1) * 8], start=False, stop=True),
                                      reads=["c32", "spt"], writes=[("ps", 6)])
                                tk.op("dve", lambda e: e.tensor_copy(lg[:], self.ps[6][:, 0:8]), reads=[("ps", 6)], writes=["lg"])
                                tk.op("dve", lambda e: e.max(mx[:], lg[:]), reads=["lg"], writes=["mx"])
                                tk.op("dve", lambda e: e.tensor_scalar(msk[:], lg[:], mx[:, 1:2], None, ALU.is_ge),
                                      reads=["lg", "mx"], writes=["msk"])
                                tk.op("dve", lambda e: e.tensor_scalar(nl1[:], mx[:, 0:1], -1.0, None, ALU.mult),
                                      reads=["mx"], writes=["nl1"])
                                tk.op("act", lambda e: e.activation(out=tmp8[:], in_=lg[:], func=AF.Exp, bias=nl1[:, 0:1], scale=1.0),
                                      reads=["lg", "nl1"], writes=["tmp8"])
                                tk.op("dve", lambda e: e.tensor_tensor(tmp8[:], tmp8[:], msk[:], ALU.mult),
                                      reads=["tmp8", "msk"], writes=["tmp8"])
                                tk.op("dve", lambda e: e.reduce_sum(den[:], tmp8[:], AX.X), reads=["tmp8"], writes=["den"])
                                tk.op("dve", lambda e: e.reciprocal(den[:], den[:]), reads=["den"], writes=["den"])
                                tk.op("dve", lambda e: e.tensor_scalar(comb[:, si, :], tmp8[:], den[:, 0:1], None, ALU.mult),
                                      reads=["tmp8", "den"], writes=["comb"])
                    self.barrier()
                with ExitStack() as st3:
                    hid = self.sb(st3, "f_hid", [128, 4, G], BF16)
                    sg = [self.sb(st3, f"f_sg{i}", [128, 512], F32) for i in range(2)]
                    if moe:
                        combB = [self.sb(st3, f"f_combB{i}", [128, G], F32) for i in range(2)]
                        dg = [self.sb(st3, f"f_dg{i}", [128, 128], F32) for i in range(2)]
                    for ex in range(nexp):
                        if moe:
                            cb = combB[ex % 2]
                            ckey = ("combB", ex % 2)
                            for si in range(G // 128):
                                dgt = dg[si % 2]
                                tk.op("dve", lambda e: e.tensor_scalar(dgt[:], ident, comb[:, si, ex:ex + 1], None, ALU.mult),
                                      reads=["comb", "c32"], writes=[("dg", si % 2)])
                                pb = 6 + si % 2
                                tk.op("pe", lambda e: e.matmul(self.ps[pb][:, 0:128], ones32, dgt[:], start=True, stop=True),
                                      reads=[("dg", si % 2), "c32"], writes=[("ps", pb)])
                                tk.op("act", lambda e: e.activation(out=cb[:, si * 128:(si + 1) * 128], in_=self.ps[pb][:, 0:128], func=AF.Copy),
                                      reads=[("ps", pb)], writes=[(ckey, si)])
                            Wg = self.wt("moe_w_gate", j)[ex]
                            Wu = self.wt("moe_w_up", j)[ex]
                            Wd = self.wt("moe_w_down", j)[ex]
                        else:
                            Wg = self.wt("ffn_w_gate", j)
                            Wu = self.wt("ffn_w_up", j)
                            Wd = self.wt("ffn_w_down", j)
                        Wg = Wg.rearrange("(k p) f -> p k f", p=128)
                        Wu = Wu.rearrange("(k p) f -> p k f", p=128)
                        Wd = Wd.rearrange("(k p) o -> p k o", p=128)
                        for fc in range(DFF // 512):
                            b = wi % 2
                            wi += 1
                            fs = slice(fc * 512, (fc + 1) * 512)
                            tk.dma("pool", lambda e: e.dma_start(out=wg[b][:], in_=Wg[:, :, fs]), writes=[("wg", b)], ring="wg", nring=2)
                            tk.dma("pool", lambda e: e.dma_start(out=wu[b][:], in_=Wu[:, :, fs]), writes=[("wu", b)], ring="wu", nring=2)
                            tk.dma("pool", lambda e: e.dma_start(out=wd[b][:], in_=Wd[:, fc * 4:(fc + 1) * 4, :]), writes=[("wd", b)], ring="wd", nring=2)
                            it = 0
                            for ft in range(4):
                                for tt in range(NT):
                                    cs = slice(tt * 512, (tt + 1) * 512)
                                    pg, pu = (0, 1) if it % 2 == 0 else (2, 3)
                                    sgt = sg[it % 2]
                                    skey = ("sg", it % 2)
                                    it += 1
                                    for k in range(8):
                                        tk.op("pe", lambda e: e.matmul(self.ps[pg][:], wg[b][:, k, ft * 128:(ft + 1) * 128], hT[:, k, cs],
                                                                       start=(k == 0), stop=(k == 7)),
                                              reads=[("wg", b), ("fh", tt)], writes=[("ps", pg)], signal=(k == 7))
                                    for k in range(8):
                                        tk.op("pe", lambda e: e.matmul(self.ps[pu][:], wu[b][:, k, ft * 128:(ft + 1) * 128], hT[:, k, cs],
                                                                       start=(k == 0), stop=(k == 7)),
                                              reads=[("wu", b), ("fh", tt)], writes=[("ps", pu)], signal=(k == 7))
                                    tk.op("act", lambda e: e.activation(out=sgt[:], in_=self.ps[pg][:], func=AF.Silu),
                                          reads=[("ps", pg)], writes=[skey])
                                    if moe:
                                        tk.op("dve", lambda e: e.tensor_tensor(sgt[:], sgt[:], self.ps[pu][:], ALU.mult),
                                              reads=[skey, ("ps", pu)], writes=[skey])
                                        tk.op("dve", lambda e: e.tensor_tensor(hid[:, ft, cs], sgt[:], cb[:, cs], ALU.mult),
                                              reads=[skey] + [(ckey, tt * 4 + q4) for q4 in range(4)], writes=[("hid", ft, tt)])
                                    else:
                                        tk.op("dve", lambda e: e.tensor_tensor(hid[:, ft, cs], sgt[:], self.ps[pu][:], ALU.mult),
                                              reads=[skey, ("ps", pu)], writes=[("hid", ft, tt)])
                            it = 0
                            for oc in range(8):
                                for tt in range(NT):
                                    cs = slice(tt * 512, (tt + 1) * 512)
                                    py = 4 + it % 2
                                    it += 1
                                    for ft in range(4):
                                        tk.op("pe", lambda e: e.matmul(self.ps[py][:], wd[b][:, ft, oc * 128:(oc + 1) * 128], hid[:, ft, cs],
                                                                       start=(ft == 0), stop=(ft == 3)),
                                              reads=[("wd", b), ("hid", ft, tt)], writes=[("ps", py)], signal=(ft == 3))
                                    tk.op("dve", lambda e: e.scalar_tensor_tensor(xT[:, oc, cs], self.ps[py][:], g2[:, oc:oc + 1], xT[:, oc, cs],
                                                                                  ALU.mult, ALU.add),
                                          reads=[("ps", py), ("fx", tt), "mod"], writes=[("fx", tt)])
                    tk.dma("sp", lambda e: e.dma_start(out=grp_ap, in_=xT[:]),
                           reads=[("fx", tt) for tt in range(NT)], writes=["xsT"], ring="fxs", nring=1)
                    self.barrier()

    def out_proj_residual(self, wo, gated, gkey, xT, xkey, n, banks=(0, 1)):
        tk = self.tk
        g1 = self.gate(0)
        for oc in range(8):
            pb = banks[oc % len(banks)]
            for k in range(8):
                tk.op("pe", lambda e: e.matmul(self.ps[pb][:, :n], wo[:, k, oc * 128:(oc + 1) * 128], gated[:, k, :n],
                                               start=(k == 0), stop=(k == 7)),
                      reads=["wo", gkey], writes=[("ps", pb)], signal=(k == 7))
            tk.op("dve", lambda e: e.scalar_tensor_tensor(xT[:, oc, :n], self.ps[pb][:, :n], g1[:, oc:oc + 1], xT[:, oc, :n],
                                                          ALU.mult, ALU.add),
                  reads=[("ps", pb), xkey, "mod"], writes=[xkey])

    def load_w(self, dst, src_v, key, ncol, piece=1024):
        for c0 in range(0, ncol, piece):
            c1 = min(ncol, c0 + piece)
            self.tk.dma("pool", lambda e: e.dma_start(out=dst[:, :, c0:c1], in_=src_v[:, :, c0:c1]),
                        writes=[key], ring="wload", nring=4)

    def phase_gla(self, l):
        tk, nc, T = self.tk, self.nc, self.T
        j = l // 2
        TG, NS = 256, 2
        with ExitStack() as st:
            w_in = self.sb(st, "g_win", [128, 8, 3088], BF16)
            w_out = self.sb(st, "g_wout", [128, 8, D], BF16)
            xT = self.sb(st, "g_xT", [128, 8, TG], F32)
            bufA = self.sb(st, "g_bufA", [128, 8, TG], F32)
            sq = self.sb(st, "g_sq", [128, 8, TG], BF16)
            hT = self.sb(st, "g_hT", [128, 8, TG], BF16)
            rstd = self.sb(st, "g_rstd", [128, 512], F32)
            qkT = self.sb(st, "g_qkT", [128, 8, TG], F32)
            rT = self.sb(st, "g_rT", [128, 8, TG], BF16)
            glow = self.sb(st, "g_glow", [32, TG], F32)
            v = self.sb(st, "g_v", [128, NS, D], BF16)
            ktok = self.sb(st, "g_ktok", [128, NS, 512], F32)
            az_ = [self.sb(st, f"g_az{i}", [128, 512], F32) for i in range(NS)]
            zs_ = [self.sb(st, f"g_zs{i}", [128, 512], F32) for i in range(NS)]
            la_ = [self.sb(st, f"g_la{i}", [128, 512], F32) for i in range(NS)]
            Eq_ = [self.sb(st, f"g_Eq{i}", [128, 512], F32) for i in range(NS)]
            Ek_ = [self.sb(st, f"g_Ek{i}", [128, 512], F32) for i in range(NS)]
            Eend_ = [self.sb(st, f"g_Eend{i}", [128, 512], F32) for i in range(NS)]
            kend = self.sb(st, "g_kend", [128, 512], BF16)
            qd = self.sb(st, "g_qd", [128, 4, 128], BF16)
            ki = self.sb(st, "g_ki", [128, 4, 128], BF16)
            am = self.sb(st, "g_am", [128, 4, 128], BF16)
            S = self.sb(st, "g_S", [128, 4, 256], F32)
            Sbf = self.sb(st, "g_Sbf", [128, 4, 256], BF16)
            rso = self.sb(st, "g_rso", [128, 4, TG], F32)
            tmpo = self.sb(st, "g_tmpo", [128, TG], F32)
            ps = self.ps
            self.load_w(w_in, self.wt("gla_w_in", j).rearrange("(k p) f -> p k f", p=128), "win", 3088, 772)
            self.load_w(w_out, self.wt("gla_w_out", j).rearrange("(k p) f -> p k f", p=128), "wo", D, 512)
            tk.op("dve", lambda e: e.memset(S[:], 0.0), writes=["S"])
            tk.op("dve", lambda e: e.memset(Sbf[:], 0.0), writes=["Sbf"])
            tk.op("dve", lambda e: e.memset(glow[:], 1.0), writes=["glow"])
            ow, _ = SP["wgu"]
            wgu = self.spt[0:17, ow + j * 512:ow + (j + 1) * 512]
            oo, _ = SP["onorm"]
            onorm = self.spt[:, oo + j * 2:oo + j * 2 + 2]
            tri = self.c32[:, C_TRI:C_TRI + 128]
            rev = self.c32[:, C_REV:C_REV + 128]
            ones = self.cbf[:, C_ONES:C_ONES + 128]
            maskb = self.c32[:, C_MASK:C_MASK + 128].unsqueeze(1).broadcast_to([128, 4, 128])
            oT = bufA
            gated = hT
            ev = 0
            for ti in range(T // TG):
                t0 = ti * TG
                tk.dma("sp", lambda e: e.dma_start(out=xT[:], in_=self.xsT_v[:, :, t0:t0 + TG]),
                       reads=["xsT"], writes=["gx"], ring="gxl", nring=1)
                self.norm_h(0, xT[:], "gx", TG, sq, bufA, rstd, [hT[:, c, :] for c in range(8)], ["gh"] * 8, 7)
                for k in range(8):
                    tk.op("pe", lambda e: e.matmul(ps[7][0:16, :TG], w_in[:, k, 3072:3088], hT[:, k, :],
                                                   start=(k == 0), stop=(k == 7)),
                          reads=["win", "gh"], writes=[("ps", 7)], signal=(k == 7))
                tk.op("dve", lambda e: e.tensor_copy(glow[0:16, :], ps[7][0:16, :TG]), reads=[("ps", 7)], writes=["glow"])
                groups = []

                def g_qk(oc):
                    pb = oc % 2
                    for k in range(8):
                        tk.op("pe", lambda e: e.matmul(ps[pb][:, :TG], w_in[:, k, oc * 128:(oc + 1) * 128], hT[:, k, :],
                                                       start=(k == 0), stop=(k == 7)),
                              reads=["win", "gh"], writes=[("ps", pb)], signal=(k == 7))
                    if oc % 2 == 0:
                        tk.op("act", lambda e: e.activation(out=qkT[:, oc, :], in_=ps[pb][:, :TG], func=AF.Copy),
                              reads=[("ps", pb)], writes=[("qk", oc)])
                    else:
                        tk.op("dve", lambda e: e.tensor_copy(qkT[:, oc, :], ps[pb][:, :TG]),
                              reads=[("ps", pb)], writes=[("qk", oc)])

                def g_r(oc):
                    pb = 2 + oc % 2
                    for k in range(8):
                        tk.op("pe", lambda e: e.matmul(ps[pb][:, :TG], w_in[:, k, 2048 + oc * 128:2048 + (oc + 1) * 128], hT[:, k, :],
                                                       start=(k == 0), stop=(k == 7)),
                              reads=["win", "gh"], writes=[("ps", pb)], signal=(k == 7))
                    tk.op("act", lambda e: e.activation(out=rT[:, oc, :], in_=ps[pb][:, :TG], func=AF.Silu),
                          reads=[("ps", pb)], writes=[("rT", oc)])

                def g_v(s, half):
                    tok = slice(s * 128, (s + 1) * 128)
                    pb = half
                    for k in range(8):
                        tk.op("pe", lambda e: e.matmul(ps[pb][:], hT[:, k, tok], w_in[:, k, 1024 + half * 512:1536 + half * 512],
                                                       start=(k == 0), stop=(k == 7)),
                              reads=["win", "gh"], writes=[("ps", pb)], signal=(k == 7))
                    if half == 0:
                        tk.op("act", lambda e: e.activation(out=v[:, s, 0:512], in_=ps[pb][:], func=AF.Copy),
                              reads=[("ps", pb)], writes=[("v", s, 0)])
                    else:
                        tk.op("dve", lambda e: e.tensor_copy(v[:, s, 512:1024], ps[pb][:]),
                              reads=[("ps", pb)], writes=[("v", s, 1)])

                def g_kt(s):
                    tok = slice(s * 128, (s + 1) * 128)
                    pb = 2 + s % 2
                    for k in range(8):
                        tk.op("pe", lambda e: e.matmul(ps[pb][:], hT[:, k, tok], w_in[:, k, 512:1024],
                                                       start=(k == 0), stop=(k == 7)),
                              reads=["win", "gh"], writes=[("ps", pb)], signal=(k == 7))
                    tk.op("dve", lambda e: e.tensor_copy(ktok[:, s, :], ps[pb][:]), reads=[("ps", pb)], writes=[("ktok", s)])

                for oc in range(8):
                    groups.append(lambda oc=oc: g_qk(oc))
                for s in range(NS):
                    groups.append(lambda s=s: g_v(s, 0))
                    groups.append(lambda s=s: g_v(s, 1))
                    groups.append(lambda s=s: g_kt(s))
                for oc in range(8):
                    groups.append(lambda oc=oc: g_r(oc))
                chain = []
                for s in range(NS):
                    tok = slice(s * 128, (s + 1) * 128)
                    zs, az, la, Eq, Ek, Eend = zs_[s], az_[s], la_[s], Eq_[s], Ek_[s], Eend_[s]
                    chain += [
                        lambda s=s, tok=tok: tk.op("pe", lambda e: e.matmul(ps[4][:], glow[0:17, tok], wgu, start=True, stop=True),
                                                   reads=["glow", "spt"], writes=[("ps", 4)]),
                        lambda s=s, zs=zs: tk.op("act", lambda e: e.activation(out=zs[:], in_=ps[4][:], func=AF.Copy),
                                                 reads=[("ps", 4)], writes=[("zs", s)]),
                        lambda s=s, zs=zs, az=az: tk.op("dve", lambda e: e.scalar_tensor_tensor(az[:], zs[:], -1.0, zs[:], ALU.mult, ALU.min),
                                                        reads=[("zs", s)], writes=[("az", s)]),
                        lambda s=s, az=az: tk.op("act", lambda e: e.activation(out=az[:], in_=az[:], func=AF.Exp),
                                                 reads=[("az", s)], writes=[("az", s)]),
                        lambda s=s, az=az: tk.op("act", lambda e: e.activation(out=az[:], in_=az[:], func=AF.Ln, bias=1.0, scale=1.0),
                                                 reads=[("az", s)], writes=[("az", s)]),
                        lambda s=s, zs=zs, az=az, la=la: tk.op("dve", lambda e: e.scalar_tensor_tensor(la[:], zs[:], 0.0, az[:], ALU.min, ALU.subtract),
                                                               reads=[("zs", s), ("az", s)], writes=[("la", s)]),
                        lambda s=s, la=la: [tk.op("pe", lambda e: e.matmul(ps[5][:, h * 128:(h + 1) * 128], la[:, h * 128:(h + 1) * 128], tri,
                                                                            start=True, stop=True),
                                                  reads=[("la", s), "c32"], writes=[("ps", 5)], signal=(h == 3)) for h in range(4)],
                        lambda s=s, Eq=Eq: tk.op("act", lambda e: e.activation(out=Eq[:], in_=ps[5][:], func=AF.Exp),
                                                 reads=[("ps", 5)], writes=[("Eq", s)]),
                        lambda s=s, Ek=Ek: tk.op("act", lambda e: e.activation(out=Ek[:], in_=ps[5][:], func=AF.Exp, scale=-1.0),
                                                 reads=[("ps", 5)], writes=[("Ek", s)]),
                        lambda s=s, la=la: tk.op("pe", lambda e: e.matmul(ps[4][:], rev, la[:], start=True, stop=True),
                                                 reads=[("la", s), "c32"], writes=[("ps", 4)]),
                        lambda s=s, Eend=Eend: tk.op("act", lambda e: e.activation(out=Eend[:], in_=ps[4][:], func=AF.Exp),
                                                     reads=[("ps", 4)], writes=[("Eend", s)]),
                    ]
                for i in range(max(len(groups), len(chain))):
                    if i < len(groups):
                        groups[i]()
                    if i < len(chain):
                        chain[i]()
                for s in range(NS):
                    tok = slice(s * 128, (s + 1) * 128)
                    Eq, Ek, Eend = Eq_[s], Ek_[s], Eend_[s]
                    tk.op("dve", lambda e: e.tensor_tensor(kend[:], ktok[:, s, :], Eend[:], ALU.mult),
                          reads=[("ktok", s), ("Eend", s)], writes=["kend"])
                    tk.op("dve", lambda e: e.scalar_tensor_tensor(qd[:], qkT[:, 0:4, tok], 128.0 ** -0.5,
                                                                  Eq[:].rearrange("p (h t) -> p h t", h=4), ALU.mult, ALU.mult),
                          reads=[("qk", c) for c in range(4)] + [("Eq", s)], writes=["qd"])
                    tk.op("dve", lambda e: e.tensor_tensor(ki[:], qkT[:, 4:8, tok], Ek[:].rearrange("p (h t) -> p h t", h=4), ALU.mult),
                          reads=[("qk", c) for c in range(4, 8)] + [("Ek", s)], writes=["ki"])
                    for h in range(4):
                        tk.op("pe", lambda e: e.matmul(ps[6][:, h * 128:(h + 1) * 128], ki[:, h, :], qd[:, h, :], start=True, stop=True),
                              reads=["ki", "qd"], writes=[("ps", 6)], signal=(h == 3))
                    tk.op("dve", lambda e: e.tensor_tensor(am[:], ps[6][:].rearrange("p (h t) -> p h t", h=4), maskb, ALU.mult),
                          reads=[("ps", 6), "c32"], writes=["am"])
                    for c in range(8):
                        h, jj = c // 2, c % 2
                        pb = c // 4
                        col = slice((c % 4) * 128, (c % 4 + 1) * 128)
                        tk.op("pe", lambda e: e.matmul(ps[pb][:, col], v[:, s, c * 128:(c + 1) * 128], am[:, h, :], start=True, stop=False),
                              reads=[("v", s, c // 4), "am"], writes=[("ps", pb)], signal=False)
                        tk.op("pe", lambda e: e.matmul(ps[pb][:, col], Sbf[:, h, jj * 128:(jj + 1) * 128], qd[:, h, :], start=False, stop=True),
                              reads=["Sbf", "qd"], writes=[("ps", pb)], signal=(c % 4 == 3))
                    tk.op("act", lambda e: e.activation(out=oT[:, 0:4, tok], in_=ps[0][:].rearrange("p (c t) -> p c t", c=4), func=AF.Copy),
                          reads=[("ps", 0)], writes=["oT"])
                    tk.op("dve", lambda e: e.tensor_copy(oT[:, 4:8, tok], ps[1][:].rearrange("p (c t) -> p c t", c=4)),
                          reads=[("ps", 1)], writes=["oT"])
                    for h in range(4):
                        pb = 2 + h // 2
                        col = slice((h % 2) * 256, (h % 2 + 1) * 256)
                        tk.op("pe", lambda e: e.matmul(ps[pb][:, col], kend[:, h * 128:(h + 1) * 128], v[:, s, h * 256:(h + 1) * 256],
                                                       start=True, stop=True),
                              reads=["kend", ("v", s, h // 2)], writes=[("ps", pb)], signal=(h % 2 == 1))
                    for h in range(4):
                        pb = 2 + h // 2
                        col = slice((h % 2) * 256, (h % 2 + 1) * 256)
                        tk.op("dve", lambda e: e.scalar_tensor_tensor(S[:, h, :], S[:, h, :], Eq[:, h * 128 + 127:h * 128 + 128], ps[pb][:, col],
                                                                      ALU.mult, ALU.add),
                              reads=["S", ("Eq", s), ("ps", pb)], writes=["S"])
                    tk.op("act", lambda e: e.activation(out=Sbf[:], in_=S[:], func=AF.Copy), reads=["S"], writes=["Sbf"])
                tk.op("act", lambda e: e.activation(out=sq[:], in_=oT[:], func=AF.Square), reads=["oT"], writes=["sq"])
                for h in range(4):
                    pb = 6 + h // 2
                    col = slice((h % 2) * TG, (h % 2 + 1) * TG)
                    for jj in range(2):
                        tk.op("pe", lambda e: e.matmul(ps[pb][:, col], ones, sq[:, 2 * h + jj, :], start=(jj == 0), stop=(jj == 1)),
                              reads=["sq", "cbf"], writes=[("ps", pb)], signal=(jj == 1 and h % 2 == 1))
                for hb in range(2):
                    tk.op("act", lambda e: e.activation(out=rso[:, 2 * hb:2 * hb + 2, :],
                                                        in_=ps[6 + hb][:, :2 * TG].rearrange("p (h t) -> p h t", h=2),
                                                        func=AF.Sqrt, bias=EPS, scale=1.0 / 256),
                          reads=[("ps", 6 + hb)], writes=["rso"])
                tk.op("dve", lambda e: e.reciprocal(rso[:], rso[:]), reads=["rso"], writes=["rso"])
                for c in range(8):
                    h, jj = c // 2, c % 2
                    tk.op("dve", lambda e: e.scalar_tensor_tensor(tmpo[:], oT[:, c, :], onorm[:, jj:jj + 1], rso[:, h, :], ALU.mult, ALU.mult),
                          reads=["oT", "rso", "spt"], writes=["tmpo"])
                    tk.op("dve", lambda e: e.tensor_tensor(gated[:, c, :], tmpo[:], rT[:, c, :], ALU.mult),
                          reads=["tmpo", ("rT", c), "gh"], writes=["gh"])
                self.out_proj_residual(w_out, gated, "gh", xT, "gx", TG, banks=(0, 1))
                tk.dma("sp", lambda e: e.dma_start(out=self.xsT_v[:, :, t0:t0 + TG], in_=xT[:]),
                       reads=["gx"], writes=["xsT"], ring="gxs", nring=1)
            self.barrier()

    def phase_fox(self, l):
        self.phase_fox_proj(l)
        self.phase_fox_attn(l)
        self.phase_fox_out(l)

    def phase_fox_proj(self, l):
        tk, nc, T = self.tk, self.nc, self.T
        j = l // 2
        TF = 512
        with ExitStack() as st:
            w_in = self.sb(st, "p_win", [128, 8, 4112], BF16)
            xT = self.sb(st, "p_xT", [128, 8, TF], F32)
            xr = self.sb(st, "p_xr", [128, 8, TF], F32)
            sq = self.sb(st, "p_sq", [128, 8, TF], BF16)
            hT = self.sb(st, "p_hT", [128, 8, TF], BF16)
            rstd = self.sb(st, "p_rstd", [128, 512], F32)
            qf = self.sb(st, "p_qf", [128, 4, TF], F32)
            qsq = self.sb(st, "p_qsq", [128, 4, TF], BF16)
            rs = self.sb(st, "p_rs", [128, 4, TF], F32)
            qn = [self.sb(st, f"p_qn{i}", [128, 4, TF], BF16) for i in range(2)]
            vt = self.sb(st, "p_vt", [128, 4, D], BF16)
            sg = self.sb(st, "p_sg", [128, 8, TF], BF16)
            zs = self.sb(st, "p_zs", [128, 16], F32)
            az = self.sb(st, "p_az", [128, 16], F32)
            lf = self.sb(st, "p_lf", [128, 16], F32)
            carry = self.sb(st, "p_carry", [128, 16], F32)
            ps = self.ps
            self.load_w(w_in, self.wt("fox_w_in", j).rearrange("(k p) f -> p k f", p=128), "win", 4112, 1028)
            tk.op("dve", lambda e: e.memset(carry[:], 0.0), writes=["carry"])
            ob, _ = SP["bf"]
            bfB = self.sb(st, "p_bfB", [128, 16], F32)
            tk.op("pe", lambda e: e.matmul(self.ps[6][:, 0:16], self.c32[0:1, C_ONES:C_ONES + 128],
                                           self.spt[0:1, ob + j * 16:ob + (j + 1) * 16], start=True, stop=True),
                  reads=["c32", "spt"], writes=[("ps", 6)])
            tk.op("dve", lambda e: e.tensor_copy(bfB[:], self.ps[6][:, 0:16]), reads=[("ps", 6)], writes=["bfB"])
            bd = self.cbf[:, C_BD:C_BD + 128]
            cm = self.c32[:, C_MASK:C_MASK + 128]
            ones32 = self.c32[:, C_ONES:C_ONES + 128]
            ob, _ = SP["bf"]
            bfr = self.spt[0:1, ob + j * 16:ob + (j + 1) * 16]
            oq, _ = SP["qn"]
            okn, _ = SP["kn"]
            qs_v = self.qs.rearrange("(c p) t -> p c t", p=128)
            ks_v = self.ks.rearrange("(c p) t -> p c t", p=128)
            gs_v = self.gs.rearrange("(c p) t -> p c t", p=128)
            vs_v = self.vs.rearrange("(n p) d -> p n d", p=128)
            gi = 0
            for ti in range(T // TF):
                t0 = ti * TF
                tk.dma("sp", lambda e: e.dma_start(out=xT[:], in_=self.xsT_v[:, :, t0:t0 + TF]),
                       reads=["xsT"], writes=["px"], ring="pxl", nring=1)
                self.norm_h(0, xT[:], "px", TF, sq, xr, rstd, [hT[:, c, :] for c in range(8)], ["ph"] * 8, 7)
                for grp in range(4):
                    isk = grp >= 2
                    base = (1024 if isk else 0) + (grp % 2) * 512
                    gain = self.spt[:, (okn if isk else oq) + j:(okn if isk else oq) + j + 1]
                    for c in range(4):
                        for k in range(8):
                            tk.op("pe", lambda e: e.matmul(ps[c][:], w_in[:, k, base + c * 128:base + (c + 1) * 128], hT[:, k, :],
                                                           start=(k == 0), stop=(k == 7)),
                                  reads=["win", "ph"], writes=[("ps", c)], signal=(k == 7))
                        tk.op("act", lambda e: e.activation(out=qf[:, c, :], in_=ps[c][:], func=AF.Copy),
                              reads=[("ps", c)], writes=["qf"])
                    tk.op("dve", lambda e: e.tensor_tensor(qsq[:], qf[:], qf[:], ALU.mult), reads=["qf"], writes=["qsq"])
                    for c in range(4):
                        tk.op("pe", lambda e: e.matmul(ps[4 + c][:], bd, qsq[:, c, :], start=True, stop=True),
                              reads=["qsq", "cbf"], writes=[("ps", 4 + c)])
                        tk.op("act", lambda e: e.activation(out=rs[:, c, :], in_=ps[4 + c][:], func=AF.Ln, bias=EPS, scale=1.0 / 64),
                              reads=[("ps", 4 + c)], writes=["rs"])
                    tk.op("act", lambda e: e.activation(out=rs[:], in_=rs[:], func=AF.Exp, scale=-0.5), reads=["rs"], writes=["rs"])
                    qb_ = qn[gi % 2]
                    qkey = ("qn", gi % 2)
                    gi += 1
                    tk.op("dve", lambda e: e.scalar_tensor_tensor(qb_[:], qf[:], gain, rs[:], ALU.mult, ALU.mult),
                          reads=["qf", "rs", "spt"], writes=[qkey])
                    dst = (ks_v if isk else qs_v)[:, (grp % 2) * 4:(grp % 2) * 4 + 4, t0:t0 + TF]
                    tk.dma("sp", lambda e: e.dma_start(out=dst, in_=qb_[:]), reads=[qkey], writes=["ks" if isk else "qs"],
                           ring="pqs", nring=2)
                for oc in range(8):
                    pb = oc % 4
                    for k in range(8):
                        tk.op("pe", lambda e: e.matmul(ps[pb][:], w_in[:, k, 3072 + oc * 128:3072 + (oc + 1) * 128], hT[:, k, :],
                                                       start=(k == 0), stop=(k == 7)),
                              reads=["win", "ph"], writes=[("ps", pb)], signal=(k == 7))
                    tk.op("act", lambda e: e.activation(out=sg[:, oc, :], in_=ps[pb][:], func=AF.Sigmoid),
                          reads=[("ps", pb)], writes=["sg"])
                tk.dma("sp", lambda e: e.dma_start(out=gs_v[:, :, t0:t0 + TF], in_=sg[:]), reads=["sg"], writes=["gs"],
                       ring="pgs", nring=1)
                for s in range(4):
                    tok = slice(s * 128, (s + 1) * 128)
                    for half in range(2):
                        pb = 4 + half
                        for k in range(8):
                            tk.op("pe", lambda e: e.matmul(ps[pb][:], hT[:, k, tok], w_in[:, k, 2048 + half * 512:2560 + half * 512],
                                                           start=(k == 0), stop=(k == 7)),
                                  reads=["win", "ph"], writes=[("ps", pb)], signal=(k == 7))
                        if half == 0:
                            tk.op("act", lambda e: e.activation(out=vt[:, s, 0:512], in_=ps[pb][:], func=AF.Copy),
                                  reads=[("ps", pb)], writes=["vt"])
                        else:
                            tk.op("dve", lambda e: e.tensor_copy(vt[:, s, 512:1024], ps[pb][:]), reads=[("ps", pb)], writes=["vt"])
                    for k in range(8):
                        tk.op("pe", lambda e: e.matmul(ps[6][:, 0:16], hT[:, k, tok], w_in[:, k, 4096:4112], start=(k == 0), stop=(k == 7)),
                              reads=["win", "ph"], writes=[("ps", 6)], signal=(k == 7))
                    tk.op("dve", lambda e: e.tensor_tensor(zs[:], ps[6][:, 0:16], bfB[:], ALU.add), reads=[("ps", 6), "bfB"], writes=["pzs"])
                    tk.op("dve", lambda e: e.scalar_tensor_tensor(az[:], zs[:], -1.0, zs[:], ALU.mult, ALU.min), reads=["pzs"], writes=["paz"])
                    tk.op("act", lambda e: e.activation(out=az[:], in_=az[:], func=AF.Exp), reads=["paz"], writes=["paz"])
                    tk.op("act", lambda e: e.activation(out=az[:], in_=az[:], func=AF.Ln, bias=1.0, scale=1.0), reads=["paz"], writes=["paz"])
                    tk.op("dve", lambda e: e.scalar_tensor_tensor(lf[:], zs[:], 0.0, az[:], ALU.min, ALU.subtract),
                          reads=["pzs", "paz"], writes=["lf"])
                    gt = ti * 4 + s
                    tk.op("pe", lambda e: e.matmul(ps[6][:, 16:32], cm, lf[:], start=True, stop=True),
                          reads=["lf", "c32"], writes=[("ps", 6)], signal=False)
                    tk.op("pe", lambda e: e.matmul(ps[6][:, 32:48], ones32, lf[:], start=True, stop=True),
                          reads=["lf", "c32"], writes=[("ps", 6)])
                    tk.op("dve", lambda e: e.tensor_tensor(self.Fk[:, gt, :], ps[6][:, 16:32], carry[:], ALU.add),
                          reads=[("ps", 6), "carry"], writes=["Fk"])
                    tk.op("dve", lambda e: e.tensor_tensor(carry[:], ps[6][:, 32:48], carry[:], ALU.add),
                          reads=[("ps", 6), "carry"], writes=["carry"])
                    if s == 1:
                        tk.op("dve", lambda e: e.tensor_copy(self.Fref[:, ti, :], carry[:]), reads=["carry"], writes=["Fref"])
                tk.dma("sp", lambda e: e.dma_start(out=vs_v[:, ti * 4:ti * 4 + 4, :], in_=vt[:]), reads=["vt"], writes=["vs"],
                       ring="pvs", nring=1)
            self.barrier()

    def phase_fox_attn(self, l):
        tk, nc, T = self.tk, self.nc, self.T
        NQB, NKT = T // 512, T // 128
        with ExitStack() as st:
            QhT = [self.sb(st, f"a_q{i}", [128, T], BF16) for i in range(2)]
            KhT = [self.sb(st, f"a_k{i}", [128, T], BF16) for i in range(2)]
            Vh = [self.sb(st, f"a_v{i}", [128, NKT, 128], BF16) for i in range(2)]
            Gh = [self.sb(st, f"a_g{i}", [64, T], BF16) for i in range(2)]
            bias = self.sb(st, "a_bias", [128, NQB, NKT, 16], F32)
            Pt = [self.sb(st, f"a_P{i}", [128, 512], BF16) for i in range(4)]
            rl = self.sb(st, "a_rl", [65, 512], F32)
            bcs = self.sb(st, "a_bcs", [64, 512], F32)
            t1 = self.sb(st, "a_t1", [64, 512], F32)
            og = [self.sb(st, f"a_og{i}", [64, 512], BF16) for i in range(2)]
            ps = self.ps
            cmb = self.cbf[:, C_MASK:C_MASK + 128]
            vs_v = self.vs.rearrange("(n p) d -> p n d", p=128)
            for i in range(2):
                tk.op("dve", lambda e: e.memset(Vh[i][:], 1.0), writes=[("Vh", i)])
                tk.op("dve", lambda e: e.memset(QhT[i][64:128, :], 0.0), writes=[("Qh", i)])
                tk.op("dve", lambda e: e.memset(KhT[i][64:128, :], 0.0), writes=[("Kh", i)])
            for qb in range(NQB):
                nk = 4 * qb + 4
                tk.op("dve", lambda e: e.tensor_tensor(bias[:, qb, 0:nk, :],
                                                       self.Fref[:, qb, :].unsqueeze(1).broadcast_to([128, nk, 16]),
                                                       self.Fk[:, 0:nk, :], ALU.subtract),
                      reads=["Fref", "Fk"], writes=["bias"])
            L = 3
            items = [(h, qb, kt) for h in range(16) for qb in range(NQB) for kt in range(4 * qb + 4)]
            head_start = {}
            for i, (h, qb, kt) in enumerate(items):
                head_start.setdefault(h, i)

            def loads(h):
                b = h % 2
                hs = slice(h * 64, (h + 1) * 64)
                q = "pool"
                tk.dma(q, lambda e: e.dma_start(out=QhT[b][0:64, :], in_=self.qs[hs, :]), reads=["qs"], writes=[("Qh", b)], ring="aq", nring=2)
                tk.dma(q, lambda e: e.dma_start(out=KhT[b][0:64, :], in_=self.ks[hs, :]), reads=["ks"], writes=[("Kh", b)], ring="ak", nring=2)
                for n0 in range(0, NKT, 8):
                    n1 = min(NKT, n0 + 8)
                    tk.dma(q, lambda e: e.dma_start(out=Vh[b][:, n0:n1, 0:64], in_=vs_v[:, n0:n1, hs]), reads=["vs"], writes=[("Vh", b)],
                           ring="av", nring=8)
                tk.dma(q, lambda e: e.dma_start(out=Gh[b][:], in_=self.gs[hs, :]), reads=["gs"], writes=[("Gh", b)], ring="ag", nring=2)

            later = []

            def tick():
                for ent in later:
                    ent[0] -= 1
                while later and later[0][0] <= 0:
                    later.pop(0)[1]()

            def emit_pv(i):
                h, qb, kt = items[i]
                b = h % 2
                nk = 4 * qb + 4
                blk = h * NQB + qb
                po = 4 + blk % 2
                pb = i % 4
                P = Pt[pb]
                c0 = max(0, kt - 4 * qb) * 128
                tk.op("pe", lambda e: e.matmul(ps[po][:, c0:512], Vh[b][:, kt, :], P[:, c0:512],
                                               start=(kt == 0), stop=(kt == nk - 1)),
                      reads=[("Vh", b), ("P", pb)], writes=[("ps", po)], signal=(kt == nk - 1))
                if kt != nk - 1:
                    return
                hs = slice(h * 64, (h + 1) * 64)
                ob_ = og[blk % 2]
                okey = ("og", blk % 2)
                tk.op("dve", lambda e: e.reciprocal(rl[64:65, :], ps[po][64:65, :]), reads=[("ps", po)], writes=["rl"])

                def epi():
                    tk.op("pe", lambda e: e.matmul(ps[6][0:64, :], self.c32[64:65, C_ONES:C_ONES + 64], rl[64:65, :], start=True, stop=True),
                          reads=["rl", "c32"], writes=[("ps", 6)])
                    tk.op("act", lambda e: e.activation(out=bcs[:], in_=ps[6][0:64, :], func=AF.Copy), reads=[("ps", 6)], writes=["bcs"])
                    tk.op("dve", lambda e: e.tensor_tensor(t1[:], ps[po][0:64, :], bcs[:], ALU.mult),
                          reads=[("ps", po), "bcs"], writes=["t1"])
                    tk.op("dve", lambda e: e.tensor_tensor(ob_[:], t1[:], Gh[b][:, qb * 512:(qb + 1) * 512], ALU.mult),
                          reads=["t1", ("Gh", b)], writes=[okey])
                    tk.dma("sp", lambda e: e.dma_start(out=self.os_[hs, qb * 512:(qb + 1) * 512], in_=ob_[:]),
                           reads=[okey], writes=["os"], ring="aos", nring=2)
                later.append([2, epi])

            loads(0)
            pend = []
            for i, (h, qb, kt) in enumerate(items):
                b = h % 2
                c0 = max(0, kt - 4 * qb) * 128
                pb = i % 4
                P = Pt[pb]
                tk.op("pe", lambda e: e.matmul(ps[pb][:, c0:512], KhT[b][:, kt * 128:(kt + 1) * 128],
                                               QhT[b][:, qb * 512 + c0:(qb + 1) * 512], start=True, stop=True),
                      reads=[("Kh", b), ("Qh", b)], writes=[("ps", pb)])
                tk.op("act", lambda e: e.activation(out=P[:, c0:512], in_=ps[pb][:, c0:512], func=AF.Exp,
                                                    bias=bias[:, qb, kt, h:h + 1], scale=0.125),
                      reads=[("ps", pb), "bias"], writes=[("P", pb)])
                if kt - 4 * qb >= 0:
                    tk.op("dve", lambda e: e.tensor_tensor(P[:, c0:c0 + 128], P[:, c0:c0 + 128], cmb, ALU.mult),
                          reads=[("P", pb), "cbf"], writes=[("P", pb)])
                tick()
                pend.append(i)
                if len(pend) > L:
                    emit_pv(pend.pop(0))
                if i == head_start[h] + L + 2 and h + 1 < 16:
                    loads(h + 1)
            while pend:
                emit_pv(pend.pop(0))
                tick()
            while later:
                later.pop(0)[1]()
            self.barrier()

    def phase_fox_out(self, l):
        tk, nc, T = self.tk, self.nc, self.T
        j = l // 2
        TF = 512
        with ExitStack() as st:
            w_out = self.sb(st, "o_wout", [128, 8, D], BF16)
            xT = [self.sb(st, f"o_xT{i}", [128, 8, TF], F32) for i in range(2)]
            gT = [self.sb(st, f"o_gT{i}", [128, 8, TF], BF16) for i in range(2)]
            self.load_w(w_out, self.wt("fox_w_out", j).rearrange("(k p) f -> p k f", p=128), "wo", D, 512)
            os_v = self.os_.rearrange("(c p) t -> p c t", p=128)
            for ti in range(T // TF):
                t0 = ti * TF
                b = ti % 2
                tk.dma("sp", lambda e: e.dma_start(out=xT[b][:], in_=self.xsT_v[:, :, t0:t0 + TF]),
                       reads=["xsT"], writes=[("ox", b)], ring="oxl", nring=2)
                tk.dma("sp", lambda e: e.dma_start(out=gT[b][:], in_=os_v[:, :, t0:t0 + TF]),
                       reads=["os"], writes=[("og", b)], ring="ogl", nring=2)
                self.out_proj_residual(w_out, gT[b], ("og", b), xT[b], ("ox", b), TF, banks=(0, 1, 2, 3))
                tk.dma("sp", lambda e: e.dma_start(out=self.xsT_v[:, :, t0:t0 + TF], in_=xT[b][:]),
                       reads=[("ox", b)], writes=["xsT"], ring="oxs", nring=2)
            self.barrier()

    def build_all(self, layers=(0, 1, 2, 3)):
        with ExitStack() as st:
            self.setup(st)

            def mods(st2):
                cond = self.sb(st2, "cond", [128, 8], BF16)
                wa = [self.sb(st2, f"wa{i}", [128, 8, 1536], BF16) for i in range(2)]
                self.mod_bufs = (cond, wa)
                self.tk.op("act", lambda e: e.activation(out=cond[:], in_=self.spv("cT"), func=AF.Silu),
                           reads=["spt"], writes=["cond"])
                for l in layers:
                    self.phase_mod(l)
            self.phase_in(barrier=True, body=mods)
            for l in layers:
                self.cur = l
                if l % 2 == 0:
                    self.phase_gla(l)
                    self.phase_ffn(l, moe=False)
                else:
                    self.phase_fox(l)
                    self.phase_ffn(l, moe=True, dyn=(self.split_last and l == layers[-1]))
            self.phase_out()
            self.tk.drain("sp")
        return self.nc


from concourse.bass_utils import run_bass_kernel_spmd

SEQ = 4096
BATCH = 4
N_CORES = 8
_PROG = {}


def _program():
    if "k" not in _PROG:
        k = K(SEQ, split_last=True)
        k.build_all((0, 1, 2, 3))
        _PROG["k"] = k
    return _PROG["k"]


def kernel(**inputs):
    inp = {n: np.asarray(v) for n, v in inputs.items()}
    k = _program()
    consts = make_consts()
    G = k.G
    in_maps = []
    for c in range(N_CORES):
        b, half = c % BATCH, c // BATCH
        if half == 0:
            d = {"x": np.ascontiguousarray(inp["x"][b], dtype=np.float32), "consts": consts, "sp": pack_small(inp, b)}
            for n in k.w:
                base, j = n.rsplit("_", 1)
                d[n] = np.ascontiguousarray(inp[base][int(j)], dtype=np.float32)
        else:
            d = dict(in_maps[b])
        d["sel"] = np.array([[half, half * G, 0, 0]], np.int32)
        in_maps.append(d)
    res = run_bass_kernel_spmd(k.nc, in_maps, core_ids=list(range(N_CORES)))
    out = np.stack([np.concatenate([np.asarray(res.results[b]["out"], dtype=np.float32)[:G],
                                    np.asarray(res.results[b + BATCH]["out"], dtype=np.float32)[G:]], 0)
                    for b in range(BATCH)], 0)
    return out
```

```python
from contextlib import ExitStack


class Tok:
    __slots__ = ("sem", "count", "eng")

    def __init__(self, sem=None, count=None, eng=None):
        self.sem = sem
        self.count = count
        self.eng = eng


class Tracker:
    ROLL = 30000

    def __init__(self, nc, stack: ExitStack):
        self.nc = nc
        self.stack = stack
        self.engs = {"pe": nc.tensor, "act": nc.scalar, "dve": nc.vector,
                     "pool": nc.gpsimd, "sp": nc.sync}
        self.sem = {}
        self.cnt = {}
        self.nsem = 0
        for e in self.engs:
            self._new_sem(e)
        self.waited = {e: {} for e in self.engs}
        self.last_w = {}
        self.readers = {}
        self.pending = {e: [] for e in self.engs}
        self.rings = {}
        self.n_ins = 0

    def _alloc(self, name):
        self.nsem += 1
        return self.stack.enter_context(self.nc.semaphore(f"{name}_{self.nsem}"))

    def _new_sem(self, e):
        self.sem[e] = self._alloc(f"e_{e}")
        self.cnt[e] = 0

    def _wait(self, eng, tok):
        if tok is None:
            return
        if tok.eng == eng and eng == "pe":
            return
        assert tok.count is not None, "dependency on an unsignaled instruction"
        w = self.waited[eng]
        k = id(tok.sem)
        if w.get(k, 0) >= tok.count:
            return
        self.engs[eng].wait_ge(tok.sem, tok.count)
        w[k] = tok.count

    def _deps(self, reads, writes):
        deps = []
        for b in reads:
            t = self.last_w.get(b)
            if t is not None:
                deps.append(t)
        for b in writes:
            t = self.last_w.get(b)
            if t is not None:
                deps.append(t)
            deps.extend(self.readers.get(b, ()))
        return deps

    def _record(self, tok, reads, writes):
        for b in reads:
            self.readers.setdefault(b, []).append(tok)
        for b in writes:
            self.last_w[b] = tok
            self.readers[b] = []

    def op(self, eng, fn, reads=(), writes=(), signal=True):
        for t in self._deps(reads, writes):
            self._wait(eng, t)
        ins = fn(self.engs[eng])
        self.n_ins += 1
        if signal:
            if self.cnt[eng] >= self.ROLL:
                self._new_sem(eng)
            self.cnt[eng] += 1
            ins.then_inc(self.sem[eng], 1)
            tok = Tok(self.sem[eng], self.cnt[eng], eng)
            for p in self.pending[eng]:
                p.sem, p.count = tok.sem, tok.count
            self.pending[eng] = []
        else:
            tok = Tok(None, None, eng)
            self.pending[eng].append(tok)
        self._record(tok, reads, writes)
        return tok

    def dma(self, eng, fn, reads=(), writes=(), ring="d", nring=4):
        r = self.rings.get(ring)
        if r is None:
            r = self.rings[ring] = {"sems": [self._alloc(f"r_{ring}") for _ in range(nring)],
                                    "cnt": [0] * nring, "i": 0}
        i = r["i"]
        r["i"] = (i + 1) % len(r["sems"])
        sem = r["sems"][i]
        if r["cnt"][i] > 0:
            self._wait(eng, Tok(sem, r["cnt"][i], "dma"))
        for t in self._deps(reads, writes):
            self._wait(eng, t)
        ins = fn(self.engs[eng])
        self.n_ins += 1
        r["cnt"][i] += 16
        ins.then_inc(sem, 16)
        tok = Tok(sem, r["cnt"][i], "dma")
        self._record(tok, reads, writes)
        return tok

    def drain(self, eng="sp"):
        for r in self.rings.values():
            for sem, c in zip(r["sems"], r["cnt"]):
                if c:
                    self._wait(eng, Tok(sem, c, "dma"))
        for e in self.engs:
            assert not self.pending[e], f"unsignaled tail on {e}"
            if self.cnt[e]:
                self._wait(eng, Tok(self.sem[e], self.cnt[e], e))


import numpy as np
from contextlib import ExitStack
import concourse.bass as bass
import concourse.mybir as mybir

F32 = mybir.dt.float32
BF16 = mybir.dt.bfloat16
AF = mybir.ActivationFunctionType
ALU = mybir.AluOpType
AX = mybir.AxisListType

D = 1024
KC = 8
DFF = 3584
NE = 8
EPS = 1e-6

C_ID, C_MASK, C_TRI, C_REV, C_BD, C_ONES, C_SEL = 0, 128, 256, 386, 514, 642, 770
NCONST = 898


def make_consts():
    c = np.zeros((128, NCONST), np.float32)
    s = np.arange(128)[:, None]
    t = np.arange(128)[None, :]
    c[:, C_ID:C_ID + 128] = (s == t)
    c[:, C_MASK:C_MASK + 128] = (s <= t)
    c[:, C_TRI:C_TRI + 128] = (s <= t) / 16.0
    c[:, C_TRI + 128] = 1.0 / 16.0
    c[:, C_REV:C_REV + 128] = (s > t) / 16.0
    c[:, C_BD:C_BD + 128] = (s // 64 == t // 64)
    c[:, C_ONES:C_ONES + 128] = 1.0
    c[127, C_SEL:C_SEL + 128] = 1.0
    return c


SP = {}
_o = 0
for _n, _w in [("cT", 8), ("adab", 4 * 48), ("ngain", 4 * 2 * 8), ("wgu", 2 * 512), ("onorm", 2 * 2),
               ("bf", 2 * 16), ("qn", 2), ("kn", 2), ("wr", 2 * 8 * 8), ("br", 2 * 8)]:
    SP[_n] = (_o, _w)
    _o += _w
NSP = _o


def pack_small(inp, b):
    sp = np.zeros((128, NSP), np.float32)

    def put(name, arr):
        o, w = SP[name]
        arr = np.asarray(arr, np.float32).reshape(arr.shape[0], -1)
        assert arr.shape[1] == w, (name, arr.shape, w)
        sp[:arr.shape[0], o:o + w] = arr

    fm = lambda v, n: np.asarray(v, np.float32).reshape(n, 128).T
    put("cT", fm(inp["c"][b], 8))
    put("adab", np.stack([fm(inp["ada_b"][l], 48) for l in range(4)], 1))
    put("ngain", np.stack([np.stack([fm(inp["norm_gain"][l, s], 8) for s in range(2)], 1) for l in range(4)], 1))
    wgu = np.zeros((32, 2, 512), np.float32)
    for j in range(2):
        wgu[:16, j] = inp["gla_w_gate_up"][j]
        wgu[16, j] = inp["gla_b_gate"][j]
    put("wgu", wgu)
    put("onorm", np.stack([fm(inp["gla_o_norm"][j], 2) for j in range(2)], 1))
    put("bf", np.asarray(inp["fox_b_f"], np.float32).reshape(1, 32))
    put("qn", np.stack([np.tile(inp["fox_q_norm"][j], 2) for j in range(2)], 1))
    put("kn", np.stack([np.tile(inp["fox_k_norm"][j], 2) for j in range(2)], 1))
    put("wr", np.stack([np.asarray(inp["moe_w_router"][j]).reshape(8, 128, 8).transpose(1, 0, 2) for j in range(2)], 1))
    put("br", np.asarray(inp["moe_b_router"], np.float32).reshape(1, 16))
    return sp


class K:
    def __init__(self, T, dbg=(), G=None, split_last=False):
        self.T = T
        self.G = G or min(2048, T)
        self.split_last = split_last
        self.dbg = set(dbg)
        nc = self.nc = bass.Bass("TRN2", target_bir_lowering=False)
        dt = nc.dram_tensor
        self.x = dt("x", [T, D], F32, kind="ExternalInput").ap()
        self.consts_d = dt("consts", [128, NCONST], F32, kind="ExternalInput").ap()
        self.sp_d = dt("sp", [128, NSP], F32, kind="ExternalInput").ap()
        if split_last:
            self.sel_d = dt("sel", [1, 4], mybir.dt.int32, kind="ExternalInput").ap()
        self.w = {}
        self.wshape = {"ada_w": [D, 6 * D], "gla_w_in": [D, 3088], "gla_w_out": [D, D],
                       "fox_w_in": [D, 4112], "fox_w_out": [D, D],
                       "ffn_w_gate": [D, DFF], "ffn_w_up": [D, DFF], "ffn_w_down": [DFF, D],
                       "moe_w_gate": [NE, D, DFF], "moe_w_up": [NE, D, DFF], "moe_w_down": [NE, DFF, D]}
        self.out = dt("out", [T, D], F32, kind="ExternalOutput").ap()
        self.xsT = dt("xsT", [D, T], F32).ap()
        self.xsT_v = self.xsT.rearrange("(c p) t -> p c t", p=128)
        self.qs = dt("qs", [D, T], BF16).ap()
        self.ks = dt("ks", [D, T], BF16).ap()
        self.vs = dt("vs", [T, D], BF16).ap()
        self.gs = dt("gs", [D, T], BF16).ap()
        self.os_ = dt("os", [D, T], BF16).ap()
        self.dbg_out = {}

    def wt(self, name, j):
        key = f"{name}_{j}"
        if key not in self.w:
            self.w[key] = self.nc.dram_tensor(key, self.wshape[name], F32, kind="ExternalInput").ap()
        return self.w[key]

    def sb(self, st, name, shape, dtype):
        self.uid = getattr(self, "uid", 0) + 1
        return st.enter_context(self.nc.sbuf_tensor(f"{name}_u{self.uid}", shape, dtype))

    def setup(self, st):
        nc = self.nc
        self.tk = tk = Tracker(nc, st)
        self.c32 = self.sb(st, "c32", [128, NCONST], F32)
        self.spt = self.sb(st, "spt", [128, NSP], F32)
        self.cbf = self.sb(st, "cbf", [128, NCONST], BF16)
        self.modL = self.sb(st, "mod", [128, 4, 48], F32)
        self.AL = self.sb(st, "modA", [128, 4, 2, 8], F32)
        self.cur = 0
        self.ps = [st.enter_context(nc.psum_tensor(f"ps{i}", [128, 512], F32)) for i in range(8)]
        self.Fk = self.sb(st, "Fk", [128, self.T // 128, 16], F32)
        self.Fref = self.sb(st, "Fref", [128, max(1, self.T // 512), 16], F32)
        self.bfb = self.sb(st, "bfb", [1, 32], BF16)
        tk.dma("sp", lambda e: e.dma_start(out=self.c32[:], in_=self.consts_d), writes=["c32"], ring="c")
        tk.dma("sp", lambda e: e.dma_start(out=self.spt[:], in_=self.sp_d), writes=["spt"], ring="c")
        tk.op("dve", lambda e: e.tensor_copy(self.cbf[:], self.c32[:]), reads=["c32"], writes=["cbf"])
        ob, _ = SP["bf"]
        tk.op("dve", lambda e: e.tensor_copy(self.bfb[:], self.spt[0:1, ob:ob + 32]), reads=["spt"], writes=["bfb"])
        if self.split_last:
            self.sel_sb = self.sb(st, "sel_sb", [1, 4], mybir.dt.int32)
            self.r_col = st.enter_context(nc.sync.register("r_col"))
            sem = tk._alloc("selsem")
            nc.sync.dma_start(out=self.sel_sb[:], in_=self.sel_d).then_inc(sem, 16)
            nc.sync.wait_ge(sem, 16)
            nc.sync.reg_load(self.r_col, self.sel_sb[0:1, 1:2])

    def spv(self, name):
        o, w = SP[name]
        return self.spt[:, o:o + w]

    def barrier(self):
        tk = self.tk
        tk.drain("sp")
        self.nc.all_engine_barrier()
        tk.last_w.clear()
        tk.readers.clear()

    def phase_in(self, barrier=True, body=None):
        tk, nc = self.tk, self.nc
        with ExitStack() as st:
            if body is not None:
                body(st)
            xin = [self.sb(st, f"xin{i}", [128, D], F32) for i in range(2)]
            xT = [self.sb(st, f"xTi{i}", [128, 8, 128], F32) for i in range(2)]
            ident = self.c32[:, C_ID:C_ID + 128]
            for tt in range(self.T // 128):
                b = tt % 2
                tk.dma("sp", lambda e: e.dma_start(out=xin[b][:], in_=self.x[tt * 128:(tt + 1) * 128, :]),
                       writes=[("xin", b)], ring="xin", nring=2)
                for half in range(2):
                    pb = (tt * 2 + half) % 6
                    for j in range(4):
                        c = half * 4 + j
                        tk.op("pe", lambda e: e.transpose(self.ps[pb][:, j * 128:(j + 1) * 128],
                                                          xin[b][:, c * 128:(c + 1) * 128], ident),
                              reads=[("xin", b), "c32"], writes=[("ps", pb)], signal=(j == 3))
                    src = self.ps[pb][:].rearrange("p (c t) -> p c t", c=4)
                    dst = xT[b][:, half * 4:(half + 1) * 4, :]
                    if half == 0:
                        tk.op("dve", lambda e: e.tensor_copy(dst, src), reads=[("ps", pb)], writes=[("xTi", b, half)])
                    else:
                        tk.op("act", lambda e: e.activation(out=dst, in_=src, func=AF.Copy),
                              reads=[("ps", pb)], writes=[("xTi", b, half)])
                tk.dma("sp", lambda e: e.dma_start(out=self.xsT_v[:, :, tt * 128:(tt + 1) * 128], in_=xT[b][:]),
                       reads=[("xTi", b, 0), ("xTi", b, 1)], writes=["xsT"], ring="xst", nring=2)
            if barrier:
                self.barrier()

    def phase_out(self):
        tk, nc = self.tk, self.nc
        with ExitStack() as st:
            xin = [self.sb(st, f"xo{i}", [128, D], F32) for i in range(2)]
            xT = [self.sb(st, f"xTo{i}", [128, 8, 128], F32) for i in range(2)]
            ident = self.c32[:, C_ID:C_ID + 128]
            for tt in range(self.T // 128):
                b = tt % 2
                tk.dma("sp", lambda e: e.dma_start(out=xT[b][:], in_=self.xsT_v[:, :, tt * 128:(tt + 1) * 128]),
                       reads=["xsT"], writes=[("xTo", b)], ring="xld", nring=2)
                for half in range(2):
                    pb = (tt * 2 + half) % 8
                    for j in range(4):
                        c = half * 4 + j
                        tk.op("pe", lambda e: e.transpose(self.ps[pb][:, j * 128:(j + 1) * 128], xT[b][:, c, :], ident),
                              reads=[("xTo", b), "c32"], writes=[("ps", pb)], signal=(j == 3))
                    dst = xin[b][:, half * 512:(half + 1) * 512]
                    if half == 0:
                        tk.op("dve", lambda e: e.tensor_copy(dst, self.ps[pb][:]), reads=[("ps", pb)], writes=[("xo", b, half)])
                    else:
                        tk.op("act", lambda e: e.activation(out=dst, in_=self.ps[pb][:], func=AF.Copy),
                              reads=[("ps", pb)], writes=[("xo", b, half)])
                tk.dma("sp", lambda e: e.dma_start(out=self.out[tt * 128:(tt + 1) * 128, :], in_=xin[b][:]),
                       reads=[("xo", b, 0), ("xo", b, 1)], writes=[("out", tt)], ring="ost", nring=2)
            self.barrier()

    def phase_mod(self, l):
        tk, nc = self.tk, self.nc
        self.cur = l
        if True:
            cond, wa = self.mod_bufs
            wv = self.wt("ada_w", l).rearrange("(k p) j -> p k j", p=128)
            pm = self.ps[7]
            for q in range(4):
                b = q % 2
                tk.dma("pool", lambda e: e.dma_start(out=wa[b][:], in_=wv[:, :, q * 1536:(q + 1) * 1536]),
                       writes=[("wa", b)], ring="wa", nring=2)
                for jc in range(12):
                    col = q * 12 + jc
                    for k in range(8):
                        tk.op("pe", lambda e: e.matmul(pm[:, col:col + 1], wa[b][:, k, jc * 128:(jc + 1) * 128],
                                                       cond[:, k:k + 1], start=(k == 0), stop=(k == 7)),
                              reads=[("wa", b), "cond"], writes=[("ps", 7)], signal=(k == 7 and jc == 11))
            o, _ = SP["adab"]
            tk.op("dve", lambda e: e.tensor_tensor(self.mod[:], pm[:, 0:48], self.spt[:, o + l * 48:o + (l + 1) * 48], ALU.add),
                  reads=[("ps", 7), "spt"], writes=["mod"])
            og, _ = SP["ngain"]
            for s in range(2):
                sc = self.mod[:, 8:16] if s == 0 else self.mod[:, 32:40]
                gain = self.spt[:, og + (l * 2 + s) * 8:og + (l * 2 + s + 1) * 8]
                tk.op("dve", lambda e: e.scalar_tensor_tensor(self.A[:, s, :], sc, 1.0, gain, ALU.add, ALU.mult),
                      reads=["mod", "spt"], writes=["modA"])

    @property
    def mod(self):
        return self.modL[:, self.cur, :]

    @property
    def A(self):
        return self.AL[:, self.cur, :, :]

    def shift(self, s):
        return self.mod[:, 0:8] if s == 0 else self.mod[:, 24:32]

    def gate(self, s):
        return self.mod[:, 16:24] if s == 0 else self.mod[:, 40:48]

    def norm_h(self, s, xT, xkey, n, sq, xr, rstd, outs, okeys, pbank, h32=None):
        tk = self.tk
        ones = self.cbf[:, C_ONES:C_ONES + 128]
        tk.op("act", lambda e: e.activation(out=sq[:, :, :n], in_=xT, func=AF.Square),
              reads=[xkey], writes=["sq"])
        for c in range(8):
            tk.op("pe", lambda e: e.matmul(self.ps[pbank][:, :n], ones, sq[:, c, :n], start=(c == 0), stop=(c == 7)),
                  reads=["sq", "cbf"], writes=[("ps", pbank)], signal=(c == 7))
        tk.op("act", lambda e: e.activation(out=rstd[:, :n], in_=self.ps[pbank][:, :n], func=AF.Sqrt, bias=EPS, scale=1.0 / D),
              reads=[("ps", pbank)], writes=["rstd"])
        tk.op("dve", lambda e: e.reciprocal(rstd[:, :n], rstd[:, :n]), reads=["rstd"], writes=["rstd"])
        rb = rstd[:, :n].unsqueeze(1).broadcast_to([128, 8, n])
        tk.op("dve", lambda e: e.tensor_tensor(xr[:, :, :n], xT, rb, ALU.mult),
              reads=[xkey, "rstd"], writes=["xr"])
        sh = self.shift(s)
        for c in range(8):
            dst = outs[c] if h32 is None else h32[:, c, :n]
            tk.op("act", lambda e: e.activation(out=dst, in_=xr[:, c, :n], func=AF.Identity,
                                                bias=sh[:, c:c + 1], scale=self.A[:, s, c:c + 1]),
                  reads=["xr", "mod", "modA"], writes=[okeys[c]] if h32 is None else [("h32", c)])
            if h32 is not None:
                tk.op("dve", lambda e: e.tensor_copy(outs[c], h32[:, c, :n]), reads=[("h32", c)], writes=[okeys[c]])

    def phase_ffn(self, l, moe, dyn=False):
        tk, nc, G = self.tk, self.nc, self.G
        j = l // 2
        NT = G // 512
        with ExitStack() as st:
            xT = self.sb(st, "f_xT", [128, 8, G], F32)
            hT = self.sb(st, "f_hT", [128, 8, G], BF16)
            wg = [self.sb(st, f"f_wg{i}", [128, 8, 512], BF16) for i in range(2)]
            wu = [self.sb(st, f"f_wu{i}", [128, 8, 512], BF16) for i in range(2)]
            wd = [self.sb(st, f"f_wd{i}", [128, 4, D], BF16) for i in range(2)]
            if moe:
                comb = self.sb(st, "f_comb", [128, G // 128, 8], F32)
                wr = self.spv("wr").rearrange("p (j k e) -> p j k e", j=2, k=8)
                obr, _ = SP["br"]
            g2 = self.gate(1)
            nexp = NE if moe else 1
            ident = self.c32[:, C_ID:C_ID + 128]
            ones32 = self.c32[:, C_ONES:C_ONES + 128]
            wi = 0
            for grp in range(1 if dyn else self.T // G):
                t0 = grp * G
                if dyn:
                    grp_ap = bass.AP(self.xsT.tensor, self.r_col, [[self.T, 128], [128 * self.T, 8], [1, G]])
                else:
                    grp_ap = self.xsT_v[:, :, t0:t0 + G]
                tk.dma("sp", lambda e: e.dma_start(out=xT[:], in_=grp_ap),
                       reads=["xsT"], writes=[("fx", tt) for tt in range(NT)], ring="fxl", nring=1)
                with ExitStack() as st2:
                    sq = self.sb(st2, "f_sq", [128, 8, 512], BF16)
                    xr = self.sb(st2, "f_xr", [128, 8, 512], F32)
                    rstd = self.sb(st2, "f_rstd", [128, 512], F32)
                    if moe:
                        lg = self.sb(st2, "f_lg", [128, 8], F32)
                        mx = self.sb(st2, "f_mx", [128, 8], F32)
                        tmp8 = self.sb(st2, "f_tmp8", [128, 8], F32)
                        msk = self.sb(st2, "f_msk", [128, 8], F32)
                        nl1 = self.sb(st2, "f_nl1", [128, 1], F32)
                        den = self.sb(st2, "f_den", [128, 1], F32)
                        h32 = xr
                    for tt in range(NT):
                        cs = slice(tt * 512, (tt + 1) * 512)
                        self.norm_h(1, xT[:, :, cs], ("fx", tt), 512, sq, xr, rstd,
                                    [hT[:, c, cs] for c in range(8)], [("fh", tt)] * 8, 7,
                                    h32=(h32 if moe else None))
                        if moe:
                            for s4 in range(4):
                                si = tt * 4 + s4
                                for k in range(8):
                                    tk.op("pe", lambda e: e.matmul(self.ps[6][:, 0:8], h32[:, k, s4 * 128:(s4 + 1) * 128],
                                                                   wr[:, j, k, :], start=(k == 0), stop=False),
                                          reads=[("h32", k), "spt"], writes=[("ps", 6)], signal=False)
                                tk.op("pe", lambda e: e.matmul(self.ps[6][:, 0:8], ones32[0:1, :],
                                                               self.spt[0:1, obr + j * 8:obr + (j + 1) * 8], start=False, stop=True),
                                      reads=["c32", "spt"], writes=[("ps", 6)])
                                tk.op("dve", lambda e: e.tensor_copy(lg[:], self.ps[6][:, 0:8]), reads=[("ps", 6)], writes=["lg"])
                                tk.op("dve", lambda e: e.max(mx[:], lg[:]), reads=["lg"], writes=["mx"])
                                tk.op("dve", lambda e: e.tensor_scalar(msk[:], lg[:], mx[:, 1:2], None, ALU.is_ge),
                                      reads=["lg", "mx"], writes=["msk"])
                                tk.op("dve", lambda e: e.tensor_scalar(nl1[:], mx[:, 0:1], -1.0, None, ALU.mult),
                                      reads=["mx"], writes=["nl1"])
                                tk.op("act", lambda e: e.activation(out=tmp8[:], in_=lg[:], func=AF.Exp, bias=nl1[:, 0:1], scale=1.0),
                                      reads=["lg", "nl1"], writes=["tmp8"])
                                tk.op("dve", lambda e: e.tensor_tensor(tmp8[:], tmp8[:], msk[:], ALU.mult),
                                      reads=["tmp8", "msk"], writes=["tmp8"])
                                tk.op("dve", lambda e: e.reduce_sum(den[:], tmp8[:], AX.X), reads=["tmp8"], writes=["den"])
                                tk.op("dve", lambda e: e.reciprocal(den[:], den[:]), reads=["den"], writes=["den"])
                                tk.op("dve", lambda e: e.tensor_scalar(comb[:, si, :], tmp8[:], den[:, 0:1], None, ALU.mult),
                                      reads=["tmp8", "den"], writes=["comb"])
                    self.barrier()
                with ExitStack() as st3:
                    hid = self.sb(st3, "f_hid", [128, 4, G], BF16)
                    sg = [self.sb(st3, f"f_sg{i}", [128, 512], F32) for i in range(2)]
                    if moe:
                        combB = [self.sb(st3, f"f_combB{i}", [128, G], F32) for i in range(2)]
                        dg = [self.sb(st3, f"f_dg{i}", [128, 128], F32) for i in range(2)]
                    for ex in range(nexp):
                        if moe:
                            cb = combB[ex % 2]
                            ckey = ("combB", ex % 2)
                            for si in range(G // 128):
                                dgt = dg[si % 2]
                                tk.op("dve", lambda e: e.tensor_scalar(dgt[:], ident, comb[:, si, ex:ex + 1], None, ALU.mult),
                                      reads=["comb", "c32"], writes=[("dg", si % 2)])
                                pb = 6 + si % 2
                                tk.op("pe", lambda e: e.matmul(self.ps[pb][:, 0:128], ones32, dgt[:], start=True, stop=True),
                                      reads=[("dg", si % 2), "c32"], writes=[("ps", pb)])
                                tk.op("act", lambda e: e.activation(out=cb[:, si * 128:(si + 1) * 128], in_=self.ps[pb][:, 0:128], func=AF.Copy),
                                      reads=[("ps", pb)], writes=[(ckey, si)])
                            Wg = self.wt("moe_w_gate", j)[ex]
                            Wu = self.wt("moe_w_up", j)[ex]
                            Wd = self.wt("moe_w_down", j)[ex]
                        else:
                            Wg = self.wt("ffn_w_gate", j)
                            Wu = self.wt("ffn_w_up", j)
                            Wd = self.wt("ffn_w_down", j)
                        Wg = Wg.rearrange("(k p) f -> p k f", p=128)
                        Wu = Wu.rearrange("(k p) f -> p k f", p=128)
                        Wd = Wd.rearrange("(k p) o -> p k o", p=128)
                        for fc in range(DFF // 512):
                            b = wi % 2
                            wi += 1
                            fs = slice(fc * 512, (fc + 1) * 512)
                            tk.dma("pool", lambda e: e.dma_start(out=wg[b][:], in_=Wg[:, :, fs]), writes=[("wg", b)], ring="wg", nring=2)
                            tk.dma("pool", lambda e: e.dma_start(out=wu[b][:], in_=Wu[:, :, fs]), writes=[("wu", b)], ring="wu", nring=2)
                            tk.dma("pool", lambda e: e.dma_start(out=wd[b][:], in_=Wd[:, fc * 4:(fc + 1) * 4, :]), writes=[("wd", b)], ring="wd", nring=2)
                            it = 0
                            for ft in range(4):
                                for tt in range(NT):
                                    cs = slice(tt * 512, (tt + 1) * 512)
                                    pg, pu = (0, 1) if it % 2 == 0 else (2, 3)
                                    sgt = sg[it % 2]
                                    skey = ("sg", it % 2)
                                    it += 1
                                    for k in range(8):
                                        tk.op("pe", lambda e: e.matmul(self.ps[pg][:], wg[b][:, k, ft * 128:(ft + 1) * 128], hT[:, k, cs],
                                                                       start=(k == 0), stop=(k == 7)),
                                              reads=[("wg", b), ("fh", tt)], writes=[("ps", pg)], signal=(k == 7))
                                    for k in range(8):
                                        tk.op("pe", lambda e: e.matmul(self.ps[pu][:], wu[b][:, k, ft * 128:(ft + 1) * 128], hT[:, k, cs],
                                                                       start=(k == 0), stop=(k == 7)),
                                              reads=[("wu", b), ("fh", tt)], writes=[("ps", pu)], signal=(k == 7))
                                    tk.op("act", lambda e: e.activation(out=sgt[:], in_=self.ps[pg][:], func=AF.Silu),
                                          reads=[("ps", pg)], writes=[skey])
                                    if moe:
                                        tk.op("dve", lambda e: e.tensor_tensor(sgt[:], sgt[:], self.ps[pu][:], ALU.mult),
                                              reads=[skey, ("ps", pu)], writes=[skey])
                                        tk.op("dve", lambda e: e.tensor_tensor(hid[:, ft, cs], sgt[:], cb[:, cs], ALU.mult),
                                              reads=[skey] + [(ckey, tt * 4 + q4) for q4 in range(4)], writes=[("hid", ft, tt)])
                                    else:
                                        tk.op("dve", lambda e: e.tensor_tensor(hid[:, ft, cs], sgt[:], self.ps[pu][:], ALU.mult),
                                              reads=[skey, ("ps", pu)], writes=[("hid", ft, tt)])
                            it = 0
                            for oc in range(8):
                                for tt in range(NT):
                                    cs = slice(tt * 512, (tt + 1) * 512)
                                    py = 4 + it % 2
                                    it += 1
                                    for ft in range(4):
                                        tk.op("pe", lambda e: e.matmul(self.ps[py][:], wd[b][:, ft, oc * 128:(oc + 1) * 128], hid[:, ft, cs],
                                                                       start=(ft == 0), stop=(ft == 3)),
                                              reads=[("wd", b), ("hid", ft, tt)], writes=[("ps", py)], signal=(ft == 3))
                                    tk.op("dve", lambda e: e.scalar_tensor_tensor(xT[:, oc, cs], self.ps[py][:], g2[:, oc:oc + 1], xT[:, oc, cs],
                                                                                  ALU.mult, ALU.add),
                                          reads=[("ps", py), ("fx", tt), "mod"], writes=[("fx", tt)])
                    tk.dma("sp", lambda e: e.dma_start(out=grp_ap, in_=xT[:]),
                           reads=[("fx", tt) for tt in range(NT)], writes=["xsT"], ring="fxs", nring=1)
                    self.barrier()

    def out_proj_residual(self, wo, gated, gkey, xT, xkey, n, banks=(0, 1)):
        tk = self.tk
        g1 = self.gate(0)
        for oc in range(8):
            pb = banks[oc % len(banks)]
            for k in range(8):
                tk.op("pe", lambda e: e.matmul(self.ps[pb][:, :n], wo[:, k, oc * 128:(oc + 1) * 128], gated[:, k, :n],
                                               start=(k == 0), stop=(k == 7)),
                      reads=["wo", gkey], writes=[("ps", pb)], signal=(k == 7))
            tk.op("dve", lambda e: e.scalar_tensor_tensor(xT[:, oc, :n], self.ps[pb][:, :n], g1[:, oc:oc + 1], xT[:, oc, :n],
                                                          ALU.mult, ALU.add),
                  reads=[("ps", pb), xkey, "mod"], writes=[xkey])

    def load_w(self, dst, src_v, key, ncol, piece=1024):
        for c0 in range(0, ncol, piece):
            c1 = min(ncol, c0 + piece)
            self.tk.dma("pool", lambda e: e.dma_start(out=dst[:, :, c0:c1], in_=src_v[:, :, c0:c1]),
                        writes=[key], ring="wload", nring=4)

    def phase_gla(self, l):
        tk, nc, T = self.tk, self.nc, self.T
        j = l // 2
        TG, NS = 256, 2
        with ExitStack() as st:
            w_in = self.sb(st, "g_win", [128, 8, 3088], BF16)
            w_out = self.sb(st, "g_wout", [128, 8, D], BF16)
            xT = self.sb(st, "g_xT", [128, 8, TG], F32)
            bufA = self.sb(st, "g_bufA", [128, 8, TG], F32)
            sq = self.sb(st, "g_sq", [128, 8, TG], BF16)
            hT = self.sb(st, "g_hT", [128, 8, TG], BF16)
            rstd = self.sb(st, "g_rstd", [128, 512], F32)
            qkT = self.sb(st, "g_qkT", [128, 8, TG], F32)
            rT = self.sb(st, "g_rT", [128, 8, TG], BF16)
            glow = self.sb(st, "g_glow", [32, TG], F32)
            v = self.sb(st, "g_v", [128, NS, D], BF16)
            ktok = self.sb(st, "g_ktok", [128, NS, 512], F32)
            az_ = [self.sb(st, f"g_az{i}", [128, 512], F32) for i in range(NS)]
            zs_ = [self.sb(st, f"g_zs{i}", [128, 512], F32) for i in range(NS)]
            la_ = [self.sb(st, f"g_la{i}", [128, 512], F32) for i in range(NS)]
            Eq_ = [self.sb(st, f"g_Eq{i}", [128, 512], F32) for i in range(NS)]
            Ek_ = [self.sb(st, f"g_Ek{i}", [128, 512], F32) for i in range(NS)]
            Eend_ = [self.sb(st, f"g_Eend{i}", [128, 512], F32) for i in range(NS)]
            kend = self.sb(st, "g_kend", [128, 512], BF16)
            qd = self.sb(st, "g_qd", [128, 4, 128], BF16)
            ki = self.sb(st, "g_ki", [128, 4, 128], BF16)
            am = self.sb(st, "g_am", [128, 4, 128], BF16)
            S = self.sb(st, "g_S", [128, 4, 256], F32)
            Sbf = self.sb(st, "g_Sbf", [128, 4, 256], BF16)
            rso = self.sb(st, "g_rso", [128, 4, TG], F32)
            tmpo = self.sb(st, "g_tmpo", [128, TG], F32)
            ps = self.ps
            self.load_w(w_in, self.wt("gla_w_in", j).rearrange("(k p) f -> p k f", p=128), "win", 3088, 772)
            self.load_w(w_out, self.wt("gla_w_out", j).rearrange("(k p) f -> p k f", p=128), "wo", D, 512)
            tk.op("dve", lambda e: e.memset(S[:], 0.0), writes=["S"])
            tk.op("dve", lambda e: e.memset(Sbf[:], 0.0), writes=["Sbf"])
            tk.op("dve", lambda e: e.memset(glow[:], 1.0), writes=["glow"])
            ow, _ = SP["wgu"]
            wgu = self.spt[0:17, ow + j * 512:ow + (j + 1) * 512]
            oo, _ = SP["onorm"]
            onorm = self.spt[:, oo + j * 2:oo + j * 2 + 2]
            tri = self.c32[:, C_TRI:C_TRI + 128]
            rev = self.c32[:, C_REV:C_REV + 128]
            ones = self.cbf[:, C_ONES:C_ONES + 128]
            maskb = self.c32[:, C_MASK:C_MASK + 128].unsqueeze(1).broadcast_to([128, 4, 128])
            oT = bufA
            gated = hT
            ev = 0
            for ti in range(T // TG):
                t0 = ti * TG
                tk.dma("sp", lambda e: e.dma_start(out=xT[:], in_=self.xsT_v[:, :, t0:t0 + TG]),
                       reads=["xsT"], writes=["gx"], ring="gxl", nring=1)
                self.norm_h(0, xT[:], "gx", TG, sq, bufA, rstd, [hT[:, c, :] for c in range(8)], ["gh"] * 8, 7)
                for k in range(8):
                    tk.op("pe", lambda e: e.matmul(ps[7][0:16, :TG], w_in[:, k, 3072:3088], hT[:, k, :],
                                                   start=(k == 0), stop=(k == 7)),
                          reads=["win", "gh"], writes=[("ps", 7)], signal=(k == 7))
                tk.op("dve", lambda e: e.tensor_copy(glow[0:16, :], ps[7][0:16, :TG]), reads=[("ps", 7)], writes=["glow"])
                groups = []

                def g_qk(oc):
                    pb = oc % 2
                    for k in range(8):
                        tk.op("pe", lambda e: e.matmul(ps[pb][:, :TG], w_in[:, k, oc * 128:(oc + 1) * 128], hT[:, k, :],
                                                       start=(k == 0), stop=(k == 7)),
                              reads=["win", "gh"], writes=[("ps", pb)], signal=(k == 7))
                    if oc % 2 == 0:
                        tk.op("act", lambda e: e.activation(out=qkT[:, oc, :], in_=ps[pb][:, :TG], func=AF.Copy),
                              reads=[("ps", pb)], writes=[("qk", oc)])
                    else:
                        tk.op("dve", lambda e: e.tensor_copy(qkT[:, oc, :], ps[pb][:, :TG]),
                              reads=[("ps", pb)], writes=[("qk", oc)])

                def g_r(oc):
                    pb = 2 + oc % 2
                    for k in range(8):
                        tk.op("pe", lambda e: e.matmul(ps[pb][:, :TG], w_in[:, k, 2048 + oc * 128:2048 + (oc + 1) * 128], hT[:, k, :],
                                                       start=(k == 0), stop=(k == 7)),
                              reads=["win", "gh"], writes=[("ps", pb)], signal=(k == 7))
                    tk.op("act", lambda e: e.activation(out=rT[:, oc, :], in_=ps[pb][:, :TG], func=AF.Silu),
                          reads=[("ps", pb)], writes=[("rT", oc)])

                def g_v(s, half):
                    tok = slice(s * 128, (s + 1) * 128)
                    pb = half
                    for k in range(8):
                        tk.op("pe", lambda e: e.matmul(ps[pb][:], hT[:, k, tok], w_in[:, k, 1024 + half * 512:1536 + half * 512],
                                                       start=(k == 0), stop=(k == 7)),
                              reads=["win", "gh"], writes=[("ps", pb)], signal=(k == 7))
                    if half == 0:
                        tk.op("act", lambda e: e.activation(out=v[:, s, 0:512], in_=ps[pb][:], func=AF.Copy),
                              reads=[("ps", pb)], writes=[("v", s, 0)])
                    else:
                        tk.op("dve", lambda e: e.tensor_copy(v[:, s, 512:1024], ps[pb][:]),
                              reads=[("ps", pb)], writes=[("v", s, 1)])

                def g_kt(s):
                    tok = slice(s * 128, (s + 1) * 128)
                    pb = 2 + s % 2
                    for k in range(8):
                        tk.op("pe", lambda e: e.matmul(ps[pb][:], hT[:, k, tok], w_in[:, k, 512:1024],
                                                       start=(k == 0), stop=(k == 7)),
                              reads=["win", "gh"], writes=[("ps", pb)], signal=(k == 7))
                    tk.op("dve", lambda e: e.tensor_copy(ktok[:, s, :], ps[pb][:]), reads=[("ps", pb)], writes=[("ktok", s)])

                for oc in range(8):
                    groups.append(lambda oc=oc: g_qk(oc))
                for s in range(NS):
                    groups.append(lambda s=s: g_v(s, 0))
                    groups.append(lambda s=s: g_v(s, 1))
                    groups.append(lambda s=s: g_kt(s))
                for oc in range(8):
                    groups.append(lambda oc=oc: g_r(oc))
                chain = []
                for s in range(NS):
                    tok = slice(s * 128, (s + 1) * 128)
                    zs, az, la, Eq, Ek, Eend = zs_[s], az_[s], la_[s], Eq_[s], Ek_[s], Eend_[s]
                    chain += [
                        lambda s=s, tok=tok: tk.op("pe", lambda e: e.matmul(ps[4][:], glow[0:17, tok], wgu, start=True, stop=True),
                                                   reads=["glow", "spt"], writes=[("ps", 4)]),
                        lambda s=s, zs=zs: tk.op("act", lambda e: e.activation(out=zs[:], in_=ps[4][:], func=AF.Copy),
                                                 reads=[("ps", 4)], writes=[("zs", s)]),
                        lambda s=s, zs=zs, az=az: tk.op("dve", lambda e: e.scalar_tensor_tensor(az[:], zs[:], -1.0, zs[:], ALU.mult, ALU.min),
                                                        reads=[("zs", s)], writes=[("az", s)]),
                        lambda s=s, az=az: tk.op("act", lambda e: e.activation(out=az[:], in_=az[:], func=AF.Exp),
                                                 reads=[("az", s)], writes=[("az", s)]),
                        lambda s=s, az=az: tk.op("act", lambda e: e.activation(out=az[:], in_=az[:], func=AF.Ln, bias=1.0, scale=1.0),
                                                 reads=[("az", s)], writes=[("az", s)]),
                        lambda s=s, zs=zs, az=az, la=la: tk.op("dve", lambda e: e.scalar_tensor_tensor(la[:], zs[:], 0.0, az[:], ALU.min, ALU.subtract),
                                                               reads=[("zs", s), ("az", s)], writes=[("la", s)]),
                        lambda s=s, la=la: [tk.op("pe", lambda e: e.matmul(ps[5][:, h * 128:(h + 1) * 128], la[:, h * 128:(h + 1) * 128], tri,
                                                                            start=True, stop=True),
                                                  reads=[("la", s), "c32"], writes=[("ps", 5)], signal=(h == 3)) for h in range(4)],
                        lambda s=s, Eq=Eq: tk.op("act", lambda e: e.activation(out=Eq[:], in_=ps[5][:], func=AF.Exp),
                                                 reads=[("ps", 5)], writes=[("Eq", s)]),
                        lambda s=s, Ek=Ek: tk.op("act", lambda e: e.activation(out=Ek[:], in_=ps[5][:], func=AF.Exp, scale=-1.0),
                                                 reads=[("ps", 5)], writes=[("Ek", s)]),
                        lambda s=s, la=la: tk.op("pe", lambda e: e.matmul(ps[4][:], rev, la[:], start=True, stop=True),
                                                 reads=[("la", s), "c32"], writes=[("ps", 4)]),
                        lambda s=s, Eend=Eend: tk.op("act", lambda e: e.activation(out=Eend[:], in_=ps[4][:], func=AF.Exp),
                                                     reads=[("ps", 4)], writes=[("Eend", s)]),
                    ]
                for i in range(max(len(groups), len(chain))):
                    if i < len(groups):
                        groups[i]()
                    if i < len(chain):
                        chain[i]()
                for s in range(NS):
                    tok = slice(s * 128, (s + 1) * 128)
                    Eq, Ek, Eend = Eq_[s], Ek_[s], Eend_[s]
                    tk.op("dve", lambda e: e.tensor_tensor(kend[:], ktok[:, s, :], Eend[:], ALU.mult),
                          reads=[("ktok", s), ("Eend", s)], writes=["kend"])
                    tk.op("dve", lambda e: e.scalar_tensor_tensor(qd[:], qkT[:, 0:4, tok], 128.0 ** -0.5,
                                                                  Eq[:].rearrange("p (h t) -> p h t", h=4), ALU.mult, ALU.mult),
                          reads=[("qk", c) for c in range(4)] + [("Eq", s)], writes=["qd"])
                    tk.op("dve", lambda e: e.tensor_tensor(ki[:], qkT[:, 4:8, tok], Ek[:].rearrange("p (h t) -> p h t", h=4), ALU.mult),
                          reads=[("qk", c) for c in range(4, 8)] + [("Ek", s)], writes=["ki"])
                    for h in range(4):
                        tk.op("pe", lambda e: e.matmul(ps[6][:, h * 128:(h + 1) * 128], ki[:, h, :], qd[:, h, :], start=True, stop=True),
                              reads=["ki", "qd"], writes=[("ps", 6)], signal=(h == 3))
                    tk.op("dve", lambda e: e.tensor_tensor(am[:], ps[6][:].rearrange("p (h t) -> p h t", h=4), maskb, ALU.mult),
                          reads=[("ps", 6), "c32"], writes=["am"])
                    for c in range(8):
                        h, jj = c // 2, c % 2
                        pb = c // 4
                        col = slice((c % 4) * 128, (c % 4 + 1) * 128)
                        tk.op("pe", lambda e: e.matmul(ps[pb][:, col], v[:, s, c * 128:(c + 1) * 128], am[:, h, :], start=True, stop=False),
                              reads=[("v", s, c // 4), "am"], writes=[("ps", pb)], signal=False)
                        tk.op("pe", lambda e: e.matmul(ps[pb][:, col], Sbf[:, h, jj * 128:(jj + 1) * 128], qd[:, h, :], start=False, stop=True),
                              reads=["Sbf", "qd"], writes=[("ps", pb)], signal=(c % 4 == 3))
                    tk.op("act", lambda e: e.activation(out=oT[:, 0:4, tok], in_=ps[0][:].rearrange("p (c t) -> p c t", c=4), func=AF.Copy),
                          reads=[("ps", 0)], writes=["oT"])
                    tk.op("dve", lambda e: e.tensor_copy(oT[:, 4:8, tok], ps[1][:].rearrange("p (c t) -> p c t", c=4)),
                          reads=[("ps", 1)], writes=["oT"])
                    for h in range(4):
                        pb = 2 + h // 2
                        col = slice((h % 2) * 256, (h % 2 + 1) * 256)
                        tk.op("pe", lambda e: e.matmul(ps[pb][:, col], kend[:, h * 128:(h + 1) * 128], v[:, s, h * 256:(h + 1) * 256],
                                                       start=True, stop=True),
                              reads=["kend", ("v", s, h // 2)], writes=[("ps", pb)], signal=(h % 2 == 1))
                    for h in range(4):
                        pb = 2 + h // 2
                        col = slice((h % 2) * 256, (h % 2 + 1) * 256)
                        tk.op("dve", lambda e: e.scalar_tensor_tensor(S[:, h, :], S[:, h, :], Eq[:, h * 128 + 127:h * 128 + 128], ps[pb][:, col],
                                                                      ALU.mult, ALU.add),
                              reads=["S", ("Eq", s), ("ps", pb)], writes=["S"])
                    tk.op("act", lambda e: e.activation(out=Sbf[:], in_=S[:], func=AF.Copy), reads=["S"], writes=["Sbf"])
                tk.op("act", lambda e: e.activation(out=sq[:], in_=oT[:], func=AF.Square), reads=["oT"], writes=["sq"])
                for h in range(4):
                    pb = 6 + h // 2
                    col = slice((h % 2) * TG, (h % 2 + 1) * TG)
                    for jj in range(2):
                        tk.op("pe", lambda e: e.matmul(ps[pb][:, col], ones, sq[:, 2 * h + jj, :], start=(jj == 0), stop=(jj == 1)),
                              reads=["sq", "cbf"], writes=[("ps", pb)], signal=(jj == 1 and h % 2 == 1))
                for hb in range(2):
                    tk.op("act", lambda e: e.activation(out=rso[:, 2 * hb:2 * hb + 2, :],
                                                        in_=ps[6 + hb][:, :2 * TG].rearrange("p (h t) -> p h t", h=2),
                                                        func=AF.Sqrt, bias=EPS, scale=1.0 / 256),
                          reads=[("ps", 6 + hb)], writes=["rso"])
                tk.op("dve", lambda e: e.reciprocal(rso[:], rso[:]), reads=["rso"], writes=["rso"])
                for c in range(8):
                    h, jj = c // 2, c % 2
                    tk.op("dve", lambda e: e.scalar_tensor_tensor(tmpo[:], oT[:, c, :], onorm[:, jj:jj + 1], rso[:, h, :], ALU.mult, ALU.mult),
                          reads=["oT", "rso", "spt"], writes=["tmpo"])
                    tk.op("dve", lambda e: e.tensor_tensor(gated[:, c, :], tmpo[:], rT[:, c, :], ALU.mult),
                          reads=["tmpo", ("rT", c), "gh"], writes=["gh"])
                self.out_proj_residual(w_out, gated, "gh", xT, "gx", TG, banks=(0, 1))
                tk.dma("sp", lambda e: e.dma_start(out=self.xsT_v[:, :, t0:t0 + TG], in_=xT[:]),
                       reads=["gx"], writes=["xsT"], ring="gxs", nring=1)
            self.barrier()

    def phase_fox(self, l):
        self.phase_fox_proj(l)
        self.phase_fox_attn(l)
        self.phase_fox_out(l)

    def phase_fox_proj(self, l):
        tk, nc, T = self.tk, self.nc, self.T
        j = l // 2
        TF = 512
        with ExitStack() as st:
            w_in = self.sb(st, "p_win", [128, 8, 4112], BF16)
            xT = self.sb(st, "p_xT", [128, 8, TF], F32)
            xr = self.sb(st, "p_xr", [128, 8, TF], F32)
            sq = self.sb(st, "p_sq", [128, 8, TF], BF16)
            hT = self.sb(st, "p_hT", [128, 8, TF], BF16)
            rstd = self.sb(st, "p_rstd", [128, 512], F32)
            qf = self.sb(st, "p_qf", [128, 4, TF], F32)
            qsq = self.sb(st, "p_qsq", [128, 4, TF], BF16)
            rs = self.sb(st, "p_rs", [128, 4, TF], F32)
            qn = [self.sb(st, f"p_qn{i}", [128, 4, TF], BF16) for i in range(2)]
            vt = self.sb(st, "p_vt", [128, 4, D], BF16)
            sg = self.sb(st, "p_sg", [128, 8, TF], BF16)
            zs = self.sb(st, "p_zs", [128, 16], F32)
            az = self.sb(st, "p_az", [128, 16], F32)
            lf = self.sb(st, "p_lf", [128, 16], F32)
            carry = self.sb(st, "p_carry", [128, 16], F32)
            ps = self.ps
            self.load_w(w_in, self.wt("fox_w_in", j).rearrange("(k p) f -> p k f", p=128), "win", 4112, 1028)
            tk.op("dve", lambda e: e.memset(carry[:], 0.0), writes=["carry"])
            ob, _ = SP["bf"]
            bfB = self.sb(st, "p_bfB", [128, 16], F32)
            tk.op("pe", lambda e: e.matmul(self.ps[6][:, 0:16], self.c32[0:1, C_ONES:C_ONES + 128],
                                           self.spt[0:1, ob + j * 16:ob + (j + 1) * 16], start=True, stop=True),
                  reads=["c32", "spt"], writes=[("ps", 6)])
            tk.op("dve", lambda e: e.tensor_copy(bfB[:], self.ps[6][:, 0:16]), reads=[("ps", 6)], writes=["bfB"])
            bd = self.cbf[:, C_BD:C_BD + 128]
            cm = self.c32[:, C_MASK:C_MASK + 128]
            ones32 = self.c32[:, C_ONES:C_ONES + 128]
            ob, _ = SP["bf"]
            bfr = self.spt[0:1, ob + j * 16:ob + (j + 1) * 16]
            oq, _ = SP["qn"]
            okn, _ = SP["kn"]
            qs_v = self.qs.rearrange("(c p) t -> p c t", p=128)
            ks_v = self.ks.rearrange("(c p) t -> p c t", p=128)
            gs_v = self.gs.rearrange("(c p) t -> p c t", p=128)
            vs_v = self.vs.rearrange("(n p) d -> p n d", p=128)
            gi = 0
            for ti in range(T // TF):
                t0 = ti * TF
                tk.dma("sp", lambda e: e.dma_start(out=xT[:], in_=self.xsT_v[:, :, t0:t0 + TF]),
                       reads=["xsT"], writes=["px"], ring="pxl", nring=1)
                self.norm_h(0, xT[:], "px", TF, sq, xr, rstd, [hT[:, c, :] for c in range(8)], ["ph"] * 8, 7)
                for grp in range(4):
                    isk = grp >= 2
                    base = (1024 if isk else 0) + (grp % 2) * 512
                    gain = self.spt[:, (okn if isk else oq) + j:(okn if isk else oq) + j + 1]
                    for c in range(4):
                        for k in range(8):
                            tk.op("pe", lambda e: e.matmul(ps[c][:], w_in[:, k, base + c * 128:base + (c + 1) * 128], hT[:, k, :],
                                                           start=(k == 0), stop=(k == 7)),
                                  reads=["win", "ph"], writes=[("ps", c)], signal=(k == 7))
                        tk.op("act", lambda e: e.activation(out=qf[:, c, :], in_=ps[c][:], func=AF.Copy),
                              reads=[("ps", c)], writes=["qf"])
                    tk.op("dve", lambda e: e.tensor_tensor(qsq[:], qf[:], qf[:], ALU.mult), reads=["qf"], writes=["qsq"])
                    for c in range(4):
                        tk.op("pe", lambda e: e.matmul(ps[4 + c][:], bd, qsq[:, c, :], start=True, stop=True),
                              reads=["qsq", "cbf"], writes=[("ps", 4 + c)])
                        tk.op("act", lambda e: e.activation(out=rs[:, c, :], in_=ps[4 + c][:], func=AF.Ln, bias=EPS, scale=1.0 / 64),
                              reads=[("ps", 4 + c)], writes=["rs"])
                    tk.op("act", lambda e: e.activation(out=rs[:], in_=rs[:], func=AF.Exp, scale=-0.5), reads=["rs"], writes=["rs"])
                    qb_ = qn[gi % 2]
                    qkey = ("qn", gi % 2)
                    gi += 1
                    tk.op("dve", lambda e: e.scalar_tensor_tensor(qb_[:], qf[:], gain, rs[:], ALU.mult, ALU.mult),
                          reads=["qf", "rs", "spt"], writes=[qkey])
                    dst = (ks_v if isk else qs_v)[:, (grp % 2) * 4:(grp % 2) * 4 + 4, t0:t0 + TF]
                    tk.dma("sp", lambda e: e.dma_start(out=dst, in_=qb_[:]), reads=[qkey], writes=["ks" if isk else "qs"],
                           ring="pqs", nring=2)
                for oc in range(8):
                    pb = oc % 4
                    for k in range(8):
                        tk.op("pe", lambda e: e.matmul(ps[pb][:], w_in[:, k, 3072 + oc * 128:3072 + (oc + 1) * 128], hT[:, k, :],
                                                       start=(k == 0), stop=(k == 7)),
                              reads=["win", "ph"], writes=[("ps", pb)], signal=(k == 7))
                    tk.op("act", lambda e: e.activation(out=sg[:, oc, :], in_=ps[pb][:], func=AF.Sigmoid),
                          reads=[("ps", pb)], writes=["sg"])
                tk.dma("sp", lambda e: e.dma_start(out=gs_v[:, :, t0:t0 + TF], in_=sg[:]), reads=["sg"], writes=["gs"],
                       ring="pgs", nring=1)
                for s in range(4):
                    tok = slice(s * 128, (s + 1) * 128)
                    for half in range(2):
                        pb = 4 + half
                        for k in range(8):
                            tk.op("pe", lambda e: e.matmul(ps[pb][:], hT[:, k, tok], w_in[:, k, 2048 + half * 512:2560 + half * 512],
                                                           start=(k == 0), stop=(k == 7)),
                                  reads=["win", "ph"], writes=[("ps", pb)], signal=(k == 7))
                        if half == 0:
                            tk.op("act", lambda e: e.activation(out=vt[:, s, 0:512], in_=ps[pb][:], func=AF.Copy),
                                  reads=[("ps", pb)], writes=["vt"])
                        else:
                            tk.op("dve", lambda e: e.tensor_copy(vt[:, s, 512:1024], ps[pb][:]), reads=[("ps", pb)], writes=["vt"])
                    for k in range(8):
                        tk.op("pe", lambda e: e.matmul(ps[6][:, 0:16], hT[:, k, tok], w_in[:, k, 4096:4112], start=(k == 0), stop=(k == 7)),
                              reads=["win", "ph"], writes=[("ps", 6)], signal=(k == 7))
                    tk.op("dve", lambda e: e.tensor_tensor(zs[:], ps[6][:, 0:16], bfB[:], ALU.add), reads=[("ps", 6), "bfB"], writes=["pzs"])
                    tk.op("dve", lambda e: e.scalar_tensor_tensor(az[:], zs[:], -1.0, zs[:], ALU.mult, ALU.min), reads=["pzs"], writes=["paz"])
                    tk.op("act", lambda e: e.activation(out=az[:], in_=az[:], func=AF.Exp), reads=["paz"], writes=["paz"])
                    tk.op("act", lambda e: e.activation(out=az[:], in_=az[:], func=AF.Ln, bias=1.0, scale=1.0), reads=["paz"], writes=["paz"])
                    tk.op("dve", lambda e: e.scalar_tensor_tensor(lf[:], zs[:], 0.0, az[:], ALU.min, ALU.subtract),
                          reads=["pzs", "paz"], writes=["lf"])
                    gt = ti * 4 + s
                    tk.op("pe", lambda e: e.matmul(ps[6][:, 16:32], cm, lf[:], start=True, stop=True),
                          reads=["lf", "c32"], writes=[("ps", 6)], signal=False)
                    tk.op("pe", lambda e: e.matmul(ps[6][:, 32:48], ones32, lf[:], start=True, stop=True),
                          reads=["lf", "c32"], writes=[("ps", 6)])
                    tk.op("dve", lambda e: e.tensor_tensor(self.Fk[:, gt, :], ps[6][:, 16:32], carry[:], ALU.add),
                          reads=[("ps", 6), "carry"], writes=["Fk"])
                    tk.op("dve", lambda e: e.tensor_tensor(carry[:], ps[6][:, 32:48], carry[:], ALU.add),
                          reads=[("ps", 6), "carry"], writes=["carry"])
                    if s == 1:
                        tk.op("dve", lambda e: e.tensor_copy(self.Fref[:, ti, :], carry[:]), reads=["carry"], writes=["Fref"])
                tk.dma("sp", lambda e: e.dma_start(out=vs_v[:, ti * 4:ti * 4 + 4, :], in_=vt[:]), reads=["vt"], writes=["vs"],
                       ring="pvs", nring=1)
            self.barrier()

    def phase_fox_attn(self, l):
        tk, nc, T = self.tk, self.nc, self.T
        NQB, NKT = T // 512, T // 128
        with ExitStack() as st:
            QhT = [self.sb(st, f"a_q{i}", [128, T], BF16) for i in range(2)]
            KhT = [self.sb(st, f"a_k{i}", [128, T], BF16) for i in range(2)]
            Vh = [self.sb(st, f"a_v{i}", [128, NKT, 128], BF16) for i in range(2)]
            Gh = [self.sb(st, f"a_g{i}", [64, T], BF16) for i in range(2)]
            bias = self.sb(st, "a_bias", [128, NQB, NKT, 16], F32)
            SB = [0, 1, 2, 3, 7]
            Pt = [self.sb(st, f"a_P{i}", [128, 512], BF16) for i in range(len(SB))]
            rl = self.sb(st, "a_rl", [65, 512], F32)
            bcs = self.sb(st, "a_bcs", [64, 512], F32)
            t1 = self.sb(st, "a_t1", [64, 512], F32)
            og = [self.sb(st, f"a_og{i}", [64, 512], BF16) for i in range(2)]
            ps = self.ps
            cmb = self.cbf[:, C_MASK:C_MASK + 128]
            vs_v = self.vs.rearrange("(n p) d -> p n d", p=128)
            for i in range(2):
                tk.op("dve", lambda e: e.memset(Vh[i][:], 1.0), writes=[("Vh", i)])
                tk.op("dve", lambda e: e.memset(QhT[i][64:128, :], 0.0), writes=[("Qh", i)])
                tk.op("dve", lambda e: e.memset(KhT[i][64:128, :], 0.0), writes=[("Kh", i)])
            for qb in range(NQB):
                nk = 4 * qb + 4
                tk.op("dve", lambda e: e.tensor_tensor(bias[:, qb, 0:nk, :],
                                                       self.Fref[:, qb, :].unsqueeze(1).broadcast_to([128, nk, 16]),
                                                       self.Fk[:, 0:nk, :], ALU.subtract),
                      reads=["Fref", "Fk"], writes=["bias"])
            L = 4
            items = [(h, qb, kt) for h in range(16) for qb in range(NQB) for kt in range(4 * qb + 4)]
            head_start = {}
            for i, (h, qb, kt) in enumerate(items):
                head_start.setdefault(h, i)

            def loads(h):
                b = h % 2
                hs = slice(h * 64, (h + 1) * 64)
                q = "pool"
                tk.dma(q, lambda e: e.dma_start(out=QhT[b][0:64, :], in_=self.qs[hs, :]), reads=["qs"], writes=[("Qh", b)], ring="aq", nring=2)
                tk.dma(q, lambda e: e.dma_start(out=KhT[b][0:64, :], in_=self.ks[hs, :]), reads=["ks"], writes=[("Kh", b)], ring="ak", nring=2)
                for n0 in range(0, NKT, 8):
                    n1 = min(NKT, n0 + 8)
                    tk.dma(q, lambda e: e.dma_start(out=Vh[b][:, n0:n1, 0:64], in_=vs_v[:, n0:n1, hs]), reads=["vs"], writes=[("Vh", b)],
                           ring="av", nring=8)
                tk.dma(q, lambda e: e.dma_start(out=Gh[b][:], in_=self.gs[hs, :]), reads=["gs"], writes=[("Gh", b)], ring="ag", nring=2)

            later = []

            def tick():
                for ent in later:
                    ent[0] -= 1
                while later and later[0][0] <= 0:
                    later.pop(0)[1]()

            def emit_pv(i):
                h, qb, kt = items[i]
                b = h % 2
                nk = 4 * qb + 4
                blk = h * NQB + qb
                po = 4 + blk % 2
                pb = i % len(SB)
                P = Pt[pb]
                c0 = max(0, kt - 4 * qb) * 128
                tk.op("pe", lambda e: e.matmul(ps[po][:, c0:512], Vh[b][:, kt, :], P[:, c0:512],
                                               start=(kt == 0), stop=(kt == nk - 1)),
                      reads=[("Vh", b), ("P", pb)], writes=[("ps", po)], signal=(kt == nk - 1))
                if kt != nk - 1:
                    return
                hs = slice(h * 64, (h + 1) * 64)
                ob_ = og[blk % 2]
                okey = ("og", blk % 2)
                tk.op("dve", lambda e: e.reciprocal(rl[64:65, :], ps[po][64:65, :]), reads=[("ps", po)], writes=["rl"])

                def epi():
                    tk.op("pe", lambda e: e.matmul(ps[6][0:64, :], self.c32[64:65, C_ONES:C_ONES + 64], rl[64:65, :], start=True, stop=True),
                          reads=["rl", "c32"], writes=[("ps", 6)])
                    tk.op("act", lambda e: e.activation(out=bcs[:], in_=ps[6][0:64, :], func=AF.Copy), reads=[("ps", 6)], writes=["bcs"])
                    tk.op("dve", lambda e: e.tensor_tensor(t1[:], ps[po][0:64, :], bcs[:], ALU.mult),
                          reads=[("ps", po), "bcs"], writes=["t1"])
                    tk.op("dve", lambda e: e.tensor_tensor(ob_[:], t1[:], Gh[b][:, qb * 512:(qb + 1) * 512], ALU.mult),
                          reads=["t1", ("Gh", b)], writes=[okey])
                    tk.dma("sp", lambda e: e.dma_start(out=self.os_[hs, qb * 512:(qb + 1) * 512], in_=ob_[:]),
                           reads=[okey], writes=["os"], ring="aos", nring=2)
                later.append([2, epi])

            loads(0)
            pend = []
            for i, (h, qb, kt) in enumerate(items):
                b = h % 2
                c0 = max(0, kt - 4 * qb) * 128
                pb = i % len(SB)
                sbk = SB[pb]
                P = Pt[pb]
                tk.op("pe", lambda e: e.matmul(ps[sbk][:, c0:512], KhT[b][:, kt * 128:(kt + 1) * 128],
                                               QhT[b][:, qb * 512 + c0:(qb + 1) * 512], start=True, stop=True),
                      reads=[("Kh", b), ("Qh", b)], writes=[("ps", sbk)])
                tk.op("act", lambda e: e.activation(out=P[:, c0:512], in_=ps[sbk][:, c0:512], func=AF.Exp,
                                                    bias=bias[:, qb, kt, h:h + 1], scale=0.125),
                      reads=[("ps", sbk), "bias"], writes=[("P", pb)])
                if kt - 4 * qb >= 0:
                    tk.op("dve", lambda e: e.tensor_tensor(P[:, c0:c0 + 128], P[:, c0:c0 + 128], cmb, ALU.mult),
                          reads=[("P", pb), "cbf"], writes=[("P", pb)])
                tick()
                pend.append(i)
                if len(pend) > L:
                    emit_pv(pend.pop(0))
                if i == head_start[h] + L + 2 and h + 1 < 16:
                    loads(h + 1)
            while pend:
                emit_pv(pend.pop(0))
                tick()
            while later:
                later.pop(0)[1]()
            self.barrier()

    def phase_fox_out(self, l):
        tk, nc, T = self.tk, self.nc, self.T
        j = l // 2
        TF = 512
        with ExitStack() as st:
            w_out = self.sb(st, "o_wout", [128, 8, D], BF16)
            xT = [self.sb(st, f"o_xT{i}", [128, 8, TF], F32) for i in range(2)]
            gT = [self.sb(st, f"o_gT{i}", [128, 8, TF], BF16) for i in range(2)]
            self.load_w(w_out, self.wt("fox_w_out", j).rearrange("(k p) f -> p k f", p=128), "wo", D, 512)
            os_v = self.os_.rearrange("(c p) t -> p c t", p=128)
            for ti in range(T // TF):
                t0 = ti * TF
                b = ti % 2
                tk.dma("sp", lambda e: e.dma_start(out=xT[b][:], in_=self.xsT_v[:, :, t0:t0 + TF]),
                       reads=["xsT"], writes=[("ox", b)], ring="oxl", nring=2)
                tk.dma("sp", lambda e: e.dma_start(out=gT[b][:], in_=os_v[:, :, t0:t0 + TF]),
                       reads=["os"], writes=[("og", b)], ring="ogl", nring=2)
                self.out_proj_residual(w_out, gT[b], ("og", b), xT[b], ("ox", b), TF, banks=(0, 1, 2, 3))
                tk.dma("sp", lambda e: e.dma_start(out=self.xsT_v[:, :, t0:t0 + TF], in_=xT[b][:]),
                       reads=[("ox", b)], writes=["xsT"], ring="oxs", nring=2)
            self.barrier()

    def build_all(self, layers=(0, 1, 2, 3)):
        with ExitStack() as st:
            self.setup(st)

            def mods(st2):
                cond = self.sb(st2, "cond", [128, 8], BF16)
                wa = [self.sb(st2, f"wa{i}", [128, 8, 1536], BF16) for i in range(2)]
                self.mod_bufs = (cond, wa)
                self.tk.op("act", lambda e: e.activation(out=cond[:], in_=self.spv("cT"), func=AF.Silu),
                           reads=["spt"], writes=["cond"])
                for l in layers:
                    self.phase_mod(l)
            self.phase_in(barrier=True, body=mods)
            for l in layers:
                self.cur = l
                if l % 2 == 0:
                    self.phase_gla(l)
                    self.phase_ffn(l, moe=False)
                else:
                    self.phase_fox(l)
                    self.phase_ffn(l, moe=True, dyn=(self.split_last and l == layers[-1]))
            self.phase_out()
            self.tk.drain("sp")
        return self.nc


from concourse.bass_utils import run_bass_kernel_spmd

SEQ = 4096
BATCH = 4
N_CORES = 8
_PROG = {}


def _program():
    if "k" not in _PROG:
        k = K(SEQ, split_last=True)
        k.build_all((0, 1, 2, 3))
        _PROG["k"] = k
    return _PROG["k"]


def kernel(**inputs):
    inp = {n: np.asarray(v) for n, v in inputs.items()}
    k = _program()
    consts = make_consts()
    G = k.G
    in_maps = []
    for c in range(N_CORES):
        b, half = c % BATCH, c // BATCH
        if half == 0:
            d = {"x": np.ascontiguousarray(inp["x"][b], dtype=np.float32), "consts": consts, "sp": pack_small(inp, b)}
            for n in k.w:
                base, j = n.rsplit("_", 1)
                d[n] = np.ascontiguousarray(inp[base][int(j)], dtype=np.float32)
        else:
            d = dict(in_maps[b])
        d["sel"] = np.array([[half, half * G, 0, 0]], np.int32)
        in_maps.append(d)
    res = run_bass_kernel_spmd(k.nc, in_maps, core_ids=list(range(N_CORES)))
    out = np.stack([np.concatenate([np.asarray(res.results[b]["out"], dtype=np.float32)[:G],
                                    np.asarray(res.results[b + BATCH]["out"], dtype=np.float32)[G:]], 0)
                    for b in range(BATCH)], 0)
    return out
```
